# Optimizing a Trainium2 kernel written in Bass

```python
import jax, jax.numpy as jnp
from jax import lax
import numpy as np


D_MODEL = 1024
BATCH = 8
SEQ = 4096
DEPTH = 4

GROUP_W = D_MODEL // 2
HEAD_DIM = 64
N_HEADS = GROUP_W // HEAD_DIM
D_MIX = 3 * GROUP_W
NORM_EPS = 1e-6
DECAY_LORA = 64
ICL_LORA = 64
RWKV_IN = 3 * GROUP_W + DECAY_LORA + ICL_LORA
GN_EPS = 64e-5
CONV_W = 4
LRU_C = 8.0
Q_LORA = D_MODEL // 4
KV_LORA = D_MODEL // 8
QK_NOPE = 64
QK_ROPE = 32
V_DIM = HEAD_DIM
ROPE_BASE = 10000.0
Q_BLOCK = 128
SPLITS = (RWKV_IN, GROUP_W, GROUP_W, GROUP_W, Q_LORA, KV_LORA, QK_ROPE, GROUP_W)
D_IN = RWKV_IN + GROUP_W + GROUP_W + GROUP_W + Q_LORA + KV_LORA + QK_ROPE + GROUP_W

kernel_name = "hybrid_rwkv7_rglru_mla_heads"


def _split_points(sizes):
    pts, acc = [], 0
    for s in sizes[:-1]:
        acc += s
        pts.append(acc)
    return pts


def rms_norm(x, g, eps=NORM_EPS):
    xf = x.astype(jnp.float32)
    y = xf * lax.rsqrt(jnp.mean(xf * xf, axis=-1, keepdims=True) + eps)
    return (y * g.astype(jnp.float32)).astype(x.dtype)


def token_shift(u):
    return jnp.pad(u[:, :-1], ((0, 0), (1, 0), (0, 0)))


def rwkv7_mix(u, mu, w0, w2, a0, a2, k_k, k_a, r_k, lnx_g, lnx_b):
    B, T, _ = u.shape
    u = u.astype(jnp.float32)
    u = u + mu * (token_shift(u) - u)
    r, k, v, wl, al = jnp.split(u, _split_points((GROUP_W, GROUP_W, GROUP_W, DECAY_LORA, ICL_LORA)), axis=-1)
    log_w = -jax.nn.softplus(-(w0 + jnp.tanh(wl) @ w2)) - 0.5
    decay = jnp.exp(-jnp.exp(log_w))
    a = jax.nn.sigmoid(a0 + al @ a2)
    heads = lambda t: t.reshape(B, T, N_HEADS, HEAD_DIM)
    kk = heads(k * k_k)
    kk = kk / jnp.maximum(jnp.sqrt(jnp.sum(kk * kk, axis=-1, keepdims=True)), 1e-12)
    k = k * (1.0 + (a - 1.0) * k_a)
    r_h, w_h, k_h, v_h, a_h = heads(r), heads(decay), heads(k), heads(v), heads(a)
    b_h = kk * a_h

    def step(S, inp):
        r_t, w_t, k_t, v_t, kk_t, b_t = inp
        sa = jnp.einsum('bhij,bhj->bhi', S, kk_t)
        S = S * w_t[:, :, None, :] - sa[..., None] * b_t[:, :, None, :] + v_t[..., None] * k_t[:, :, None, :]
        return S, jnp.einsum('bhij,bhj->bhi', S, r_t)

    xs = tuple(jnp.swapaxes(t, 0, 1) for t in (r_h, w_h, k_h, v_h, kk, b_h))
    S0 = jnp.zeros((B, N_HEADS, HEAD_DIM, HEAD_DIM), jnp.float32)
    _, y = lax.scan(step, S0, xs)
    y = jnp.swapaxes(y, 0, 1)
    mean = jnp.mean(y, axis=-1, keepdims=True)
    var = jnp.mean(jnp.square(y - mean), axis=-1, keepdims=True)
    y = ((y - mean) * lax.rsqrt(var + GN_EPS)).reshape(B, T, GROUP_W) * lnx_g + lnx_b
    bonus = jnp.sum(r_h * k_h * r_k, axis=-1, keepdims=True) * v_h
    return y + bonus.reshape(B, T, GROUP_W)


def rglru_mix(u, conv_w, conv_b, ga_w, ga_b, gx_w, gx_b, lam):
    B, T, _ = u.shape
    uf = u.astype(jnp.float32)
    up = jnp.pad(uf, ((0, 0), (CONV_W - 1, 0), (0, 0)))
    xc = conv_b + sum(up[:, j:j + T] * conv_w[j] for j in range(CONV_W))
    xb = xc.reshape(B, T, N_HEADS, HEAD_DIM)
    gate_r = jax.nn.sigmoid(jnp.einsum('bthi,hij->bthj', xb, ga_w).reshape(B, T, GROUP_W) + ga_b)
    gate_i = jax.nn.sigmoid(jnp.einsum('bthi,hij->bthj', xb, gx_w).reshape(B, T, GROUP_W) + gx_b)
    log_a = -LRU_C * gate_r * jax.nn.softplus(-lam)
    a = jnp.exp(log_a)
    bx = jnp.sqrt(-jnp.expm1(2.0 * log_a)) * (gate_i * xc)

    def combine(lhs, rhs):
        a_l, b_l = lhs
        a_r, b_r = rhs
        return a_l * a_r, a_r * b_l + b_r

    _, h = lax.associative_scan(combine, (a, bx), axis=1)
    return h


def apply_rope(x, cos, sin):
    half = x.shape[-1] // 2
    x1, x2 = x[..., :half], x[..., half:]
    return jnp.concatenate([x1 * cos - x2 * sin, x1 * sin + x2 * cos], axis=-1)


def mla_mix(q_lat, kv_lat, k_rope, q_norm_g, w_uq, kv_norm_g, w_ukv, cos, sin):
    B, T, _ = q_lat.shape
    q = (rms_norm(q_lat, q_norm_g) @ w_uq).reshape(B, T, N_HEADS, QK_NOPE + QK_ROPE)
    q_nope, q_rope = q[..., :QK_NOPE], q[..., QK_NOPE:]
    kv = (rms_norm(kv_lat, kv_norm_g) @ w_ukv).reshape(B, T, N_HEADS, QK_NOPE + V_DIM)
    k_nope, v = kv[..., :QK_NOPE], kv[..., QK_NOPE:]
    q_rope = apply_rope(q_rope, cos[:, None, :], sin[:, None, :])
    k_rope = apply_rope(k_rope, cos, sin)
    scale = (QK_NOPE + QK_ROPE) ** -0.5
    nb = T // Q_BLOCK
    qn_b = jnp.swapaxes(q_nope.reshape(B, nb, Q_BLOCK, N_HEADS, QK_NOPE), 0, 1)
    qr_b = jnp.swapaxes(q_rope.reshape(B, nb, Q_BLOCK, N_HEADS, QK_ROPE), 0, 1)
    k_pos = jnp.arange(T)

    def block(args):
        qn, qr, start = args
        s = jnp.einsum('bqhd,bkhd->bhqk', qn, k_nope) + jnp.einsum('bqhr,bkr->bhqk', qr, k_rope)
        s = s.astype(jnp.float32) * scale
        q_pos = start + jnp.arange(Q_BLOCK)
        s = jnp.where(k_pos[None, :] <= q_pos[:, None], s, -jnp.inf)
        p = jax.nn.softmax(s, axis=-1).astype(v.dtype)
        return jnp.einsum('bhqk,bkhd->bqhd', p, v)

    o = lax.map(block, (qn_b, qr_b, jnp.arange(nb) * Q_BLOCK))
    return jnp.swapaxes(o, 0, 1).reshape(B, T, GROUP_W)


def setup_inputs(seed: int = 0) -> dict:
    key = jax.random.key(seed)
    ks = jax.random.split(key, 32)
    f32 = jnp.float32
    nrm = lambda k, shape, fan_in: jax.random.normal(k, shape, f32) * (fan_in ** -0.5)
    gain = lambda k, shape: 1.0 + 0.02 * jax.random.normal(k, shape, f32)
    small = lambda k, shape: 0.01 * jax.random.normal(k, shape, f32)
    u_lam = jax.random.uniform(ks[21], (DEPTH, GROUP_W), f32, 0.9, 0.999)
    a_lam = u_lam ** (1.0 / LRU_C)
    return {
        'x': jax.random.normal(ks[0], (BATCH, SEQ, D_MODEL), f32),
        'ln_g': gain(ks[1], (DEPTH, D_MODEL)),
        'w_in': nrm(ks[2], (DEPTH, D_MODEL, D_IN), D_MODEL),
        'rwkv_mu': jax.random.uniform(ks[3], (DEPTH, RWKV_IN), f32),
        'rwkv_w0': jax.random.uniform(ks[4], (DEPTH, GROUP_W), f32, -6.0, 1.0),
        'rwkv_w2': nrm(ks[5], (DEPTH, DECAY_LORA, GROUP_W), DECAY_LORA),
        'rwkv_a0': 0.1 * jax.random.normal(ks[6], (DEPTH, GROUP_W), f32),
        'rwkv_a2': nrm(ks[7], (DEPTH, ICL_LORA, GROUP_W), ICL_LORA),
        'rwkv_k_k': 0.85 + 0.05 * jax.random.normal(ks[8], (DEPTH, GROUP_W), f32),
        'rwkv_k_a': 1.0 + 0.05 * jax.random.normal(ks[9], (DEPTH, GROUP_W), f32),
        'rwkv_r_k': 0.1 * jax.random.normal(ks[10], (DEPTH, N_HEADS, HEAD_DIM), f32),
        'rwkv_lnx_g': gain(ks[11], (DEPTH, GROUP_W)),
        'rwkv_lnx_b': small(ks[12], (DEPTH, GROUP_W)),
        'lru_conv_w': nrm(ks[13], (DEPTH, CONV_W, GROUP_W), CONV_W),
        'lru_conv_b': small(ks[14], (DEPTH, GROUP_W)),
        'lru_ga_w': nrm(ks[15], (DEPTH, N_HEADS, HEAD_DIM, HEAD_DIM), HEAD_DIM),
        'lru_ga_b': small(ks[16], (DEPTH, GROUP_W)),
        'lru_gx_w': nrm(ks[17], (DEPTH, N_HEADS, HEAD_DIM, HEAD_DIM), HEAD_DIM),
        'lru_gx_b': small(ks[18], (DEPTH, GROUP_W)),
        'lru_lam': jnp.log(a_lam) - jnp.log1p(-a_lam),
        'lru_out_g': gain(ks[19], (DEPTH, GROUP_W)),
        'mla_q_norm_g': gain(ks[20], (DEPTH, Q_LORA)),
        'mla_w_uq': nrm(ks[22], (DEPTH, Q_LORA, N_HEADS * (QK_NOPE + QK_ROPE)), Q_LORA),
        'mla_kv_norm_g': gain(ks[23], (DEPTH, KV_LORA)),
        'mla_w_ukv': nrm(ks[24], (DEPTH, KV_LORA, N_HEADS * (QK_NOPE + V_DIM)), KV_LORA),
        'mla_out_g': gain(ks[25], (DEPTH, GROUP_W)),
        'w_out': nrm(ks[26], (DEPTH, D_MIX, D_MODEL), D_MIX),
        'final_g': gain(ks[27], (D_MODEL,)),
    }


def reference(x, ln_g, w_in, rwkv_mu, rwkv_w0, rwkv_w2, rwkv_a0, rwkv_a2, rwkv_k_k, rwkv_k_a, rwkv_r_k,
              rwkv_lnx_g, rwkv_lnx_b, lru_conv_w, lru_conv_b, lru_ga_w, lru_ga_b, lru_gx_w, lru_gx_b, lru_lam,
              lru_out_g, mla_q_norm_g, mla_w_uq, mla_kv_norm_g, mla_w_ukv, mla_out_g, w_out, final_g):
    T = x.shape[1]
    half = QK_ROPE // 2
    inv_freq = ROPE_BASE ** (-jnp.arange(half, dtype=jnp.float32) * 2.0 / QK_ROPE)
    ang = jnp.arange(T, dtype=jnp.float32)[:, None] * inv_freq[None, :]
    cos, sin = jnp.cos(ang), jnp.sin(ang)
    pts = _split_points(SPLITS)
    h = x
    for l in range(DEPTH):
        xn = rms_norm(h, ln_g[l])
        proj = xn @ w_in[l]
        u_a, z_a, u_b, z_b, q_lat, kv_lat, k_rope, z_c = jnp.split(proj, pts, axis=-1)
        y_a = rwkv7_mix(u_a, rwkv_mu[l], rwkv_w0[l], rwkv_w2[l], rwkv_a0[l], rwkv_a2[l], rwkv_k_k[l],
                        rwkv_k_a[l], rwkv_r_k[l], rwkv_lnx_g[l], rwkv_lnx_b[l]).astype(h.dtype)
        y_b = rms_norm(rglru_mix(u_b, lru_conv_w[l], lru_conv_b[l], lru_ga_w[l], lru_ga_b[l], lru_gx_w[l],
                                 lru_gx_b[l], lru_lam[l]), lru_out_g[l]).astype(h.dtype)
        y_c = rms_norm(mla_mix(q_lat, kv_lat, k_rope, mla_q_norm_g[l], mla_w_uq[l], mla_kv_norm_g[l],
                               mla_w_ukv[l], cos, sin), mla_out_g[l]).astype(h.dtype)
        y = jnp.concatenate([y_a * jax.nn.silu(z_a), y_b * jax.nn.silu(z_b), y_c * jax.nn.silu(z_c)], axis=-1)
        h = h + y @ w_out[l]
    return rms_norm(h, final_g)
```

```python
import numpy as np
import concourse.bass as bass
import concourse.mybir as mybir

F32 = mybir.dt.float32
BF16 = mybir.dt.bfloat16
ALU = mybir.AluOpType
AF = mybir.ActivationFunctionType
AX = mybir.AxisListType


class Sem:
    def __init__(self, nc, name):
        self.h = nc.alloc_semaphore(name) if hasattr(nc, "alloc_semaphore") else None
        self.name = name
        self.count = 0


class Buf:
    __slots__ = ("t", "name", "last_write", "reads")

    def __init__(self, t, name):
        self.t = t
        self.name = name
        self.last_write = None
        self.reads = {}

    def __getitem__(self, idx):
        return self.t[idx]


SEM_LIMIT = 8000


class Eng:
    def cur_sem(self, i=0):
        sem = self.sems[i]
        if sem.count >= SEM_LIMIT:
            self.nrot = getattr(self, "nrot", 0) + 1
            sem = self.S.new_sem(f"{self.name}_r{self.nrot}")
            self.sems[i] = sem
        return sem

    def __init__(self, S, name, eng, is_dma=False, nsem=1):
        self.S = S
        self.name = name
        self.e = eng
        self.is_dma = is_dma
        self.sems = [S.new_sem(f"{name}_s{i}") for i in range(nsem)]
        self.rr = 0
        self.known = {}
        self.prog = []

    def wait(self, tok):
        sem, val = tok
        if self.known.get(id(sem), 0) >= val:
            return
        e = self.e; h = sem.h
        self.prog.append(lambda: e.wait_ge(h, val))
        self.known[id(sem)] = val
        self.S.nwaits += 1


class Sched:
    def __init__(self, nc):
        self.nc = nc
        import contextlib
        self.es = contextlib.ExitStack()
        self.nwaits = 0
        self.ninst = 0
        self.sem_list = []
        self.pe = Eng(self, "pe", nc.tensor)
        self.dve = Eng(self, "dve", nc.vector)
        self.act = Eng(self, "act", nc.scalar)
        self.pool = Eng(self, "pool", nc.gpsimd)
        self.q_sync = Eng(self, "qsync", nc.sync, is_dma=True, nsem=8)
        self.q_pool = Eng(self, "qpool", nc.gpsimd, is_dma=True, nsem=4)
        self.q_pool.prog = self.pool.prog
        self.engines = [self.pe, self.dve, self.act, self.pool, self.q_sync]

    def new_sem(self, name):
        s = Sem.__new__(Sem)
        s.is_pe = name.startswith("pe_")
        s.name = name
        s.count = 0
        s.h = self.es.enter_context(self.nc.semaphore(name))
        self.sem_list.append(s)
        return s

    def sb(self, name, shape, dtype=F32):
        t = self.nc.alloc_sbuf_tensor(name, list(shape), dtype)
        return Buf(t, name)

    def ps(self, name, shape, dtype=F32):
        t = self.nc.alloc_psum_tensor(name, list(shape), dtype)
        return Buf(t, name)

    def dram(self, name, shape, dtype=F32, kind="Internal"):
        t = self.nc.dram_tensor(name, list(shape), dtype, kind=kind)
        return Buf(t, name)

    def _deps(self, reads, writes):
        deps = []
        for b in reads:
            if b.last_write is not None:
                deps.append(b.last_write)
        for b in writes:
            if b.last_write is not None:
                deps.append(b.last_write)
            deps.extend(b.reads.values())
        return deps

    def _commit(self, tok, reads, writes):
        for b in reads:
            if b not in writes:
                b.reads[id(tok[0])] = tok
        for b in writes:
            b.last_write = tok
            b.reads = {}

    def op(self, eng, fn, reads=(), writes=()):
        for tok in self._deps(reads, writes):
            if eng is self.pe and tok[0].is_pe:
                continue
            eng.wait(tok)
        sem = eng.cur_sem()
        sem.count += 1
        h = sem.h
        eng.prog.append(lambda: fn().then_inc(h, 1))
        tok = (sem, sem.count)
        self._commit(tok, reads, writes)
        self.ninst += 1
        return tok

    def group(self, eng, fns, reads=(), writes=()):
        for tok in self._deps(reads, writes):
            if eng is self.pe and tok[0].is_pe:
                continue
            eng.wait(tok)
        fns = list(fns)
        for fn in fns[:-1]:
            eng.prog.append(fn)
            self.ninst += 1
        self.ninst += 1
        sem = eng.cur_sem()
        sem.count += 1
        h = sem.h
        last = fns[-1]
        eng.prog.append(lambda: last().then_inc(h, 1))
        tok = (sem, sem.count)
        self._commit(tok, reads, writes)
        return tok

    def dma(self, q, out_ap, in_ap, R=(), W=(), **kw):
        i = q.rr % len(q.sems)
        sem = q.sems[i]
        q.rr += 1
        if sem.count > 0:
            q.wait((sem, sem.count))
        sem = q.cur_sem(i)
        for tok in self._deps(R, W):
            q.wait(tok)
        sem.count += 16
        h = sem.h; e = q.e
        q.prog.append(lambda: e.dma_start(out=out_ap, in_=in_ap, **kw).then_inc(h, 16))
        tok = (sem, sem.count)
        self._commit(tok, R, W)
        self.ninst += 1
        return tok

    def ts(self, eng, out, in0, s1, s2=None, op0=ALU.mult, op1=None, R=(), W=(), accum=None):
        e = eng.e
        kw = {}
        if op1 is not None: kw["op1"] = op1
        if accum is not None: kw["accum_out"] = accum
        return self.op(eng, lambda: e.tensor_scalar(out=out, in0=in0, scalar1=s1, scalar2=s2, op0=op0, **kw), R, W)

    def tt(self, eng, out, in0, in1, op, R=(), W=()):
        e = eng.e
        return self.op(eng, lambda: e.tensor_tensor(out=out, in0=in0, in1=in1, op=op), R, W)

    def stt(self, eng, out, in0, scalar, in1, op0, op1, R=(), W=()):
        e = eng.e
        return self.op(eng, lambda: e.scalar_tensor_tensor(out=out, in0=in0, scalar=scalar, in1=in1, op0=op0, op1=op1), R, W)

    def cp(self, eng, out, in_, R=(), W=()):
        e = eng.e
        if eng is self.act:
            return self.op(eng, lambda: e.copy(out=out, in_=in_), R, W)
        return self.op(eng, lambda: e.tensor_copy(out=out, in_=in_), R, W)

    def actf(self, out, in_, func, R=(), W=(), scale=1.0, bias=None, accum=None):
        e = self.act.e
        kw = {}
        if bias is not None: kw["bias"] = bias
        if accum is not None: kw["accum_out"] = accum
        return self.op(self.act, lambda: e.activation(out=out, in_=in_, func=func, scale=scale, **kw), R, W)

    def mm(self, out, pairs, R=(), W=(), start=True, stop=True, **kw):
        e = self.pe.e
        n = len(pairs)
        fns = []
        for i, (l, r) in enumerate(pairs):
            st = start and i == 0
            sp = stop and i == n - 1
            fns.append((lambda l=l, r=r, st=st, sp=sp: e.matmul(out, lhsT=l, rhs=r, start=st, stop=sp, **kw)))
        return self.group(self.pe, fns, R, W)

    def tr(self, outs_ins, ident, R=(), W=()):
        e = self.pe.e
        fns = [(lambda o=o, i=i: e.transpose(out=o, in_=i, identity=ident)) for (o, i) in outs_ins]
        return self.group(self.pe, fns, R, W)

    def memset(self, eng, ap, val, W=()):
        e = eng.e
        return self.op(eng, lambda: e.memset(ap, val), (), W)

    def barrier(self):
        toks = [(s, s.count) for s in self.sem_list if s.count > 0]
        for eng in [self.pe, self.dve, self.act, self.pool, self.q_sync]:
            for tok in toks:
                eng.wait(tok)

    def finish(self, bufs):
        for b in bufs:
            if b.last_write is not None:
                self.q_sync.wait(b.last_write)


def emit_all(S):
    nc = S.nc
    def run(prog):
        def f(e):
            for g in prog: g()
        return f
    with nc.Block() as block:
        if S.q_sync.prog: block.sync(run(S.q_sync.prog))
        if S.pe.prog: block.tensor(run(S.pe.prog))
        if S.dve.prog: block.vector(run(S.dve.prog))
        if S.act.prog: block.scalar(run(S.act.prog))
        if S.pool.prog: block.gpsimd(run(S.pool.prog))
    S.es.close()


def _prune(reads):
    best = {}
    for sem, val in reads:
        k = id(sem)
        if k not in best or best[k][1] < val:
            best[k] = (sem, val)
    return list(best.values())

D = 1024
GW = 512
NH = 8
HD = 64
RW_IN = 1664
NA = 2176
NB = 1984
QL = 256
KVL = 128
DMIX = 1536

OB_UB, OB_ZB, OB_QL, OB_KV, OB_ZC, OB_KR, OB_KRS = 0, 512, 1024, 1280, 1408, 1920, 1952

RV_MU, RV_KK, RV_KA, RV_RK, RV_LG, RV_LB = 0, 1664, 2176, 2688, 3200, 3712
NRV = 4224
CV_LNG = 0
CV_CW = 8
CV_CB = 24
CV_GAB = 28
CV_GXB = 32
CV_LAM = 36
CV_LOG = 40
CV_QNG = 44
CV_KVNG = 46
CV_MOG = 47
NCV = 52


def host_prep(inp, T):
    f = np.float32
    L = inp["w_in"].shape[0]
    out = {}
    w_in = np.asarray(inp["w_in"], f)
    out["wA"] = np.ascontiguousarray(w_in[:, :, 0:NA])
    c = 2176
    colsB = np.concatenate([
        np.arange(c, c + 512), np.arange(c + 512, c + 1024), np.arange(3200, 3456), np.arange(3456, 3584),
        np.arange(3616, 4128), np.arange(3584, 3616), np.arange(3600, 3616), np.arange(3584, 3600)])
    assert colsB.size == NB
    out["wB"] = np.ascontiguousarray(w_in[:, :, colsB])
    rv = np.zeros((L, NRV), f)
    rv[:, RV_MU:RV_MU + 1664] = inp["rwkv_mu"]
    rv[:, RV_KK:RV_KK + 512] = inp["rwkv_k_k"]
    rv[:, RV_KA:RV_KA + 512] = inp["rwkv_k_a"]
    rv[:, RV_RK:RV_RK + 512] = np.asarray(inp["rwkv_r_k"], f).reshape(L, 512)
    rv[:, RV_LG:RV_LG + 512] = inp["rwkv_lnx_g"]
    rv[:, RV_LB:RV_LB + 512] = inp["rwkv_lnx_b"]
    out["rv"] = rv
    out["w0a0"] = np.ascontiguousarray(np.concatenate([np.asarray(inp["rwkv_w0"], f), np.asarray(inp["rwkv_a0"], f)], axis=1))
    out["w2a2"] = np.ascontiguousarray(np.concatenate([np.asarray(inp["rwkv_w2"], f), np.asarray(inp["rwkv_a2"], f)], axis=1))
    cv = np.zeros((L, 128, NCV), f)
    def colfill(off, vec, n):
        cv[:, :, off:off + n] = np.asarray(vec, f).reshape(L, n, 128).transpose(0, 2, 1)
    colfill(CV_LNG, inp["ln_g"], 8)
    cw = np.asarray(inp["lru_conv_w"], f)
    for j in range(4):
        colfill(CV_CW + j * 4, cw[:, j], 4)
    colfill(CV_CB, inp["lru_conv_b"], 4)
    colfill(CV_GAB, inp["lru_ga_b"], 4)
    colfill(CV_GXB, inp["lru_gx_b"], 4)
    colfill(CV_LAM, inp["lru_lam"], 4)
    colfill(CV_LOG, inp["lru_out_g"], 4)
    colfill(CV_QNG, inp["mla_q_norm_g"], 2)
    colfill(CV_KVNG, inp["mla_kv_norm_g"], 1)
    colfill(CV_MOG, inp["mla_out_g"], 4)
    out["cv"] = cv
    gw = np.zeros((L, 4, 128, 2, 128), f)
    for gi, nm in enumerate(["lru_ga_w", "lru_gx_w"]):
        w = np.asarray(inp[nm], f)
        for h in range(8):
            ct, o = h // 2, (h % 2) * 64
            gw[:, ct, o:o + 64, gi, o:o + 64] = w[:, h]
    out["gw"] = gw
    wuq = np.asarray(inp["mla_w_uq"], f).reshape(L, 256, 8, 96)
    wq = np.zeros((L, 256, 8, 128), f)
    wq[..., 0:32] = wuq[..., 64:96]
    wq[..., 64:128] = wuq[..., 0:64]
    out["wq"] = np.ascontiguousarray(wq.reshape(L, 256, 1024))
    wqs = np.zeros((L, 256, 8, 128), f)
    wqs[..., 0:16] = wuq[..., 80:96]
    wqs[..., 16:32] = wuq[..., 64:80]
    out["wqs"] = np.ascontiguousarray(wqs.reshape(L, 256, 1024))
    wukv = np.asarray(inp["mla_w_ukv"], f).reshape(L, 128, 8, 128)
    wk = np.zeros((L, 128, 8, 128), f)
    wk[..., 64:128] = wukv[..., 0:64]
    out["wk"] = np.ascontiguousarray(wk.reshape(L, 128, 1024))
    out["wv"] = np.ascontiguousarray(wukv[..., 64:128].reshape(L, 128, 512))
    out["w_out"] = np.asarray(inp["w_out"], f)
    out["final_g"] = np.asarray(inp["final_g"], f).reshape(1, 1024)
    import ml_dtypes
    bf = ml_dtypes.bfloat16
    out["ident_bf"] = np.eye(128, dtype=f).astype(bf)
    out["ident_f"] = np.eye(128, dtype=f)
    half = 16
    inv_freq = (10000.0 ** (-np.arange(half, dtype=f) * 2.0 / 32)).astype(f)
    ang = np.arange(T, dtype=f)[:, None] * inv_freq[None, :]
    cos, sin = np.cos(ang).astype(f).T, np.sin(ang).astype(f).T
    cs = np.zeros((128, 2, T), f)
    cs[32:128, 0] = 1.0
    cs[0:16, 0], cs[16:32, 0] = cos, cos
    cs[0:16, 1], cs[16:32, 1] = -sin, sin
    out["cs"] = cs
    ii = np.arange(128)
    m = np.zeros((128, 4, 128), f)
    m[:, 0] = (ii[:, None] < ii[None, :])
    m[:, 1] = (ii[:, None] <= ii[None, :])
    m[:, 2] = (ii[:, None] > ii[None, :])
    m[:, 3] = 1.0
    out["masks"] = m
    out["mask_bf"] = (ii[:, None] <= ii[None, :]).astype(f).astype(bf)
    return out

import contextlib

EPS = 1e-6


class Ctx:
    pass


def declare_inputs(S, T, L):
    C = Ctx()
    di = lambda n, shp, dt=F32: S.dram(n, shp, dt, kind="ExternalInput")
    C.x = di("x", [T, D])
    C.wA = di("wA", [L, D, NA]); C.wB = di("wB", [L, D, NB])
    C.rv = di("rv", [L, NRV]); C.w0a0 = di("w0a0", [L, 1024]); C.w2a2 = di("w2a2", [L, 128, 512])
    C.cv = di("cv", [L, 128, NCV]); C.gw = di("gw", [L, 4, 128, 2, 128])
    C.wq = di("wq", [L, 256, 1024]); C.wqs = di("wqs", [L, 256, 1024])
    C.wk = di("wk", [L, 128, 1024]); C.wv = di("wv", [L, 128, 512])
    C.w_out = di("w_out", [L, DMIX, D]); C.final_g = di("final_g", [1, D])
    C.ident_bf = di("ident_bf", [128, 128], BF16); C.ident_f = di("ident_f", [128, 128])
    C.cs = di("cs", [128, 2, T]); C.masks = di("masks", [128, 4, 128]); C.mask_bf = di("mask_bf", [128, 128], BF16)
    return C


class Scope:
    def __init__(self, S):
        self.S = S
        self.es = contextlib.ExitStack()

    _n = [0]

    def sb(self, name, shape, dtype=F32):
        Scope._n[0] += 1
        name = f"{name}_u{Scope._n[0]}"
        t = self.es.enter_context(self.S.nc.sbuf_tensor(name, list(shape), dtype))
        return Buf(t, name)

    def close(self):
        self.S.barrier()
        self.es.close()


def load_consts(S, C, G):
    nc = S.nc
    G.ident_bf = S.sb("ident_bf_sb", [128, 128], BF16)
    G.ident_f = S.sb("ident_f_sb", [128, 128])
    G.masks = S.sb("masks_sb", [128, 4, 128])
    G.mask_bf = S.sb("mask_bf_sb", [128, 128], BF16)
    G.ones_f = S.sb("ones_f", [128, 128])
    S.dma(S.q_sync, G.ident_bf[:], C.ident_bf[:], W=[G.ident_bf])
    S.dma(S.q_sync, G.ident_f[:], C.ident_f[:], W=[G.ident_f])
    S.dma(S.q_sync, G.masks[:], C.masks[:], W=[G.masks])
    S.dma(S.q_sync, G.mask_bf[:], C.mask_bf[:], W=[G.mask_bf])
    S.memset(S.dve, G.ones_f[:], 1.0, W=[G.ones_f])
    G.ps = [S.ps(f"ps{i}", [128, 512], F32) for i in range(6)]
    G.psb = [S.ps(f"psb{i}", [128, 1024], BF16) for i in range(2)]


def rr(lst, state, key):
    i = state.get(key, 0)
    state[key] = i + 1
    return lst[i % len(lst)]


def phase_in_proj(S, C, G, l, hsrc, pa, pbT, T):
    nc = S.nc
    sc = Scope(S)
    st = {}
    wA = sc.sb("wA_sb", [128, 8, NA], BF16)
    wB = sc.sb("wB_sb", [128, 8, NB], BF16)
    cvt = sc.sb("p1_cv", [128, NCV])
    S.dma(S.q_sync, cvt[:], C.cv[l], W=[cvt])
    stg = [sc.sb(f"p1_stg{i}", [128, NA]) for i in range(2)]
    wAv = C.wA[l].rearrange("(k p) c -> p k c", p=128)
    wBv = C.wB[l].rearrange("(k p) c -> p k c", p=128)
    n = 0
    for k in range(8):
        for (src, dst, nc_) in ((wAv, wA, NA), (wBv, wB, NB)):
            sg = stg[n % 2]
            S.dma(S.q_sync if n % 2 == 0 else S.q_pool, sg[:, 0:nc_], src[:, k, :], W=[sg])
            eng = S.dve if n % 2 == 0 else S.pool
            S.ts(eng, dst[:, k, :], sg[:, 0:nc_], cvt[:, CV_LNG + k:CV_LNG + k + 1], R=[sg, cvt], W=[dst])
            n += 1
    hts = [sc.sb(f"p1_h{i}", [128, D]) for i in range(3)]
    junk = sc.sb("p1_junk", [128, D], BF16)
    xnb = [sc.sb(f"p1_xnb{i}", [128, D], BF16) for i in range(2)]
    ss = [sc.sb(f"p1_ss{i}", [128, 4]) for i in range(2)]
    xnT = [sc.sb(f"p1_xnT{i}", [128, 8, 512], BF16) for i in range(2)]
    oA = [sc.sb(f"p1_oA{i}", [128, NA]) for i in range(2)]
    oB = [sc.sb(f"p1_oB{i}", [128, 512]) for i in range(3)]
    nblk = T // 512
    ev = 0
    for j in range(nblk):
        xT = xnT[j % 2]
        for tt in range(4):
            t = 4 * j + tt
            ht = rr(hts, st, "h")
            S.dma(S.q_sync, ht[:], hsrc[t * 128:(t + 1) * 128, :], R=[hsrc], W=[ht])
            s_ = rr(ss, st, "ss")
            S.actf(junk[:], ht[:], AF.Square, R=[ht], W=[junk, s_], accum=s_[:, 0:1])
            S.actf(s_[:, 1:2], s_[:, 0:1], AF.Sqrt, R=[s_], W=[s_], scale=1.0 / D, bias=G.eps_col[:, 0:1])
            S.op(S.dve, lambda a=s_: nc.vector.reciprocal(out=a[:, 2:3], in_=a[:, 1:2]), [s_], [s_])
            xb = rr(xnb, st, "xnb")
            S.ts(S.dve, xb[:], ht[:], s_[:, 2:3], R=[ht, s_], W=[xb])
            pb = rr(G.psb, st, "psb")
            S.tr([(pb[:, k * 128:(k + 1) * 128], xb[:, k * 128:(k + 1) * 128]) for k in range(8)], G.ident_bf[:],
                 R=[xb, G.ident_bf], W=[pb])
            S.cp(S.act, xT[:, :, tt * 128:(tt + 1) * 128], pb[:].rearrange("p (k c) -> p k c", k=8), R=[pb], W=[xT])
            o = rr(oA, st, "oA")
            c0 = 0
            while c0 < NA:
                cn = min(512, NA - c0)
                p = rr(G.ps, st, "ps")
                S.mm(p[:, 0:cn], [(xT[:, k, tt * 128:(tt + 1) * 128], wA[:, k, c0:c0 + cn]) for k in range(8)],
                     R=[xT, wA], W=[p])
                S.cp(S.act if ev % 2 == 0 else S.dve, o[:, c0:c0 + cn], p[:, 0:cn], R=[p], W=[o])
                ev += 1
                c0 += cn
            S.dma(S.q_sync, pa[t * 128:(t + 1) * 128, :], o[:], R=[o], W=[pa])
        r0 = 0
        while r0 < NB:
            rn = min(128, NB - r0)
            p = rr(G.ps, st, "ps")
            S.mm(p[0:rn, :], [(wB[:, k, r0:r0 + rn], xT[:, k, :]) for k in range(8)], R=[xT, wB], W=[p])
            o = rr(oB, st, "oB")
            S.cp(S.act if ev % 2 == 0 else S.dve, o[0:rn, :], p[0:rn, :], R=[p], W=[o])
            ev += 1
            S.dma(S.q_pool, pbT[r0:r0 + rn, j * 512:(j + 1) * 512], o[0:rn, :], R=[o], W=[pbT])
            r0 += rn
    sc.close()


def phase_lru(S, C, G, l, pbT, ybT, T):
    nc = S.nc
    sc = Scope(S)
    st = {}
    cvt = sc.sb("p3_cv", [128, NCV])
    S.dma(S.q_sync, cvt[:], C.cv[l], W=[cvt])
    gwt = sc.sb("p3_gw", [128, 4, 2, 128])
    S.dma(S.q_sync, gwt[:], C.gw[l].rearrange("c p g q -> p c g q"), W=[gwt])
    c8 = sc.sb("p3_c8", [128, 12])
    S.actf(c8[:, 0:4], cvt[:, CV_LAM:CV_LAM + 4], AF.Exp, R=[cvt], W=[c8], scale=-1.0)
    S.actf(c8[:, 4:8], c8[:, 0:4], AF.Ln, R=[c8], W=[c8], bias=1.0)
    S.ts(S.dve, c8[:, 8:12], c8[:, 4:8], -8.0, R=[c8], W=[c8])
    ubs = [sc.sb(f"p3_ub{i}", [128, 515]) for i in range(3)]
    xcs = [sc.sb(f"p3_xc{i}", [128, 512]) for i in range(2)]
    grs = [sc.sb(f"p3_gr{i}", [128, 512]) for i in range(2)]
    gis = [sc.sb(f"p3_gi{i}", [128, 512]) for i in range(2)]
    a_s = [sc.sb(f"p3_a{i}", [128, 512]) for i in range(2)]
    oms = [sc.sb(f"p3_om{i}", [128, 512]) for i in range(2)]
    bxs = [sc.sb(f"p3_bx{i}", [128, 512]) for i in range(2)]
    hos = [sc.sb(f"p3_ho{i}", [128, 512]) for i in range(3)]
    zero = sc.sb("p3_zero", [128, 1])
    S.memset(S.dve, zero[:], 0.0, W=[zero])
    nblk = T // 512
    for ct in range(4):
        r0 = OB_UB + ct * 128
        cw = lambda j: cvt[:, CV_CW + j * 4 + ct:CV_CW + j * 4 + ct + 1]
        col = lambda off: cvt[:, off + ct:off + ct + 1]
        hprev, hprev_buf = zero[:, 0:1], zero
        for j in range(nblk):
            ub = rr(ubs, st, "ub")
            if j == 0:
                S.memset(S.pool, ub[:, 0:3], 0.0, W=[ub])
                S.dma(S.q_sync, ub[:, 3:515], pbT[r0:r0 + 128, 0:512], R=[pbT], W=[ub])
            else:
                S.dma(S.q_sync, ub[:], pbT[r0:r0 + 128, j * 512 - 3:(j + 1) * 512], R=[pbT], W=[ub])
            xc = rr(xcs, st, "xc")
            S.ts(S.dve, xc[:], ub[:, 0:512], cw(0), col(CV_CB), op0=ALU.mult, op1=ALU.add, R=[ub, cvt], W=[xc])
            for jj in range(1, 4):
                S.stt(S.dve, xc[:], ub[:, jj:jj + 512], cw(jj), xc[:], ALU.mult, ALU.add, R=[ub, cvt, xc], W=[xc])
            p1 = rr(G.ps, st, "ps"); p2 = rr(G.ps, st, "ps")
            S.mm(p1[:], [(gwt[:, ct, 0, :], xc[:])], R=[gwt, xc], W=[p1])
            S.mm(p2[:], [(gwt[:, ct, 1, :], xc[:])], R=[gwt, xc], W=[p2])
            gr = rr(grs, st, "gr"); gi = rr(gis, st, "gi")
            S.actf(gr[:], p1[:], AF.Sigmoid, R=[p1, cvt], W=[gr], bias=col(CV_GAB))
            S.actf(gi[:], p2[:], AF.Sigmoid, R=[p2, cvt], W=[gi], bias=col(CV_GXB))
            a = rr(a_s, st, "a"); om = rr(oms, st, "om"); bx = rr(bxs, st, "bx")
            S.actf(a[:], gr[:], AF.Exp, R=[gr, c8], W=[a], scale=c8[:, 8 + ct:9 + ct])
            S.tt(S.pool, bx[:], gi[:], xc[:], ALU.mult, R=[gi, xc], W=[bx])
            S.tt(S.dve, om[:], a[:], a[:], ALU.mult, R=[a], W=[om])
            S.actf(om[:], om[:], AF.Sqrt, R=[om], W=[om], scale=-1.0, bias=1.0)
            S.tt(S.pool, bx[:], bx[:], om[:], ALU.mult, R=[bx, om], W=[bx])
            ho = rr(hos, st, "ho")
            S.op(S.dve, lambda ho=ho, a=a, bx=bx, hp=hprev: nc.vector.tensor_tensor_scan(
                out=ho[:], data0=a[:], data1=bx[:], initial=hp, op0=ALU.mult, op1=ALU.add),
                [a, bx, hprev_buf], [ho])
            S.dma(S.q_pool, ybT[ct * 128:(ct + 1) * 128, j * 512:(j + 1) * 512], ho[:], R=[ho], W=[ybT])
            hprev, hprev_buf = ho[:, 511:512], ho
    sc.close()


def phase_mla(S, C, G, l, pbT, ycT, T):
    nc = S.nc
    sc = Scope(S)
    st = {}
    nblk = T // 512
    cvt = sc.sb("p4_cv", [128, NCV])
    S.dma(S.q_sync, cvt[:], C.cv[l], W=[cvt])
    wq = sc.sb("p4_wq", [128, 2, 1024], BF16)
    wqs = sc.sb("p4_wqs", [128, 2, 1024], BF16)
    wk = sc.sb("p4_wk", [128, 1024], BF16)
    wv = sc.sb("p4_wv", [128, 512], BF16)
    sc2 = Scope(S)
    stg = [sc2.sb(f"p4_stg{i}", [128, 1024]) for i in range(2)]
    jobs = [(C.wq[l][0:128, :], wq[:, 0, :], 1024, wq), (C.wq[l][128:256, :], wq[:, 1, :], 1024, wq),
            (C.wqs[l][0:128, :], wqs[:, 0, :], 1024, wqs), (C.wqs[l][128:256, :], wqs[:, 1, :], 1024, wqs),
            (C.wk[l], wk[:], 1024, wk), (C.wv[l], wv[:], 512, wv)]
    for n, (src, dst, ncol, dbuf) in enumerate(jobs):
        sg = stg[n % 2]
        S.dma(S.q_sync, sg[:, 0:ncol], src, W=[sg])
        S.cp(S.dve if n % 2 == 0 else S.pool, dst, sg[:, 0:ncol], R=[sg], W=[dbuf])
    sc2.close()
    E65 = sc.sb("p4_E96", [128, 64])
    S.memset(S.dve, E65[:], 0.0, W=[E65])
    S.memset(S.dve, E65[64:65, :], 1.0, W=[E65])
    KT = [sc.sb(f"p4_KT{j}", [128, 8, 512], BF16) for j in range(nblk)]
    VP = [sc.sb(f"p4_VP{j}", [128, 4, 8, 96], BF16) for j in range(nblk)]
    for j in range(nblk):
        S.memset(S.pool, KT[j][32:64, :, :], 0.0, W=[KT[j]])
        S.memset(S.pool, VP[j][:, :, :, 64:96], 0.0, W=[VP[j]])
        S.memset(S.pool, VP[j][:, :, :, 64:65], 1.0, W=[VP[j]])
    QT = [[sc.sb(f"p4_QT{b}_{h}", [128, 512], BF16) for h in range(8)] for b in range(1)]
    cst = [sc.sb(f"p4_cs{i}", [128, 2, 512]) for i in range(1)]
    qls = [sc.sb(f"p4_ql{i}", [128, 2, 512]) for i in range(1)]
    kvls = [sc.sb(f"p4_kvl{i}", [128, 512]) for i in range(2)]
    krs = [sc.sb(f"p4_kr{i}", [32, 2, 512]) for i in range(2)]
    sq = sc.sb("p4_sq", [128, 2, 512])
    rq = sc.sb("p4_rq", [128, 512]); rk = sc.sb("p4_rk", [128, 512])
    qn = sc.sb("p4_qn", [128, 2, 512], BF16)
    ckv = sc.sb("p4_ckv", [128, 512], BF16)
    t1s = [sc.sb(f"p4_t1{i}", [128, 512]) for i in range(2)]
    t2s = [sc.sb(f"p4_t2{i}", [128, 512]) for i in range(2)]
    pTs = [sc.sb(f"p4_pT{i}", [128, 512], BF16) for i in range(3)]
    osbs = [sc.sb(f"p4_osb{i}", [96, 512]) for i in range(2)]
    rls = [sc.sb(f"p4_rl{i}", [64, 512]) for i in range(2)]
    ohs = [sc.sb(f"p4_oh{i}", [64, 512]) for i in range(2)]
    psS = G.ps[0:3]; psO = G.ps[3:5]; psL = G.ps[5]
    scale = 96.0 ** -0.5
    for j in range(nblk):
        blk = slice(j * 512, (j + 1) * 512)
        ql = rr(qls, st, "ql"); kvl = rr(kvls, st, "kvl"); kr = rr(krs, st, "kr"); cs = rr(cst, st, "cs")
        for k in range(2):
            S.dma(S.q_sync, ql[:, k, :], pbT[OB_QL + k * 128:OB_QL + (k + 1) * 128, blk], R=[pbT], W=[ql])
        S.dma(S.q_sync, kvl[:], pbT[OB_KV:OB_KV + 128, blk], R=[pbT], W=[kvl])
        S.dma(S.q_pool, kr[:, 0, :], pbT[OB_KR:OB_KR + 32, blk], R=[pbT], W=[kr])
        S.dma(S.q_pool, kr[:, 1, :], pbT[OB_KRS:OB_KRS + 32, blk], R=[pbT], W=[kr])
        S.dma(S.q_pool, cs[:], C.cs[:, :, blk], W=[cs])
        S.actf(sq[:], ql[:], AF.Square, R=[ql], W=[sq])
        pA = rr(psS, st, "psS")
        S.mm(pA[:], [(G.ones_f[:], sq[:, 0, :]), (G.ones_f[:], sq[:, 1, :])], R=[G.ones_f, sq], W=[pA])
        S.actf(rq[:], pA[:], AF.Sqrt, R=[pA], W=[rq], scale=1.0 / QL, bias=G.eps_col[:, 0:1])
        S.op(S.dve, lambda: nc.vector.reciprocal(out=rq[:], in_=rq[:]), [rq], [rq])
        for k in range(2):
            S.stt(S.dve, qn[:, k, :], ql[:, k, :], cvt[:, CV_QNG + k:CV_QNG + k + 1], rq[:], ALU.mult, ALU.mult,
                  R=[ql, cvt, rq], W=[qn])
        S.actf(sq[:, 0, :], kvl[:], AF.Square, R=[kvl], W=[sq])
        pB = rr(psS, st, "psS")
        S.mm(pB[:], [(G.ones_f[:], sq[:, 0, :])], R=[G.ones_f, sq], W=[pB])
        S.actf(rk[:], pB[:], AF.Sqrt, R=[pB], W=[rk], scale=1.0 / KVL, bias=G.eps_col[:, 0:1])
        S.op(S.dve, lambda: nc.vector.reciprocal(out=rk[:], in_=rk[:]), [rk], [rk])
        S.stt(S.dve, ckv[:], kvl[:], cvt[:, CV_KVNG:CV_KVNG + 1], rk[:], ALU.mult, ALU.mult, R=[kvl, cvt, rk], W=[ckv])
        t1 = rr(t1s, st, "t1"); t2 = rr(t2s, st, "t2")
        S.tt(S.dve, t1[0:32, :], kr[:, 0, :], cs[0:32, 0, :], ALU.mult, R=[kr, cs], W=[t1])
        S.tt(S.pool, t2[0:32, :], kr[:, 1, :], cs[0:32, 1, :], ALU.mult, R=[kr, cs], W=[t2])
        for h in range(8):
            S.tt(S.pool if h % 2 else S.dve, KT[j][0:32, h, :], t1[0:32, :], t2[0:32, :], ALU.add, R=[t1, t2], W=[KT[j]])
        for h in range(8):
            Qh = QT[0][h]
            pq = rr(psS, st, "psS")
            S.mm(pq[:], [(wq[:, k, h * 128:(h + 1) * 128], qn[:, k, :]) for k in range(2)], R=[wq, qn], W=[pq])
            pqs = rr(psS, st, "psS")
            S.mm(pqs[:], [(wqs[:, k, h * 128:(h + 1) * 128], qn[:, k, :]) for k in range(2)], R=[wqs, qn], W=[pqs])
            t1 = rr(t1s, st, "t1"); t2 = rr(t2s, st, "t2")
            S.tt(S.dve, t1[:], pq[:], cs[:, 0, :], ALU.mult, R=[pq, cs], W=[t1])
            S.tt(S.dve, t2[:], pqs[:], cs[:, 1, :], ALU.mult, R=[pqs, cs], W=[t2])
            S.tt(S.pool, Qh[:], t1[:], t2[:], ALU.add, R=[t1, t2], W=[Qh])
            pk = rr(psS, st, "psS")
            S.mm(pk[:], [(wk[:, h * 128:(h + 1) * 128], ckv[:])], R=[wk, ckv], W=[pk])
            S.cp(S.act, KT[j][64:128, h, :], pk[64:128, :], R=[pk], W=[KT[j]])
        for tt in range(4):
            pv = rr(psS, st, "psS")
            S.mm(pv[:], [(ckv[:, tt * 128:(tt + 1) * 128], wv[:])], R=[ckv, wv], W=[pv])
            S.cp(S.act if tt % 2 else S.dve, VP[j][:, tt, :, 0:64], pv[:].rearrange("p (h d) -> p h d", h=8),
                 R=[pv], W=[VP[j]])
        nkt = 4 * j + 4
        for h in range(8):
            Qh = QT[0][h]
            po = rr(psO, st, "psO")
            for kt in range(nkt):
                i = kt - 4 * j
                c0 = 128 * max(i, 0)
                N = 512 - c0
                kb, ko = kt // 4, (kt % 4) * 128
                pS = rr(psS, st, "psS")
                S.mm(pS[:, 0:N], [(KT[kb][:, h, ko:ko + 128], Qh[:, c0:512])], R=[KT[kb], Qh], W=[pS])
                pT = rr(pTs, st, "pT")
                S.actf(pT[:, 0:N], pS[:, 0:N], AF.Exp, R=[pS], W=[pT], scale=scale)
                if i >= 0:
                    S.tt(S.pool, pT[:, 0:128], pT[:, 0:128], G.mask_bf[:], ALU.mult, R=[pT, G.mask_bf], W=[pT])
                S.mm(po[0:96, c0:512], [(VP[kb][:, kt % 4, h, :], pT[:, 0:N])], R=[VP[kb], pT], W=[po],
                     start=(kt == 0), stop=(kt == nkt - 1))
            osb = rr(osbs, st, "osb")
            S.cp(S.act, osb[:], po[0:96, :], R=[po], W=[osb])
            S.mm(psL[0:64, :], [(E65[0:96, :], osb[0:96, :])], R=[E65, osb], W=[psL])
            rl = rr(rls, st, "rl")
            S.op(S.dve, lambda rl=rl: nc.vector.reciprocal(out=rl[:], in_=psL[0:64, :]), [psL], [rl])
            oh = rr(ohs, st, "oh")
            S.tt(S.pool, oh[:], osb[0:64, :], rl[:], ALU.mult, R=[osb, rl], W=[oh])
            S.dma(S.q_sync, ycT[h * 64:(h + 1) * 64, blk], oh[:], R=[oh], W=[ycT])
    sc.close()


INV_DT = F32
CDEC = 0.6065306597126334
GN_EPS = 64e-5


def bc_mid(ap, n):
    return ap.unsqueeze(1).broadcast_to([ap.shape[0], n, ap.shape[1]])


def bc_last(ap, n):
    return ap.unsqueeze(2).broadcast_to([ap.shape[0], ap.shape[1], n])


def h3(ap):
    return ap.rearrange("p (h d) -> p h d", h=8)


def phase_rwkv(S, C, G, l, pa, yga, T, dbg=None):
    nc = S.nc
    sc = Scope(S)
    st = {}
    nch = T // 128
    rvt = sc.sb("p2_rv", [128, NRV])
    S.dma(S.q_sync, rvt[:], C.rv[l:l + 1, :].partition_broadcast(128), W=[rvt])
    w0a0 = sc.sb("p2_w0a0", [1, 1024])
    S.dma(S.q_sync, w0a0[:], C.w0a0[l:l + 1, :], W=[w0a0])
    w2a2 = sc.sb("p2_w2a2", [128, 512])
    S.dma(S.q_sync, w2a2[:], C.w2a2[l], W=[w2a2])
    mu_b = rvt[:, RV_MU:RV_MU + 1664]
    kk_b = rvt[:, RV_KK:RV_KK + 512]; ka_b = rvt[:, RV_KA:RV_KA + 512]; rk_b = rvt[:, RV_RK:RV_RK + 512]
    lg_b = rvt[:, RV_LG:RV_LG + 512]; lb_b = rvt[:, RV_LB:RV_LB + 512]
    M_strict, M_incl, M_lower = G.masks[:, 0, :], G.masks[:, 1, :], G.masks[:, 2, :]
    uas = [sc.sb(f"p2_ua{i}", [128, 1664]) for i in range(2)]
    ups = [sc.sb(f"p2_up{i}", [128, 1664]) for i in range(2)]
    zas = [sc.sb(f"p2_za{i}", [128, 512]) for i in range(2)]
    f32t = lambda n: sc.sb("p2_" + n, [128, 512])
    lw = sc.sb("p2_lw", [128, 128])
    sgw, av, gam, ig, gae = f32t("sgw"), f32t("av"), f32t("gam"), f32t("ig"), f32t("gae")
    kk, sqt, kkn, e1, knew = f32t("kk"), f32t("sqt"), f32t("kkn"), f32t("e1"), f32t("knew")
    ssq = sc.sb("p2_ssq", [128, 32])
    gC = sc.sb("p2_gC", [64, 8])
    kap_b = sc.sb("p2_kapb", [128, 512], BF16); ktl_b = sc.sb("p2_ktlb", [128, 512], BF16)
    btl_b = sc.sb("p2_btlb", [128, 512], BF16); rtl_b = sc.sb("p2_rtlb", [128, 512], BF16)
    v_b = sc.sb("p2_vb", [128, 512], BF16)
    KR = sc.sb("p2_KR", [64, 8, 2, 128], BF16)
    KB = sc.sb("p2_KB", [64, 8, 2, 128], BF16)
    AK = sc.sb("p2_AK", [128, 8, 2, 128], BF16)
    AB2 = sc.sb("p2_AB2", [128, 8, 128], BF16)
    Qs = [sc.sb(f"p2_Q{i}", [128, 8, 128], INV_DT) for i in range(2)]
    QTs = [sc.sb(f"p2_QT{i}", [128, 8, 128], INV_DT) for i in range(2)]
    Ys = [sc.sb(f"p2_Y{i}", [128, 8, 128], INV_DT) for i in range(2)]
    R_sb = sc.sb("p2_R", [128, 512], INV_DT)
    negU = sc.sb("p2_negU", [128, 512], BF16)
    Hf = sc.sb("p2_Hf", [64, 512]); Hb = sc.sb("p2_Hb", [64, 512], BF16); Htmp = sc.sb("p2_Htmp", [64, 512])
    S.memset(S.dve, Hf[:], 0.0, W=[Hf]); S.memset(S.dve, Hb[:], 0.0, W=[Hb])
    yt, yc, sq2, rkt, sz, bon = f32t("yt"), f32t("yc"), f32t("sq2"), f32t("rkt"), f32t("sz"), f32t("bon")
    ygo = [sc.sb(f"p2_ygo{i}", [128, 512]) for i in range(2)]
    ident_i = G.ident_f if INV_DT == F32 else G.ident_bf
    evn = [0]
    def ev_eng():
        evn[0] += 1
        return S.act if evn[0] % 2 else S.dve
    for c in range(nch):
        ua = rr(uas, st, "ua"); up = rr(ups, st, "up"); za = rr(zas, st, "za")
        rows = slice(c * 128, (c + 1) * 128)
        S.dma(S.q_sync, ua[:], pa[rows, 0:1664], R=[pa], W=[ua])
        S.dma(S.q_pool, za[:], pa[rows, 1664:2176], R=[pa], W=[za])
        if c == 0:
            S.memset(S.pool, up[0:1, :], 0.0, W=[up])
            S.dma(S.q_sync, up[1:128, :], pa[0:127, 0:1664], R=[pa], W=[up])
        else:
            S.dma(S.q_sync, up[:], pa[c * 128 - 1:c * 128 + 127, 0:1664], R=[pa], W=[up])
        S.tt(S.pool, up[:], up[:], ua[:], ALU.subtract, R=[up, ua], W=[up])
        S.tt(S.dve, up[:], up[:], mu_b, ALU.mult, R=[up, rvt], W=[up])
        S.tt(S.pool, up[:], up[:], ua[:], ALU.add, R=[up, ua], W=[up])
        um = up
        r_, k_, v_ = um[:, 0:512], um[:, 512:1024], um[:, 1024:1536]
        pl_ = rr(G.ps, st, "ps")
        S.tr([(pl_[:, 0:128], um[:, 1536:1664])], G.ident_f[:], R=[um, G.ident_f], W=[pl_])
        S.actf(lw[0:64, :], pl_[0:64, 0:128], AF.Tanh, R=[pl_], W=[lw])
        S.cp(S.dve, lw[64:128, :], pl_[64:128, 0:128], R=[pl_], W=[lw])
        pzw = rr(G.ps, st, "ps"); pza = rr(G.ps, st, "ps")
        S.mm(pzw[:], [(lw[0:64, :], w2a2[0:64, :]), (G.ones_f[0:1, :], w0a0[0:1, 0:512])], R=[lw, w2a2, w0a0, G.ones_f], W=[pzw])
        S.mm(pza[:], [(lw[64:128, :], w2a2[64:128, :]), (G.ones_f[0:1, :], w0a0[0:1, 512:1024])], R=[lw, w2a2, w0a0, G.ones_f], W=[pza])
        S.actf(sgw[:], pzw[:], AF.Sigmoid, R=[pzw], W=[sgw])
        S.actf(av[:], pza[:], AF.Sigmoid, R=[pza], W=[av])
        pci = rr(G.ps, st, "ps"); pce = rr(G.ps, st, "ps"); pgc = rr(G.ps, st, "ps")
        S.mm(pci[:], [(M_incl, sgw[:])], R=[G.masks, sgw], W=[pci])
        S.mm(pce[:], [(M_strict, sgw[:])], R=[G.masks, sgw], W=[pce])
        S.group(S.pe, [(lambda h=h: nc.tensor.matmul(pgc[0:64, h:h + 1], lhsT=sgw[:, h * 64:(h + 1) * 64], rhs=G.ones_f[:, 0:1],
                                                      start=True, stop=True)) for h in range(8)], [sgw, G.ones_f], [pgc])
        S.actf(gam[:], pci[:], AF.Exp, R=[pci], W=[gam], scale=-CDEC)
        S.actf(ig[:], pci[:], AF.Exp, R=[pci], W=[ig], scale=CDEC)
        S.actf(gae[:], pce[:], AF.Exp, R=[pce], W=[gae], scale=-CDEC)
        S.actf(gC[:], pgc[0:64, 0:8], AF.Exp, R=[pgc], W=[gC], scale=-CDEC)
        S.tt(S.dve, kk[:], k_, kk_b, ALU.mult, R=[um, rvt], W=[kk])
        S.tt(S.pool, sqt[:], kk[:], kk[:], ALU.mult, R=[kk], W=[sqt])
        S.op(S.dve, lambda: nc.vector.tensor_reduce(out=ssq[:, 0:8], in_=h3(sqt[:]), axis=AX.X, op=ALU.add), [sqt], [ssq])
        S.actf(ssq[:, 8:16], ssq[:, 0:8], AF.Sqrt, R=[ssq], W=[ssq])
        S.ts(S.dve, ssq[:, 8:16], ssq[:, 8:16], 1e-12, op0=ALU.max, R=[ssq], W=[ssq])
        S.op(S.dve, lambda: nc.vector.reciprocal(out=ssq[:, 16:24], in_=ssq[:, 8:16]), [ssq], [ssq])
        S.tt(S.dve, h3(kkn[:]), h3(kk[:]), bc_last(ssq[:, 16:24], 64), ALU.mult, R=[kk, ssq], W=[kkn])
        S.stt(S.dve, e1[:], av[:], -1.0, ka_b, ALU.add, ALU.mult, R=[av, rvt], W=[e1])
        S.stt(S.dve, knew[:], e1[:], 1.0, k_, ALU.add, ALU.mult, R=[e1, um], W=[knew])
        S.tt(S.pool, kap_b[:], kkn[:], gae[:], ALU.mult, R=[kkn, gae], W=[kap_b])
        S.tt(S.dve, ktl_b[:], knew[:], ig[:], ALU.mult, R=[knew, ig], W=[ktl_b])
        S.tt(S.pool, e1[:], kkn[:], av[:], ALU.mult, R=[kkn, av], W=[e1])
        S.tt(S.pool, btl_b[:], e1[:], ig[:], ALU.mult, R=[e1, ig], W=[btl_b])
        S.tt(S.dve, rtl_b[:], r_, gam[:], ALU.mult, R=[um, gam], W=[rtl_b])
        S.cp(S.pool, v_b[:], v_, R=[um], W=[v_b])
        for (src, dst, wi) in ((kap_b, KR, 0), (rtl_b, KR, 1), (ktl_b, KB, 0), (btl_b, KB, 1)):
            pb = rr(G.psb, st, "psb")
            S.tr([(pb[0:64, h * 128:(h + 1) * 128], src[:, h * 64:(h + 1) * 64]) for h in range(8)], G.ident_bf[:],
                 R=[src, G.ident_bf], W=[pb])
            S.cp(ev_eng(), dst[:, :, wi, :], pb[0:64, :].rearrange("p (h t) -> p h t", h=8), R=[pb], W=[dst])
        Q, QT, Y = rr(Qs, st, "Q"), rr(QTs, st, "QT"), rr(Ys, st, "Y")
        for hp in range(4):
            p1 = rr(G.ps, st, "ps"); p2 = rr(G.ps, st, "ps")
            for hh in range(2):
                h = hp * 2 + hh
                S.mm(p1[:, hh * 256:(hh + 1) * 256], [(KB[:, h, 0, :], KR[:, h, :, :])], R=[KB, KR], W=[p1])
                S.mm(p2[:, hh * 256:(hh + 1) * 256], [(KB[:, h, 1, :], KR[:, h, :, :])], R=[KB, KR], W=[p2])
            hs = slice(hp * 2, hp * 2 + 2)
            p1v = p1[:].rearrange("p (h w t) -> p h w t", h=2, w=2)
            p2v = p2[:].rearrange("p (h w t) -> p h w t", h=2, w=2)
            for hh in range(2):
                h = hp * 2 + hh
                S.tt(S.dve, AK[:, h, :, :], p1v[:, hh, :, :], G.masks[:, 0:2, :], ALU.mult, R=[p1, G.masks], W=[AK])
            S.tt(S.dve, AB2[:, hs, :], p2v[:, :, 1, :], bc_mid(M_incl, 2), ALU.mult, R=[p2, G.masks], W=[AB2])
            S.stt(S.dve, QT[:, hs, :], p2v[:, :, 0, :], -1.0, bc_mid(M_strict, 2), ALU.mult, ALU.mult, R=[p2, G.masks], W=[QT])
            S.tt(S.pool, Y[:, hs, :], QT[:, hs, :], bc_mid(ident_i[:], 2), ALU.add, R=[QT, ident_i], W=[Y])
        for hq in range(2):
            p3 = rr(G.ps, st, "ps")
            for hh in range(4):
                h = hq * 4 + hh
                S.mm(p3[:, hh * 128:(hh + 1) * 128], [(KR[:, h, 0, :], KB[:, h, 1, :])], R=[KR, KB], W=[p3])
            hs = slice(hq * 4, hq * 4 + 4)
            S.stt(S.dve, Q[:, hs, :], p3[:].rearrange("p (h t) -> p h t", h=4), -1.0, bc_mid(M_lower, 4), ALU.mult, ALU.mult,
                  R=[p3, G.masks], W=[Q])
        for lev in range(1, 7):
            Qn, QTn, Yn = rr(Qs, st, "Q"), rr(QTs, st, "QT"), rr(Ys, st, "Y")
            last = lev == 6
            for hq in range(2):
                hs = slice(hq * 4, hq * 4 + 4)
                pq = rr(G.ps, st, "ps")
                for hh in range(4):
                    h = hq * 4 + hh
                    S.mm(pq[:, hh * 128:(hh + 1) * 128], [(QT[:, h, :], Q[:, h, :])], R=[QT, Q], W=[pq])
                S.cp(ev_eng(), Qn[:, hs, :], pq[:].rearrange("p (h t) -> p h t", h=4), R=[pq], W=[Qn])
                if not last:
                    pqt = rr(G.ps, st, "ps")
                    for hh in range(4):
                        h = hq * 4 + hh
                        S.mm(pqt[:, hh * 128:(hh + 1) * 128], [(Q[:, h, :], QT[:, h, :])], R=[QT, Q], W=[pqt])
                    S.cp(ev_eng(), QTn[:, hs, :], pqt[:].rearrange("p (h t) -> p h t", h=4), R=[pqt], W=[QTn])
            for hq in range(2):
                hs = slice(hq * 4, hq * 4 + 4)
                py = rr(G.ps, st, "ps")
                for hh in range(4):
                    h = hq * 4 + hh
                    S.mm(py[:, hh * 128:(hh + 1) * 128], [(Qn[:, h, :], Y[:, h, :])], R=[Qn, Y], W=[py])
                S.tt(S.dve, Yn[:, hs, :], py[:].rearrange("p (h t) -> p h t", h=4), Y[:, hs, :], ALU.add, R=[py, Y], W=[Yn])
            Q, QT, Y = Qn, QTn, Yn
        pR = rr(G.ps, st, "ps")
        for h in range(8):
            cs_ = slice(h * 64, (h + 1) * 64)
            S.mm(pR[:, cs_], [(KR[:, h, 0, :], Hb[:, cs_]), (AK[:, h, 0, :], v_b[:, cs_])], R=[KR, Hb, AK, v_b], W=[pR])
        S.cp(S.act, R_sb[:], pR[:], R=[pR], W=[R_sb])
        pU = rr(G.ps, st, "ps")
        for h in range(8):
            cs_ = slice(h * 64, (h + 1) * 64)
            S.mm(pU[:, cs_], [(Y[:, h, :], R_sb[:, cs_])], R=[Y, R_sb], W=[pU])
        S.ts(S.dve, negU[:], pU[:], -1.0, R=[pU], W=[negU])
        pY = rr(G.ps, st, "ps")
        for h in range(8):
            cs_ = slice(h * 64, (h + 1) * 64)
            S.mm(pY[:, cs_], [(KR[:, h, 1, :], Hb[:, cs_]), (AK[:, h, 1, :], v_b[:, cs_]), (AB2[:, h, :], negU[:, cs_])],
                 R=[KR, Hb, AK, v_b, AB2, negU], W=[pY])
        S.cp(S.act, yt[:], pY[:], R=[pY], W=[yt])
        pH = rr(G.ps, st, "ps")
        for h in range(8):
            cs_ = slice(h * 64, (h + 1) * 64)
            S.mm(pH[0:64, cs_], [(ktl_b[:, cs_], v_b[:, cs_]), (btl_b[:, cs_], negU[:, cs_])], R=[ktl_b, v_b, btl_b, negU], W=[pH])
        S.tt(S.dve, Htmp[:], pH[0:64, :], Hf[:], ALU.add, R=[pH, Hf], W=[Htmp])
        S.tt(S.dve, h3(Hf[:]), h3(Htmp[:]), bc_last(gC[:], 64), ALU.mult, R=[Htmp, gC], W=[Hf])
        S.cp(S.pool, Hb[:], Hf[:], R=[Hf], W=[Hb])
        S.op(S.dve, lambda: nc.vector.tensor_reduce(out=ssq[:, 24:32], in_=h3(yt[:]), axis=AX.X, op=ALU.add), [yt], [ssq])
        S.ts(S.dve, ssq[:, 24:32], ssq[:, 24:32], -1.0 / 64, R=[ssq], W=[ssq])
        S.tt(S.pool, h3(yc[:]), h3(yt[:]), bc_last(ssq[:, 24:32], 64), ALU.add, R=[yt, ssq], W=[yc])
        S.tt(S.pool, sq2[:], yc[:], yc[:], ALU.mult, R=[yc], W=[sq2])
        S.op(S.dve, lambda: nc.vector.tensor_reduce(out=ssq[:, 0:8], in_=h3(sq2[:]), axis=AX.X, op=ALU.add), [sq2], [ssq])
        S.actf(ssq[:, 8:16], ssq[:, 0:8], AF.Sqrt, R=[ssq], W=[ssq], scale=1.0 / 64, bias=G.eps_col[:, 1:2])
        S.op(S.dve, lambda: nc.vector.reciprocal(out=ssq[:, 16:24], in_=ssq[:, 8:16]), [ssq], [ssq])
        S.tt(S.dve, h3(yc[:]), h3(yc[:]), bc_last(ssq[:, 16:24], 64), ALU.mult, R=[yc, ssq], W=[yc])
        S.tt(S.pool, yc[:], yc[:], lg_b, ALU.mult, R=[yc, rvt], W=[yc])
        S.tt(S.pool, yc[:], yc[:], lb_b, ALU.add, R=[yc, rvt], W=[yc])
        S.tt(S.pool, rkt[:], r_, knew[:], ALU.mult, R=[um, knew], W=[rkt])
        S.tt(S.pool, rkt[:], rkt[:], rk_b, ALU.mult, R=[rkt, rvt], W=[rkt])
        S.op(S.dve, lambda: nc.vector.tensor_reduce(out=ssq[:, 24:32], in_=h3(rkt[:]), axis=AX.X, op=ALU.add), [rkt], [ssq])
        S.tt(S.dve, h3(bon[:]), h3(v_), bc_last(ssq[:, 24:32], 64), ALU.mult, R=[um, ssq], W=[bon])
        S.tt(S.pool, yc[:], yc[:], bon[:], ALU.add, R=[yc, bon], W=[yc])
        S.actf(sz[:], za[:], AF.Silu, R=[za], W=[sz])
        yo = rr(ygo, st, "ygo")
        S.tt(S.dve, yo[:], yc[:], sz[:], ALU.mult, R=[yc, sz], W=[yo])
        S.dma(S.q_pool, yga[rows, :], yo[:], R=[yo], W=[yga])
    sc.close()


def phase_out(S, C, G, l, hsrc, hdst, pbT, yga, ybT, ycT, T, final):
    nc = S.nc
    sc = Scope(S)
    st = {}
    nblk = T // 512
    cvt = sc.sb("p5_cv", [128, NCV])
    S.dma(S.q_sync, cvt[:], C.cv[l], W=[cvt])
    wo = sc.sb("p5_wo", [128, 12, 1024], BF16)
    sc2 = Scope(S)
    stg = [sc2.sb(f"p5_stg{i}", [128, 1024]) for i in range(2)]
    for k in range(12):
        sg = stg[k % 2]
        S.dma(S.q_sync if k % 2 == 0 else S.q_pool, sg[:], C.w_out[l][k * 128:(k + 1) * 128, :], W=[sg])
        S.cp(S.dve if k % 2 == 0 else S.pool, wo[:, k, :], sg[:], R=[sg], W=[wo])
    sc2.close()
    if final:
        fg = sc.sb("p5_fg", [128, D])
        S.dma(S.q_sync, fg[:], C.final_g[0:1, :].partition_broadcast(128), W=[fg])
        junk = sc.sb("p5_junk", [128, D], BF16)
        ssf = [sc.sb(f"p5_ssf{i}", [128, 4]) for i in range(2)]
    ygT = [sc.sb(f"p5_ygT{i}", [128, 12, 512], BF16) for i in range(2)]
    ybs = [sc.sb(f"p5_yb{i}", [128, 512]) for i in range(5)]
    zbs = [sc.sb(f"p5_zb{i}", [128, 512]) for i in range(3)]
    sqs = [sc.sb(f"p5_sq{i}", [128, 512]) for i in range(2)]
    rstd = [sc.sb(f"p5_rstd{i}", [128, 512]) for i in range(2)]
    t1s = [sc.sb(f"p5_t1{i}", [128, 512]) for i in range(2)]
    t2s = [sc.sb(f"p5_t2{i}", [128, 512]) for i in range(2)]
    yas = [sc.sb(f"p5_ya{i}", [128, 512]) for i in range(2)]
    yabs = [sc.sb(f"p5_yab{i}", [128, 512], BF16) for i in range(2)]
    hts = [sc.sb(f"p5_h{i}", [128, D]) for i in range(2)]
    hos = [sc.sb(f"p5_ho{i}", [128, D]) for i in range(2)]
    for j in range(nblk):
        blk = slice(j * 512, (j + 1) * 512)
        yg = ygT[j % 2]
        for (src, zoff, goff, kbase) in ((ybT, OB_ZB, CV_LOG, 4), (ycT, OB_ZC, CV_MOG, 8)):
            ys = []
            pS = rr(G.ps, st, "ps")
            for ct in range(4):
                yb = rr(ybs, st, "yb"); ys.append(yb)
                S.dma(S.q_sync, yb[:], src[ct * 128:(ct + 1) * 128, blk], R=[src], W=[yb])
                sq = rr(sqs, st, "sq")
                S.actf(sq[:], yb[:], AF.Square, R=[yb], W=[sq])
                S.mm(pS[:], [(G.ones_f[:], sq[:])], R=[G.ones_f, sq], W=[pS], start=(ct == 0), stop=(ct == 3))
            rs = rr(rstd, st, "rstd")
            S.actf(rs[:], pS[:], AF.Sqrt, R=[pS], W=[rs], scale=1.0 / 512, bias=G.eps_col[:, 0:1])
            S.op(S.dve, lambda rs=rs: nc.vector.reciprocal(out=rs[:], in_=rs[:]), [rs], [rs])
            for ct in range(4):
                zb = rr(zbs, st, "zb")
                S.dma(S.q_pool, zb[:], pbT[zoff + ct * 128:zoff + (ct + 1) * 128, blk], R=[pbT], W=[zb])
                t1 = rr(t1s, st, "t1"); t2 = rr(t2s, st, "t2")
                S.actf(t1[:], zb[:], AF.Silu, R=[zb], W=[t1])
                S.stt(S.dve, t2[:], ys[ct][:], cvt[:, goff + ct:goff + ct + 1], rs[:], ALU.mult, ALU.mult, R=[ys[ct], cvt, rs], W=[t2])
                S.tt(S.pool, yg[:, kbase + ct, :], t1[:], t2[:], ALU.mult, R=[t1, t2], W=[yg])
        for tt in range(4):
            t = 4 * j + tt
            ya = rr(yas, st, "ya"); yab = rr(yabs, st, "yab")
            S.dma(S.q_sync, ya[:], yga[t * 128:(t + 1) * 128, :], R=[yga], W=[ya])
            S.cp(S.pool, yab[:], ya[:], R=[ya], W=[yab])
            pb = rr(G.psb, st, "psb")
            S.tr([(pb[:, ct * 128:(ct + 1) * 128], yab[:, ct * 128:(ct + 1) * 128]) for ct in range(4)], G.ident_bf[:],
                 R=[yab, G.ident_bf], W=[pb])
            S.cp(S.act, yg[:, 0:4, tt * 128:(tt + 1) * 128], pb[:, 0:512].rearrange("p (c t) -> p c t", c=4), R=[pb], W=[yg])
        for tt in range(4):
            t = 4 * j + tt
            ht = rr(hts, st, "h"); ho = rr(hos, st, "ho")
            S.dma(S.q_sync, ht[:], hsrc[t * 128:(t + 1) * 128, :], R=[hsrc], W=[ht])
            for half in range(2):
                cs_ = slice(half * 512, (half + 1) * 512)
                pO = rr(G.ps, st, "ps")
                S.mm(pO[:], [(yg[:, k, tt * 128:(tt + 1) * 128], wo[:, k, cs_]) for k in range(12)], R=[yg, wo], W=[pO])
                S.tt(S.dve, ho[:, cs_], pO[:], ht[:, cs_], ALU.add, R=[pO, ht], W=[ho])
            if final:
                s_ = rr(ssf, st, "ssf")
                S.actf(junk[:], ho[:], AF.Square, R=[ho], W=[junk, s_], accum=s_[:, 0:1])
                S.actf(s_[:, 1:2], s_[:, 0:1], AF.Sqrt, R=[s_], W=[s_], scale=1.0 / D, bias=G.eps_col[:, 0:1])
                S.op(S.dve, lambda a=s_: nc.vector.reciprocal(out=a[:, 2:3], in_=a[:, 1:2]), [s_], [s_])
                S.stt(S.dve, ht[:], ho[:], s_[:, 2:3], fg[:], ALU.mult, ALU.mult, R=[ho, s_, fg], W=[ht])
                S.dma(S.q_pool, hdst[t * 128:(t + 1) * 128, :], ht[:], R=[ht], W=[hdst])
            else:
                S.dma(S.q_pool, hdst[t * 128:(t + 1) * 128, :], ho[:], R=[ho], W=[hdst])
    sc.close()


def build(T, L, phases=None, debug=False):
    nc = bass.Bass("TRN2", target_bir_lowering=False)
    S = Sched(nc)
    C = declare_inputs(S, T, L)
    G = Ctx()
    load_consts(S, C, G)
    G.eps_col = S.sb("eps_col", [128, 2])
    S.memset(S.dve, G.eps_col[:, 0:1], EPS, W=[G.eps_col])
    S.memset(S.dve, G.eps_col[:, 1:2], GN_EPS, W=[G.eps_col])
    full = phases is None
    if full:
        phases = ("p1", "p2", "p3", "p4", "p5")
    dbg = lambda name: "ExternalOutput" if (debug and name in phases) else "Internal"
    pa = S.dram("pa", [T, NA], F32, kind=dbg("p1"))
    pbT = S.dram("pbT", [NB, T], F32, kind=dbg("p1"))
    ybT = S.dram("ybT", [512, T], F32, kind=dbg("p3"))
    ycT = S.dram("ycT", [512, T], F32, kind=dbg("p4"))
    yga = S.dram("yga", [T, 512], F32, kind=dbg("p2"))
    hb = [S.dram(f"hbuf{i}", [T, D], F32) for i in range(2)]
    hout = S.dram("hout", [T, D], F32, kind="ExternalOutput" if (full or "p5" in phases) else "Internal")
    outs = []
    nl = L if full else 1
    for l in range(nl):
        hsrc = C.x if l == 0 else hb[(l - 1) % 2]
        last = l == nl - 1
        hdst = hout if last else hb[l % 2]
        phase_in_proj(S, C, G, l, hsrc, pa, pbT, T)
        if "p2" in phases: phase_rwkv(S, C, G, l, pa, yga, T)
        if "p3" in phases: phase_lru(S, C, G, l, pbT, ybT, T)
        if "p4" in phases: phase_mla(S, C, G, l, pbT, ycT, T)
        if "p5" in phases: phase_out(S, C, G, l, hsrc, hdst, pbT, yga, ybT, ycT, T, final=(full and last))
    if debug:
        for nm, b in (("p1", pa), ("p1", pbT), ("p3", ybT), ("p4", ycT), ("p2", yga)):
            if nm in phases: outs.append(b)
    if full or "p5" in phases: outs.append(hout)
    S.finish(outs)
    emit_all(S)
    return nc, S

from concourse.bass_utils import run_bass_kernel_spmd

T_FULL = 4096
L_FULL = 4
_IN_NAMES = ["x", "wA", "wB", "rv", "w0a0", "w2a2", "cv", "gw", "wq", "wqs", "wk", "wv", "w_out", "final_g",
             "ident_bf", "ident_f", "cs", "masks", "mask_bf"]
_NC_CACHE = {}


def kernel(**inputs):
    x = np.asarray(inputs["x"], np.float32)
    B, T, _ = x.shape
    L = np.asarray(inputs["w_in"]).shape[0]
    hp = host_prep(inputs, T)
    key = (T, L)
    if key not in _NC_CACHE:
        _NC_CACHE[key] = build(T, L)[0]
    nc = _NC_CACHE[key]
    in_maps = []
    for b in range(B):
        m = {k: hp[k] for k in _IN_NAMES if k != "x"}
        m["x"] = np.ascontiguousarray(x[b])
        in_maps.append(m)
    res = run_bass_kernel_spmd(nc, in_maps, core_ids=list(range(B)))
    return np.stack([np.asarray(r["hout"], np.float32) for r in res.results], axis=0)
```

```python
import numpy as np
import concourse.bass as bass
import concourse.mybir as mybir

F32 = mybir.dt.float32
BF16 = mybir.dt.bfloat16
ALU = mybir.AluOpType
AF = mybir.ActivationFunctionType
AX = mybir.AxisListType


class Sem:
    def __init__(self, nc, name):
        self.h = nc.alloc_semaphore(name) if hasattr(nc, "alloc_semaphore") else None
        self.name = name
        self.count = 0


class Buf:
    __slots__ = ("t", "name", "last_write", "reads")

    def __init__(self, t, name):
        self.t = t
        self.name = name
        self.last_write = None
        self.reads = {}

    def __getitem__(self, idx):
        return self.t[idx]


SEM_LIMIT = 8000


class Eng:
    def cur_sem(self, i=0):
        sem = self.sems[i]
        if sem.count >= SEM_LIMIT:
            self.nrot = getattr(self, "nrot", 0) + 1
            sem = self.S.new_sem(f"{self.name}_r{self.nrot}")
            self.sems[i] = sem
        return sem

    def __init__(self, S, name, eng, is_dma=False, nsem=1):
        self.S = S
        self.name = name
        self.e = eng
        self.is_dma = is_dma
        self.sems = [S.new_sem(f"{name}_s{i}") for i in range(nsem)]
        self.rr = 0
        self.known = {}
        self.prog = []

    def wait(self, tok):
        sem, val = tok
        if self.known.get(id(sem), 0) >= val:
            return
        e = self.e; h = sem.h
        self.prog.append(lambda: e.wait_ge(h, val))
        self.known[id(sem)] = val
        self.S.nwaits += 1


class Sched:
    def __init__(self, nc):
        self.nc = nc
        import contextlib
        self.es = contextlib.ExitStack()
        self.nwaits = 0
        self.ninst = 0
        self.sem_list = []
        self.pe = Eng(self, "pe", nc.tensor)
        self.dve = Eng(self, "dve", nc.vector)
        self.act = Eng(self, "act", nc.scalar)
        self.pool = Eng(self, "pool", nc.gpsimd)
        self.q_sync = Eng(self, "qsync", nc.sync, is_dma=True, nsem=8)
        self.q_pool = Eng(self, "qpool", nc.gpsimd, is_dma=True, nsem=4)
        self.q_pool.prog = self.pool.prog
        self.engines = [self.pe, self.dve, self.act, self.pool, self.q_sync]

    def new_sem(self, name):
        s = Sem.__new__(Sem)
        s.is_pe = name.startswith("pe_")
        s.name = name
        s.count = 0
        s.h = self.es.enter_context(self.nc.semaphore(name))
        self.sem_list.append(s)
        return s

    def sb(self, name, shape, dtype=F32):
        t = self.nc.alloc_sbuf_tensor(name, list(shape), dtype)
        return Buf(t, name)

    def ps(self, name, shape, dtype=F32):
        t = self.nc.alloc_psum_tensor(name, list(shape), dtype)
        return Buf(t, name)

    def dram(self, name, shape, dtype=F32, kind="Internal"):
        t = self.nc.dram_tensor(name, list(shape), dtype, kind=kind)
        return Buf(t, name)

    def _deps(self, reads, writes):
        deps = []
        for b in reads:
            if b.last_write is not None:
                deps.append(b.last_write)
        for b in writes:
            if b.last_write is not None:
                deps.append(b.last_write)
            deps.extend(b.reads.values())
        return deps

    def _commit(self, tok, reads, writes):
        for b in reads:
            if b not in writes:
                b.reads[id(tok[0])] = tok
        for b in writes:
            b.last_write = tok
            b.reads = {}

    def op(self, eng, fn, reads=(), writes=()):
        for tok in self._deps(reads, writes):
            if eng is self.pe and tok[0].is_pe:
                continue
            eng.wait(tok)
        sem = eng.cur_sem()
        sem.count += 1
        h = sem.h
        eng.prog.append(lambda: fn().then_inc(h, 1))
        tok = (sem, sem.count)
        self._commit(tok, reads, writes)
        self.ninst += 1
        return tok

    def group(self, eng, fns, reads=(), writes=()):
        for tok in self._deps(reads, writes):
            if eng is self.pe and tok[0].is_pe:
                continue
            eng.wait(tok)
        fns = list(fns)
        for fn in fns[:-1]:
            eng.prog.append(fn)
            self.ninst += 1
        self.ninst += 1
        sem = eng.cur_sem()
        sem.count += 1
        h = sem.h
        last = fns[-1]
        eng.prog.append(lambda: last().then_inc(h, 1))
        tok = (sem, sem.count)
        self._commit(tok, reads, writes)
        return tok

    def dma(self, q, out_ap, in_ap, R=(), W=(), **kw):
        i = q.rr % len(q.sems)
        sem = q.sems[i]
        q.rr += 1
        if sem.count > 0:
            q.wait((sem, sem.count))
        sem = q.cur_sem(i)
        for tok in self._deps(R, W):
            q.wait(tok)
        sem.count += 16
        h = sem.h; e = q.e
        q.prog.append(lambda: e.dma_start(out=out_ap, in_=in_ap, **kw).then_inc(h, 16))
        tok = (sem, sem.count)
        self._commit(tok, R, W)
        self.ninst += 1
        return tok

    def ts(self, eng, out, in0, s1, s2=None, op0=ALU.mult, op1=None, R=(), W=(), accum=None):
        e = eng.e
        kw = {}
        if op1 is not None: kw["op1"] = op1
        if accum is not None: kw["accum_out"] = accum
        return self.op(eng, lambda: e.tensor_scalar(out=out, in0=in0, scalar1=s1, scalar2=s2, op0=op0, **kw), R, W)

    def tt(self, eng, out, in0, in1, op, R=(), W=()):
        e = eng.e
        return self.op(eng, lambda: e.tensor_tensor(out=out, in0=in0, in1=in1, op=op), R, W)

    def stt(self, eng, out, in0, scalar, in1, op0, op1, R=(), W=()):
        e = eng.e
        return self.op(eng, lambda: e.scalar_tensor_tensor(out=out, in0=in0, scalar=scalar, in1=in1, op0=op0, op1=op1), R, W)

    def cp(self, eng, out, in_, R=(), W=()):
        e = eng.e
        if eng is self.act:
            return self.op(eng, lambda: e.copy(out=out, in_=in_), R, W)
        return self.op(eng, lambda: e.tensor_copy(out=out, in_=in_), R, W)

    def actf(self, out, in_, func, R=(), W=(), scale=1.0, bias=None, accum=None):
        e = self.act.e
        kw = {}
        if bias is not None: kw["bias"] = bias
        if accum is not None: kw["accum_out"] = accum
        return self.op(self.act, lambda: e.activation(out=out, in_=in_, func=func, scale=scale, **kw), R, W)

    def mm(self, out, pairs, R=(), W=(), start=True, stop=True, **kw):
        e = self.pe.e
        n = len(pairs)
        fns = []
        for i, (l, r) in enumerate(pairs):
            st = start and i == 0
            sp = stop and i == n - 1
            fns.append((lambda l=l, r=r, st=st, sp=sp: e.matmul(out, lhsT=l, rhs=r, start=st, stop=sp, **kw)))
        return self.group(self.pe, fns, R, W)

    def tr(self, outs_ins, ident, R=(), W=()):
        e = self.pe.e
        fns = [(lambda o=o, i=i: e.transpose(out=o, in_=i, identity=ident)) for (o, i) in outs_ins]
        return self.group(self.pe, fns, R, W)

    def memset(self, eng, ap, val, W=()):
        e = eng.e
        return self.op(eng, lambda: e.memset(ap, val), (), W)

    def barrier(self):
        toks = [(s, s.count) for s in self.sem_list if s.count > 0]
        for eng in [self.pe, self.dve, self.act, self.pool, self.q_sync]:
            for tok in toks:
                eng.wait(tok)

    def finish(self, bufs):
        for b in bufs:
            if b.last_write is not None:
                self.q_sync.wait(b.last_write)


def emit_all(S):
    nc = S.nc
    def run(prog):
        def f(e):
            for g in prog: g()
        return f
    with nc.Block() as block:
        if S.q_sync.prog: block.sync(run(S.q_sync.prog))
        if S.pe.prog: block.tensor(run(S.pe.prog))
        if S.dve.prog: block.vector(run(S.dve.prog))
        if S.act.prog: block.scalar(run(S.act.prog))
        if S.pool.prog: block.gpsimd(run(S.pool.prog))
    S.es.close()


def _prune(reads):
    best = {}
    for sem, val in reads:
        k = id(sem)
        if k not in best or best[k][1] < val:
            best[k] = (sem, val)
    return list(best.values())

D = 1024
GW = 512
NH = 8
HD = 64
RW_IN = 1664
NA = 2176
NB = 1984
QL = 256
KVL = 128
DMIX = 1536

OB_UB, OB_ZB, OB_QL, OB_KV, OB_ZC, OB_KR, OB_KRS = 0, 512, 1024, 1280, 1408, 1920, 1952

RV_MU, RV_KK, RV_KA, RV_RK, RV_LG, RV_LB = 0, 1664, 2176, 2688, 3200, 3712
NRV = 4224
CV_LNG = 0
CV_CW = 8
CV_CB = 24
CV_GAB = 28
CV_GXB = 32
CV_LAM = 36
CV_LOG = 40
CV_QNG = 44
CV_KVNG = 46
CV_MOG = 47
NCV = 52


def host_prep(inp, T):
    f = np.float32
    L = inp["w_in"].shape[0]
    out = {}
    w_in = np.asarray(inp["w_in"], f)
    out["wA"] = np.ascontiguousarray(w_in[:, :, 0:NA])
    c = 2176
    colsB = np.concatenate([
        np.arange(c, c + 512), np.arange(c + 512, c + 1024), np.arange(3200, 3456), np.arange(3456, 3584),
        np.arange(3616, 4128), np.arange(3584, 3616), np.arange(3600, 3616), np.arange(3584, 3600)])
    assert colsB.size == NB
    out["wB"] = np.ascontiguousarray(w_in[:, :, colsB])
    rv = np.zeros((L, NRV), f)
    rv[:, RV_MU:RV_MU + 1664] = inp["rwkv_mu"]
    rv[:, RV_KK:RV_KK + 512] = inp["rwkv_k_k"]
    rv[:, RV_KA:RV_KA + 512] = inp["rwkv_k_a"]
    rv[:, RV_RK:RV_RK + 512] = np.asarray(inp["rwkv_r_k"], f).reshape(L, 512)
    rv[:, RV_LG:RV_LG + 512] = inp["rwkv_lnx_g"]
    rv[:, RV_LB:RV_LB + 512] = inp["rwkv_lnx_b"]
    out["rv"] = rv
    out["w0a0"] = np.ascontiguousarray(np.concatenate([np.asarray(inp["rwkv_w0"], f), np.asarray(inp["rwkv_a0"], f)], axis=1))
    out["w2a2"] = np.ascontiguousarray(np.concatenate([np.asarray(inp["rwkv_w2"], f), np.asarray(inp["rwkv_a2"], f)], axis=1))
    cv = np.zeros((L, 128, NCV), f)
    def colfill(off, vec, n):
        cv[:, :, off:off + n] = np.asarray(vec, f).reshape(L, n, 128).transpose(0, 2, 1)
    colfill(CV_LNG, inp["ln_g"], 8)
    cw = np.asarray(inp["lru_conv_w"], f)
    for j in range(4):
        colfill(CV_CW + j * 4, cw[:, j], 4)
    colfill(CV_CB, inp["lru_conv_b"], 4)
    colfill(CV_GAB, inp["lru_ga_b"], 4)
    colfill(CV_GXB, inp["lru_gx_b"], 4)
    colfill(CV_LAM, inp["lru_lam"], 4)
    colfill(CV_LOG, inp["lru_out_g"], 4)
    colfill(CV_QNG, inp["mla_q_norm_g"], 2)
    colfill(CV_KVNG, inp["mla_kv_norm_g"], 1)
    colfill(CV_MOG, inp["mla_out_g"], 4)
    out["cv"] = cv
    gw = np.zeros((L, 4, 128, 2, 128), f)
    for gi, nm in enumerate(["lru_ga_w", "lru_gx_w"]):
        w = np.asarray(inp[nm], f)
        for h in range(8):
            ct, o = h // 2, (h % 2) * 64
            gw[:, ct, o:o + 64, gi, o:o + 64] = w[:, h]
    out["gw"] = gw
    wuq = np.asarray(inp["mla_w_uq"], f).reshape(L, 256, 8, 96)
    wq = np.zeros((L, 256, 8, 128), f)
    wq[..., 0:32] = wuq[..., 64:96]
    wq[..., 64:128] = wuq[..., 0:64]
    out["wq"] = np.ascontiguousarray(wq.reshape(L, 256, 1024))
    wqs = np.zeros((L, 256, 8, 128), f)
    wqs[..., 0:16] = wuq[..., 80:96]
    wqs[..., 16:32] = wuq[..., 64:80]
    out["wqs"] = np.ascontiguousarray(wqs.reshape(L, 256, 1024))
    wukv = np.asarray(inp["mla_w_ukv"], f).reshape(L, 128, 8, 128)
    wk = np.zeros((L, 128, 8, 128), f)
    wk[..., 64:128] = wukv[..., 0:64]
    out["wk"] = np.ascontiguousarray(wk.reshape(L, 128, 1024))
    out["wv"] = np.ascontiguousarray(wukv[..., 64:128].reshape(L, 128, 512))
    out["w_out"] = np.asarray(inp["w_out"], f)
    out["final_g"] = np.asarray(inp["final_g"], f).reshape(1, 1024)
    import ml_dtypes
    bf = ml_dtypes.bfloat16
    out["ident_bf"] = np.eye(128, dtype=f).astype(bf)
    out["ident_f"] = np.eye(128, dtype=f)
    half = 16
    inv_freq = (10000.0 ** (-np.arange(half, dtype=f) * 2.0 / 32)).astype(f)
    ang = np.arange(T, dtype=f)[:, None] * inv_freq[None, :]
    cos, sin = np.cos(ang).astype(f).T, np.sin(ang).astype(f).T
    cs = np.zeros((128, 2, T), f)
    cs[32:128, 0] = 1.0
    cs[0:16, 0], cs[16:32, 0] = cos, cos
    cs[0:16, 1], cs[16:32, 1] = -sin, sin
    out["cs"] = cs
    ii = np.arange(128)
    m = np.zeros((128, 4, 128), f)
    m[:, 0] = (ii[:, None] < ii[None, :])
    m[:, 1] = (ii[:, None] <= ii[None, :])
    m[:, 2] = (ii[:, None] > ii[None, :])
    m[:, 3] = 1.0
    out["masks"] = m
    out["mask_bf"] = (ii[:, None] <= ii[None, :]).astype(f).astype(bf)
    return out

import contextlib

EPS = 1e-6


class Ctx:
    pass


def declare_inputs(S, T, L):
    C = Ctx()
    di = lambda n, shp, dt=F32: S.dram(n, shp, dt, kind="ExternalInput")
    C.x = di("x", [T, D])
    C.wA = di("wA", [L, D, NA]); C.wB = di("wB", [L, D, NB])
    C.rv = di("rv", [L, NRV]); C.w0a0 = di("w0a0", [L, 1024]); C.w2a2 = di("w2a2", [L, 128, 512])
    C.cv = di("cv", [L, 128, NCV]); C.gw = di("gw", [L, 4, 128, 2, 128])
    C.wq = di("wq", [L, 256, 1024]); C.wqs = di("wqs", [L, 256, 1024])
    C.wk = di("wk", [L, 128, 1024]); C.wv = di("wv", [L, 128, 512])
    C.w_out = di("w_out", [L, DMIX, D]); C.final_g = di("final_g", [1, D])
    C.ident_bf = di("ident_bf", [128, 128], BF16); C.ident_f = di("ident_f", [128, 128])
    C.cs = di("cs", [128, 2, T]); C.masks = di("masks", [128, 4, 128]); C.mask_bf = di("mask_bf", [128, 128], BF16)
    return C


class Scope:
    def __init__(self, S):
        self.S = S
        self.es = contextlib.ExitStack()

    _n = [0]

    def sb(self, name, shape, dtype=F32):
        Scope._n[0] += 1
        name = f"{name}_u{Scope._n[0]}"
        t = self.es.enter_context(self.S.nc.sbuf_tensor(name, list(shape), dtype))
        return Buf(t, name)

    def close(self):
        self.S.barrier()
        self.es.close()


def load_consts(S, C, G):
    nc = S.nc
    G.ident_bf = S.sb("ident_bf_sb", [128, 128], BF16)
    G.ident_f = S.sb("ident_f_sb", [128, 128])
    G.masks = S.sb("masks_sb", [128, 4, 128])
    G.mask_bf = S.sb("mask_bf_sb", [128, 128], BF16)
    G.ones_f = S.sb("ones_f", [128, 128])
    S.dma(S.q_sync, G.ident_bf[:], C.ident_bf[:], W=[G.ident_bf])
    S.dma(S.q_sync, G.ident_f[:], C.ident_f[:], W=[G.ident_f])
    S.dma(S.q_sync, G.masks[:], C.masks[:], W=[G.masks])
    S.dma(S.q_sync, G.mask_bf[:], C.mask_bf[:], W=[G.mask_bf])
    S.memset(S.dve, G.ones_f[:], 1.0, W=[G.ones_f])
    G.ps = [S.ps(f"ps{i}", [128, 512], F32) for i in range(6)]
    G.psb = [S.ps(f"psb{i}", [128, 1024], BF16) for i in range(2)]


def rr(lst, state, key):
    i = state.get(key, 0)
    state[key] = i + 1
    return lst[i % len(lst)]


def phase_in_proj(S, C, G, l, hsrc, pa, pbT, T):
    nc = S.nc
    sc = Scope(S)
    st = {}
    wA = sc.sb("wA_sb", [128, 8, NA], BF16)
    wB = sc.sb("wB_sb", [128, 8, NB], BF16)
    cvt = sc.sb("p1_cv", [128, NCV])
    S.dma(S.q_sync, cvt[:], C.cv[l], W=[cvt])
    stg = [sc.sb(f"p1_stg{i}", [128, NA]) for i in range(2)]
    wAv = C.wA[l].rearrange("(k p) c -> p k c", p=128)
    wBv = C.wB[l].rearrange("(k p) c -> p k c", p=128)
    n = 0
    for k in range(8):
        for (src, dst, nc_) in ((wAv, wA, NA), (wBv, wB, NB)):
            sg = stg[n % 2]
            S.dma(S.q_sync if n % 2 == 0 else S.q_pool, sg[:, 0:nc_], src[:, k, :], W=[sg])
            eng = S.dve if n % 2 == 0 else S.pool
            S.ts(eng, dst[:, k, :], sg[:, 0:nc_], cvt[:, CV_LNG + k:CV_LNG + k + 1], R=[sg, cvt], W=[dst])
            n += 1
    hts = [sc.sb(f"p1_h{i}", [128, D]) for i in range(3)]
    junk = sc.sb("p1_junk", [128, D], BF16)
    xnb = [sc.sb(f"p1_xnb{i}", [128, D], BF16) for i in range(2)]
    ss = [sc.sb(f"p1_ss{i}", [128, 4]) for i in range(2)]
    xnT = [sc.sb(f"p1_xnT{i}", [128, 8, 512], BF16) for i in range(2)]
    oA = [sc.sb(f"p1_oA{i}", [128, NA]) for i in range(2)]
    oB = [sc.sb(f"p1_oB{i}", [128, 512]) for i in range(3)]
    nblk = T // 512
    ntile = T // 128
    evc = [0]
    def ev_eng():
        evc[0] += 1
        return S.act if evc[0] % 2 else S.dve

    def stageX(t):
        j, tt = t // 4, t % 4
        xT = xnT[j % 2]
        ht = rr(hts, st, "h")
        S.dma(S.q_sync, ht[:], hsrc[t * 128:(t + 1) * 128, :], R=[hsrc], W=[ht])
        s_ = rr(ss, st, "ss")
        S.actf(junk[:], ht[:], AF.Square, R=[ht], W=[junk, s_], accum=s_[:, 0:1])
        S.actf(s_[:, 1:2], s_[:, 0:1], AF.Sqrt, R=[s_], W=[s_], scale=1.0 / D, bias=G.eps_col[:, 0:1])
        S.op(S.dve, lambda a=s_: nc.vector.reciprocal(out=a[:, 2:3], in_=a[:, 1:2]), [s_], [s_])
        xb = rr(xnb, st, "xnb")
        S.ts(S.dve, xb[:], ht[:], s_[:, 2:3], R=[ht, s_], W=[xb])
        pb = rr(G.psb, st, "psb")
        S.tr([(pb[:, k * 128:(k + 1) * 128], xb[:, k * 128:(k + 1) * 128]) for k in range(8)], G.ident_bf[:],
             R=[xb, G.ident_bf], W=[pb])
        S.cp(S.act, xT[:, :, tt * 128:(tt + 1) * 128], pb[:].rearrange("p (k c) -> p k c", k=8), R=[pb], W=[xT])

    def stageM(t):
        j, tt = t // 4, t % 4
        xT = xnT[j % 2]
        o = rr(oA, st, "oA")
        c0 = 0
        while c0 < NA:
            cn = min(512, NA - c0)
            p = rr(G.ps, st, "ps")
            S.mm(p[:, 0:cn], [(xT[:, k, tt * 128:(tt + 1) * 128], wA[:, k, c0:c0 + cn]) for k in range(8)],
                 R=[xT, wA], W=[p])
            S.cp(ev_eng(), o[:, c0:c0 + cn], p[:, 0:cn], R=[p], W=[o])
            c0 += cn
        S.dma(S.q_pool, pa[t * 128:(t + 1) * 128, :], o[:], R=[o], W=[pa])

    def stageF(j):
        xT = xnT[j % 2]
        r0 = 0
        while r0 < NB:
            rn = min(128, NB - r0)
            p = rr(G.ps, st, "ps")
            S.mm(p[0:rn, :], [(wB[:, k, r0:r0 + rn], xT[:, k, :]) for k in range(8)], R=[xT, wB], W=[p])
            o = rr(oB, st, "oB")
            S.cp(ev_eng(), o[0:rn, :], p[0:rn, :], R=[p], W=[o])
            S.dma(S.q_pool, pbT[r0:r0 + rn, j * 512:(j + 1) * 512], o[0:rn, :], R=[o], W=[pbT])
            r0 += rn

    stageX(0)
    for t in range(ntile):
        if t + 1 < ntile:
            stageX(t + 1)
        stageM(t)
        if t % 4 == 3:
            stageF(t // 4)
    sc.close()


def phase_lru(S, C, G, l, pbT, ybT, T):
    nc = S.nc
    sc = Scope(S)
    st = {}
    cvt = sc.sb("p3_cv", [128, NCV])
    S.dma(S.q_sync, cvt[:], C.cv[l], W=[cvt])
    gwt = sc.sb("p3_gw", [128, 4, 2, 128])
    S.dma(S.q_sync, gwt[:], C.gw[l].rearrange("c p g q -> p c g q"), W=[gwt])
    c8 = sc.sb("p3_c8", [128, 12])
    S.actf(c8[:, 0:4], cvt[:, CV_LAM:CV_LAM + 4], AF.Exp, R=[cvt], W=[c8], scale=-1.0)
    S.actf(c8[:, 4:8], c8[:, 0:4], AF.Ln, R=[c8], W=[c8], bias=1.0)
    S.ts(S.dve, c8[:, 8:12], c8[:, 4:8], -8.0, R=[c8], W=[c8])
    ubs = [sc.sb(f"p3_ub{i}", [128, 515]) for i in range(3)]
    xcs = [sc.sb(f"p3_xc{i}", [128, 512]) for i in range(2)]
    grs = [sc.sb(f"p3_gr{i}", [128, 512]) for i in range(2)]
    gis = [sc.sb(f"p3_gi{i}", [128, 512]) for i in range(2)]
    a_s = [sc.sb(f"p3_a{i}", [128, 512]) for i in range(2)]
    oms = [sc.sb(f"p3_om{i}", [128, 512]) for i in range(2)]
    bxs = [sc.sb(f"p3_bx{i}", [128, 512]) for i in range(2)]
    hos = [sc.sb(f"p3_ho{i}", [128, 512]) for i in range(3)]
    zero = sc.sb("p3_zero", [128, 1])
    S.memset(S.dve, zero[:], 0.0, W=[zero])
    nblk = T // 512
    for ct in range(4):
        r0 = OB_UB + ct * 128
        cw = lambda j: cvt[:, CV_CW + j * 4 + ct:CV_CW + j * 4 + ct + 1]
        col = lambda off: cvt[:, off + ct:off + ct + 1]
        hprev, hprev_buf = zero[:, 0:1], zero
        for j in range(nblk):
            ub = rr(ubs, st, "ub")
            if j == 0:
                S.memset(S.pool, ub[:, 0:3], 0.0, W=[ub])
                S.dma(S.q_sync, ub[:, 3:515], pbT[r0:r0 + 128, 0:512], R=[pbT], W=[ub])
            else:
                S.dma(S.q_sync, ub[:], pbT[r0:r0 + 128, j * 512 - 3:(j + 1) * 512], R=[pbT], W=[ub])
            xc = rr(xcs, st, "xc")
            S.ts(S.dve, xc[:], ub[:, 0:512], cw(0), col(CV_CB), op0=ALU.mult, op1=ALU.add, R=[ub, cvt], W=[xc])
            for jj in range(1, 4):
                S.stt(S.dve, xc[:], ub[:, jj:jj + 512], cw(jj), xc[:], ALU.mult, ALU.add, R=[ub, cvt, xc], W=[xc])
            p1 = rr(G.ps, st, "ps"); p2 = rr(G.ps, st, "ps")
            S.mm(p1[:], [(gwt[:, ct, 0, :], xc[:])], R=[gwt, xc], W=[p1])
            S.mm(p2[:], [(gwt[:, ct, 1, :], xc[:])], R=[gwt, xc], W=[p2])
            gr = rr(grs, st, "gr"); gi = rr(gis, st, "gi")
            S.actf(gr[:], p1[:], AF.Sigmoid, R=[p1, cvt], W=[gr], bias=col(CV_GAB))
            S.actf(gi[:], p2[:], AF.Sigmoid, R=[p2, cvt], W=[gi], bias=col(CV_GXB))
            a = rr(a_s, st, "a"); om = rr(oms, st, "om"); bx = rr(bxs, st, "bx")
            S.actf(a[:], gr[:], AF.Exp, R=[gr, c8], W=[a], scale=c8[:, 8 + ct:9 + ct])
            S.tt(S.pool, bx[:], gi[:], xc[:], ALU.mult, R=[gi, xc], W=[bx])
            S.tt(S.dve, om[:], a[:], a[:], ALU.mult, R=[a], W=[om])
            S.actf(om[:], om[:], AF.Sqrt, R=[om], W=[om], scale=-1.0, bias=1.0)
            S.tt(S.pool, bx[:], bx[:], om[:], ALU.mult, R=[bx, om], W=[bx])
            ho = rr(hos, st, "ho")
            S.op(S.dve, lambda ho=ho, a=a, bx=bx, hp=hprev: nc.vector.tensor_tensor_scan(
                out=ho[:], data0=a[:], data1=bx[:], initial=hp, op0=ALU.mult, op1=ALU.add),
                [a, bx, hprev_buf], [ho])
            S.dma(S.q_pool, ybT[ct * 128:(ct + 1) * 128, j * 512:(j + 1) * 512], ho[:], R=[ho], W=[ybT])
            hprev, hprev_buf = ho[:, 511:512], ho
    sc.close()


def phase_mla(S, C, G, l, pbT, ycT, T):
    nc = S.nc
    sc = Scope(S)
    st = {}
    nblk = T // 512
    cvt = sc.sb("p4_cv", [128, NCV])
    S.dma(S.q_sync, cvt[:], C.cv[l], W=[cvt])
    wq = sc.sb("p4_wq", [128, 2, 1024], BF16)
    wqs = sc.sb("p4_wqs", [128, 2, 1024], BF16)
    wk = sc.sb("p4_wk", [128, 1024], BF16)
    wv = sc.sb("p4_wv", [128, 512], BF16)
    sc2 = Scope(S)
    stg = [sc2.sb(f"p4_stg{i}", [128, 1024]) for i in range(2)]
    jobs = [(C.wq[l][0:128, :], wq[:, 0, :], 1024, wq), (C.wq[l][128:256, :], wq[:, 1, :], 1024, wq),
            (C.wqs[l][0:128, :], wqs[:, 0, :], 1024, wqs), (C.wqs[l][128:256, :], wqs[:, 1, :], 1024, wqs),
            (C.wk[l], wk[:], 1024, wk), (C.wv[l], wv[:], 512, wv)]
    for n, (src, dst, ncol, dbuf) in enumerate(jobs):
        sg = stg[n % 2]
        S.dma(S.q_sync, sg[:, 0:ncol], src, W=[sg])
        S.cp(S.dve if n % 2 == 0 else S.pool, dst, sg[:, 0:ncol], R=[sg], W=[dbuf])
    sc2.close()
    E65 = sc.sb("p4_E96", [128, 64])
    S.memset(S.dve, E65[:], 0.0, W=[E65])
    S.memset(S.dve, E65[64:65, :], 1.0, W=[E65])
    KT = [sc.sb(f"p4_KT{j}", [128, 8, 512], BF16) for j in range(nblk)]
    VP = [sc.sb(f"p4_VP{j}", [128, 4, 8, 96], BF16) for j in range(nblk)]
    for j in range(nblk):
        S.memset(S.pool, KT[j][32:64, :, :], 0.0, W=[KT[j]])
        S.memset(S.pool, VP[j][:, :, :, 64:96], 0.0, W=[VP[j]])
        S.memset(S.pool, VP[j][:, :, :, 64:65], 1.0, W=[VP[j]])
    QT = [[sc.sb(f"p4_QT{b}_{h}", [128, 512], BF16) for h in range(8)] for b in range(1)]
    cst = [sc.sb(f"p4_cs{i}", [128, 2, 512]) for i in range(1)]
    qls = [sc.sb(f"p4_ql{i}", [128, 2, 512]) for i in range(1)]
    kvls = [sc.sb(f"p4_kvl{i}", [128, 512]) for i in range(2)]
    krs = [sc.sb(f"p4_kr{i}", [32, 2, 512]) for i in range(2)]
    sq = sc.sb("p4_sq", [128, 2, 512])
    rq = sc.sb("p4_rq", [128, 512]); rk = sc.sb("p4_rk", [128, 512])
    qn = sc.sb("p4_qn", [128, 2, 512], BF16)
    ckv = sc.sb("p4_ckv", [128, 512], BF16)
    t1s = [sc.sb(f"p4_t1{i}", [128, 512]) for i in range(2)]
    t2s = [sc.sb(f"p4_t2{i}", [128, 512]) for i in range(2)]
    pTs = [sc.sb(f"p4_pT{i}", [128, 512], BF16) for i in range(3)]
    osbs = [sc.sb(f"p4_osb{i}", [96, 512]) for i in range(2)]
    rls = [sc.sb(f"p4_rl{i}", [64, 512]) for i in range(2)]
    ohs = [sc.sb(f"p4_oh{i}", [64, 512]) for i in range(2)]
    psS = G.ps[0:3]; psO = G.ps[3:5]; psL = G.ps[5]
    scale = 96.0 ** -0.5
    for j in range(nblk):
        blk = slice(j * 512, (j + 1) * 512)
        ql = rr(qls, st, "ql"); kvl = rr(kvls, st, "kvl"); kr = rr(krs, st, "kr"); cs = rr(cst, st, "cs")
        for k in range(2):
            S.dma(S.q_sync, ql[:, k, :], pbT[OB_QL + k * 128:OB_QL + (k + 1) * 128, blk], R=[pbT], W=[ql])
        S.dma(S.q_sync, kvl[:], pbT[OB_KV:OB_KV + 128, blk], R=[pbT], W=[kvl])
        S.dma(S.q_pool, kr[:, 0, :], pbT[OB_KR:OB_KR + 32, blk], R=[pbT], W=[kr])
        S.dma(S.q_pool, kr[:, 1, :], pbT[OB_KRS:OB_KRS + 32, blk], R=[pbT], W=[kr])
        S.dma(S.q_pool, cs[:], C.cs[:, :, blk], W=[cs])
        S.actf(sq[:], ql[:], AF.Square, R=[ql], W=[sq])
        pA = rr(psS, st, "psS")
        S.mm(pA[:], [(G.ones_f[:], sq[:, 0, :]), (G.ones_f[:], sq[:, 1, :])], R=[G.ones_f, sq], W=[pA])
        S.actf(rq[:], pA[:], AF.Sqrt, R=[pA], W=[rq], scale=1.0 / QL, bias=G.eps_col[:, 0:1])
        S.op(S.dve, lambda: nc.vector.reciprocal(out=rq[:], in_=rq[:]), [rq], [rq])
        for k in range(2):
            S.stt(S.dve, qn[:, k, :], ql[:, k, :], cvt[:, CV_QNG + k:CV_QNG + k + 1], rq[:], ALU.mult, ALU.mult,
                  R=[ql, cvt, rq], W=[qn])
        S.actf(sq[:, 0, :], kvl[:], AF.Square, R=[kvl], W=[sq])
        pB = rr(psS, st, "psS")
        S.mm(pB[:], [(G.ones_f[:], sq[:, 0, :])], R=[G.ones_f, sq], W=[pB])
        S.actf(rk[:], pB[:], AF.Sqrt, R=[pB], W=[rk], scale=1.0 / KVL, bias=G.eps_col[:, 0:1])
        S.op(S.dve, lambda: nc.vector.reciprocal(out=rk[:], in_=rk[:]), [rk], [rk])
        S.stt(S.dve, ckv[:], kvl[:], cvt[:, CV_KVNG:CV_KVNG + 1], rk[:], ALU.mult, ALU.mult, R=[kvl, cvt, rk], W=[ckv])
        t1 = rr(t1s, st, "t1"); t2 = rr(t2s, st, "t2")
        S.tt(S.dve, t1[0:32, :], kr[:, 0, :], cs[0:32, 0, :], ALU.mult, R=[kr, cs], W=[t1])
        S.tt(S.pool, t2[0:32, :], kr[:, 1, :], cs[0:32, 1, :], ALU.mult, R=[kr, cs], W=[t2])
        for h in range(8):
            S.tt(S.pool if h % 2 else S.dve, KT[j][0:32, h, :], t1[0:32, :], t2[0:32, :], ALU.add, R=[t1, t2], W=[KT[j]])
        for h in range(8):
            Qh = QT[0][h]
            pq = rr(psS, st, "psS")
            S.mm(pq[:], [(wq[:, k, h * 128:(h + 1) * 128], qn[:, k, :]) for k in range(2)], R=[wq, qn], W=[pq])
            pqs = rr(psS, st, "psS")
            S.mm(pqs[:], [(wqs[:, k, h * 128:(h + 1) * 128], qn[:, k, :]) for k in range(2)], R=[wqs, qn], W=[pqs])
            t1 = rr(t1s, st, "t1"); t2 = rr(t2s, st, "t2")
            S.tt(S.dve, t1[:], pq[:], cs[:, 0, :], ALU.mult, R=[pq, cs], W=[t1])
            S.tt(S.dve, t2[:], pqs[:], cs[:, 1, :], ALU.mult, R=[pqs, cs], W=[t2])
            S.tt(S.pool, Qh[:], t1[:], t2[:], ALU.add, R=[t1, t2], W=[Qh])
            pk = rr(psS, st, "psS")
            S.mm(pk[:], [(wk[:, h * 128:(h + 1) * 128], ckv[:])], R=[wk, ckv], W=[pk])
            S.cp(S.act, KT[j][64:128, h, :], pk[64:128, :], R=[pk], W=[KT[j]])
        for tt in range(4):
            pv = rr(psS, st, "psS")
            S.mm(pv[:], [(ckv[:, tt * 128:(tt + 1) * 128], wv[:])], R=[ckv, wv], W=[pv])
            S.cp(S.act if tt % 2 else S.dve, VP[j][:, tt, :, 0:64], pv[:].rearrange("p (h d) -> p h d", h=8),
                 R=[pv], W=[VP[j]])
        nkt = 4 * j + 4
        for h in range(8):
            Qh = QT[0][h]
            po = rr(psO, st, "psO")
            def issue_S(kt):
                i = kt - 4 * j
                c0 = 128 * max(i, 0)
                N = 512 - c0
                kb, ko = kt // 4, (kt % 4) * 128
                pS = rr(psS, st, "psS")
                S.mm(pS[:, 0:N], [(KT[kb][:, h, ko:ko + 128], Qh[:, c0:512])], R=[KT[kb], Qh], W=[pS])
                return pS
            pS_next = issue_S(0)
            for kt in range(nkt):
                i = kt - 4 * j
                c0 = 128 * max(i, 0)
                N = 512 - c0
                kb = kt // 4
                pS = pS_next
                if kt + 1 < nkt:
                    pS_next = issue_S(kt + 1)
                pT = rr(pTs, st, "pT")
                S.actf(pT[:, 0:N], pS[:, 0:N], AF.Exp, R=[pS], W=[pT], scale=scale)
                if i >= 0:
                    S.tt(S.pool, pT[:, 0:128], pT[:, 0:128], G.mask_bf[:], ALU.mult, R=[pT, G.mask_bf], W=[pT])
                S.mm(po[0:96, c0:512], [(VP[kb][:, kt % 4, h, :], pT[:, 0:N])], R=[VP[kb], pT], W=[po],
                     start=(kt == 0), stop=(kt == nkt - 1))
            osb = rr(osbs, st, "osb")
            S.cp(S.act, osb[:], po[0:96, :], R=[po], W=[osb])
            S.mm(psL[0:64, :], [(E65[0:96, :], osb[0:96, :])], R=[E65, osb], W=[psL])
            rl = rr(rls, st, "rl")
            S.op(S.dve, lambda rl=rl: nc.vector.reciprocal(out=rl[:], in_=psL[0:64, :]), [psL], [rl])
            oh = rr(ohs, st, "oh")
            S.tt(S.pool, oh[:], osb[0:64, :], rl[:], ALU.mult, R=[osb, rl], W=[oh])
            S.dma(S.q_sync, ycT[h * 64:(h + 1) * 64, blk], oh[:], R=[oh], W=[ycT])
    sc.close()


INV_DT = BF16
CDEC = 0.6065306597126334
GN_EPS = 64e-5


def bc_mid(ap, n):
    return ap.unsqueeze(1).broadcast_to([ap.shape[0], n, ap.shape[1]])


def bc_last(ap, n):
    return ap.unsqueeze(2).broadcast_to([ap.shape[0], ap.shape[1], n])


def h3(ap):
    return ap.rearrange("p (h d) -> p h d", h=8)


def phase_rwkv(S, C, G, l, pa, yga, T, dbg=None):
    nc = S.nc
    sc = Scope(S)
    st = {}
    nch = T // 128
    rvt = sc.sb("p2_rv", [128, NRV])
    S.dma(S.q_sync, rvt[:], C.rv[l:l + 1, :].partition_broadcast(128), W=[rvt])
    w0a0 = sc.sb("p2_w0a0", [1, 1024])
    S.dma(S.q_sync, w0a0[:], C.w0a0[l:l + 1, :], W=[w0a0])
    w2a2 = sc.sb("p2_w2a2", [128, 512])
    S.dma(S.q_sync, w2a2[:], C.w2a2[l], W=[w2a2])
    mu_b = rvt[:, RV_MU:RV_MU + 1664]
    kk_b = rvt[:, RV_KK:RV_KK + 512]; ka_b = rvt[:, RV_KA:RV_KA + 512]; rk_b = rvt[:, RV_RK:RV_RK + 512]
    lg_b = rvt[:, RV_LG:RV_LG + 512]; lb_b = rvt[:, RV_LB:RV_LB + 512]
    M_strict, M_incl, M_lower = G.masks[:, 0, :], G.masks[:, 1, :], G.masks[:, 2, :]
    ident_i = G.ident_f if INV_DT == F32 else G.ident_bf

    def tileset(i):
        X = Ctx()
        f32t = lambda n: sc.sb(f"p2_{n}{i}", [128, 512])
        b16t = lambda n: sc.sb(f"p2_{n}{i}", [128, 512], BF16)
        X.ua = sc.sb(f"p2_ua{i}", [128, 1664]); X.up = sc.sb(f"p2_up{i}", [128, 1664]); X.za = f32t("za")
        X.lw = sc.sb(f"p2_lw{i}", [128, 128])
        X.sgw, X.av, X.gam, X.ig, X.gae = f32t("sgw"), f32t("av"), f32t("gam"), f32t("ig"), f32t("gae")
        X.kk, X.sqt, X.kkn, X.e1, X.knew = f32t("kk"), f32t("sqt"), f32t("kkn"), f32t("e1"), f32t("knew")
        X.ssq = sc.sb(f"p2_ssq{i}", [128, 32]); X.ssb = sc.sb(f"p2_ssb{i}", [128, 32])
        X.gC = sc.sb(f"p2_gC{i}", [64, 8])
        X.kap_b, X.ktl_b, X.btl_b, X.rtl_b, X.v_b = b16t("kapb"), b16t("ktlb"), b16t("btlb"), b16t("rtlb"), b16t("vb")
        X.KR = sc.sb(f"p2_KR{i}", [64, 8, 2, 128], BF16)
        X.KB = sc.sb(f"p2_KB{i}", [64, 8, 2, 128], BF16)
        X.AK = sc.sb(f"p2_AK{i}", [128, 8, 2, 128], BF16)
        X.AB2 = sc.sb(f"p2_AB2{i}", [128, 8, 128], BF16)
        X.Qs = [sc.sb(f"p2_Q{i}_{k}", [128, 8, 128], INV_DT) for k in range(2)]
        X.QTs = [sc.sb(f"p2_QT{i}_{k}", [128, 8, 128], INV_DT) for k in range(2)]
        X.Ys = [sc.sb(f"p2_Y{i}_{k}", [128, 8, 128], INV_DT) for k in range(2)]
        X.R_sb = sc.sb(f"p2_R{i}", [128, 512], INV_DT)
        X.negU = b16t("negU")
        X.yt, X.yc, X.sq2, X.rkt, X.sz, X.bon, X.ygo = f32t("yt"), f32t("yc"), f32t("sq2"), f32t("rkt"), f32t("sz"), f32t("bon"), f32t("ygo")
        return X

    TS = [tileset(0), tileset(1)]
    Hf = sc.sb("p2_Hf", [64, 512]); Hb = sc.sb("p2_Hb", [64, 512], BF16); Htmp = sc.sb("p2_Htmp", [64, 512])
    S.memset(S.dve, Hf[:], 0.0, W=[Hf]); S.memset(S.dve, Hb[:], 0.0, W=[Hb])
    evn = [0]
    def ev_eng():
        evn[0] += 1
        return S.act if evn[0] % 2 else S.dve

    def stageA(c):
        X = TS[c % 2]
        ua, up, za = X.ua, X.up, X.za
        rows = slice(c * 128, (c + 1) * 128)
        S.dma(S.q_sync, ua[:], pa[rows, 0:1664], R=[pa], W=[ua])
        S.dma(S.q_sync, za[:], pa[rows, 1664:2176], R=[pa], W=[za])
        if c == 0:
            S.memset(S.pool, up[0:1, :], 0.0, W=[up])
            S.dma(S.q_sync, up[1:128, :], pa[0:127, 0:1664], R=[pa], W=[up])
        else:
            S.dma(S.q_sync, up[:], pa[c * 128 - 1:c * 128 + 127, 0:1664], R=[pa], W=[up])
        yield
        S.tt(S.pool, up[:], up[:], ua[:], ALU.subtract, R=[up, ua], W=[up])
        S.tt(S.dve, up[:], up[:], mu_b, ALU.mult, R=[up, rvt], W=[up])
        S.tt(S.pool, up[:], up[:], ua[:], ALU.add, R=[up, ua], W=[up])
        yield
        um = up
        r_, k_, v_ = um[:, 0:512], um[:, 512:1024], um[:, 1024:1536]
        pl_ = rr(G.ps, st, "ps")
        S.tr([(pl_[:, 0:128], um[:, 1536:1664])], G.ident_f[:], R=[um, G.ident_f], W=[pl_])
        S.actf(X.lw[0:64, :], pl_[0:64, 0:128], AF.Tanh, R=[pl_], W=[X.lw])
        S.cp(S.dve, X.lw[64:128, :], pl_[64:128, 0:128], R=[pl_], W=[X.lw])
        yield
        pzw = rr(G.ps, st, "ps"); pza = rr(G.ps, st, "ps")
        S.mm(pzw[:], [(X.lw[0:64, :], w2a2[0:64, :]), (G.ones_f[0:1, :], w0a0[0:1, 0:512])], R=[X.lw, w2a2, w0a0, G.ones_f], W=[pzw])
        S.mm(pza[:], [(X.lw[64:128, :], w2a2[64:128, :]), (G.ones_f[0:1, :], w0a0[0:1, 512:1024])], R=[X.lw, w2a2, w0a0, G.ones_f], W=[pza])
        S.actf(X.sgw[:], pzw[:], AF.Sigmoid, R=[pzw], W=[X.sgw])
        S.actf(X.av[:], pza[:], AF.Sigmoid, R=[pza], W=[X.av])
        yield
        pci = rr(G.ps, st, "ps"); pce = rr(G.ps, st, "ps"); pgc = rr(G.ps, st, "ps")
        S.mm(pci[:], [(M_incl, X.sgw[:])], R=[G.masks, X.sgw], W=[pci])
        S.mm(pce[:], [(M_strict, X.sgw[:])], R=[G.masks, X.sgw], W=[pce])
        sgw = X.sgw
        S.group(S.pe, [(lambda h=h: nc.tensor.matmul(pgc[0:64, h:h + 1], lhsT=sgw[:, h * 64:(h + 1) * 64], rhs=G.ones_f[:, 0:1],
                                                      start=True, stop=True)) for h in range(8)], [X.sgw, G.ones_f], [pgc])
        S.actf(X.gam[:], pci[:], AF.Exp, R=[pci], W=[X.gam], scale=-CDEC)
        S.actf(X.ig[:], pci[:], AF.Exp, R=[pci], W=[X.ig], scale=CDEC)
        S.actf(X.gae[:], pce[:], AF.Exp, R=[pce], W=[X.gae], scale=-CDEC)
        S.actf(X.gC[:], pgc[0:64, 0:8], AF.Exp, R=[pgc], W=[X.gC], scale=-CDEC)
        yield
        ssq = X.ssq
        S.tt(S.dve, X.kk[:], k_, kk_b, ALU.mult, R=[um, rvt], W=[X.kk])
        S.tt(S.pool, X.sqt[:], X.kk[:], X.kk[:], ALU.mult, R=[X.kk], W=[X.sqt])
        S.op(S.dve, lambda: nc.vector.tensor_reduce(out=ssq[:, 0:8], in_=h3(X.sqt[:]), axis=AX.X, op=ALU.add), [X.sqt], [ssq])
        S.actf(ssq[:, 8:16], ssq[:, 0:8], AF.Sqrt, R=[ssq], W=[ssq])
        S.ts(S.dve, ssq[:, 8:16], ssq[:, 8:16], 1e-12, op0=ALU.max, R=[ssq], W=[ssq])
        S.op(S.dve, lambda: nc.vector.reciprocal(out=ssq[:, 16:24], in_=ssq[:, 8:16]), [ssq], [ssq])
        S.tt(S.dve, h3(X.kkn[:]), h3(X.kk[:]), bc_last(ssq[:, 16:24], 64), ALU.mult, R=[X.kk, ssq], W=[X.kkn])
        yield
        S.stt(S.dve, X.e1[:], X.av[:], -1.0, ka_b, ALU.add, ALU.mult, R=[X.av, rvt], W=[X.e1])
        S.stt(S.dve, X.knew[:], X.e1[:], 1.0, k_, ALU.add, ALU.mult, R=[X.e1, um], W=[X.knew])
        S.tt(S.pool, X.kap_b[:], X.kkn[:], X.gae[:], ALU.mult, R=[X.kkn, X.gae], W=[X.kap_b])
        yield
        S.tt(S.dve, X.ktl_b[:], X.knew[:], X.ig[:], ALU.mult, R=[X.knew, X.ig], W=[X.ktl_b])
        S.tt(S.pool, X.e1[:], X.kkn[:], X.av[:], ALU.mult, R=[X.kkn, X.av], W=[X.e1])
        S.tt(S.pool, X.btl_b[:], X.e1[:], X.ig[:], ALU.mult, R=[X.e1, X.ig], W=[X.btl_b])
        S.tt(S.dve, X.rtl_b[:], r_, X.gam[:], ALU.mult, R=[um, X.gam], W=[X.rtl_b])
        S.cp(S.pool, X.v_b[:], v_, R=[um], W=[X.v_b])
        yield
        for (src, dst, wi) in ((X.kap_b, X.KR, 0), (X.rtl_b, X.KR, 1), (X.ktl_b, X.KB, 0), (X.btl_b, X.KB, 1)):
            pb = rr(G.psb, st, "psb")
            S.tr([(pb[0:64, h * 128:(h + 1) * 128], src[:, h * 64:(h + 1) * 64]) for h in range(8)], G.ident_bf[:],
                 R=[src, G.ident_bf], W=[pb])
            S.cp(ev_eng(), dst[:, :, wi, :], pb[0:64, :].rearrange("p (h t) -> p h t", h=8), R=[pb], W=[dst])
            yield
        KR, KB, AK, AB2 = X.KR, X.KB, X.AK, X.AB2
        Q, QT, Y = X.Qs[0], X.QTs[0], X.Ys[0]
        for hp in range(4):
            p1 = rr(G.ps, st, "ps"); p2 = rr(G.ps, st, "ps")
            for hh in range(2):
                h = hp * 2 + hh
                S.mm(p1[:, hh * 256:(hh + 1) * 256], [(KB[:, h, 0, :], KR[:, h, :, :])], R=[KB, KR], W=[p1])
                S.mm(p2[:, hh * 256:(hh + 1) * 256], [(KB[:, h, 1, :], KR[:, h, :, :])], R=[KB, KR], W=[p2])
            hs = slice(hp * 2, hp * 2 + 2)
            p1v = p1[:].rearrange("p (h w t) -> p h w t", h=2, w=2)
            p2v = p2[:].rearrange("p (h w t) -> p h w t", h=2, w=2)
            for hh in range(2):
                h = hp * 2 + hh
                S.tt(S.dve, AK[:, h, :, :], p1v[:, hh, :, :], G.masks[:, 0:2, :], ALU.mult, R=[p1, G.masks], W=[AK])
            S.tt(S.dve, AB2[:, hs, :], p2v[:, :, 1, :], bc_mid(M_incl, 2), ALU.mult, R=[p2, G.masks], W=[AB2])
            S.stt(S.dve, QT[:, hs, :], p2v[:, :, 0, :], -1.0, bc_mid(M_strict, 2), ALU.mult, ALU.mult, R=[p2, G.masks], W=[QT])
            S.tt(S.pool, Y[:, hs, :], QT[:, hs, :], bc_mid(ident_i[:], 2), ALU.add, R=[QT, ident_i], W=[Y])
            yield
        for hq in range(2):
            p3 = rr(G.ps, st, "ps")
            for hh in range(4):
                h = hq * 4 + hh
                S.mm(p3[:, hh * 128:(hh + 1) * 128], [(KR[:, h, 0, :], KB[:, h, 1, :])], R=[KR, KB], W=[p3])
            hs = slice(hq * 4, hq * 4 + 4)
            S.stt(S.dve, Q[:, hs, :], p3[:].rearrange("p (h t) -> p h t", h=4), -1.0, bc_mid(M_lower, 4), ALU.mult, ALU.mult,
                  R=[p3, G.masks], W=[Q])
            yield
        k_ = 0
        for lev in range(1, 7):
            k_ ^= 1
            Qn, QTn, Yn = X.Qs[k_], X.QTs[k_], X.Ys[k_]
            last = lev == 6
            for hq in range(2):
                hs = slice(hq * 4, hq * 4 + 4)
                pq = rr(G.ps, st, "ps")
                for hh in range(4):
                    h = hq * 4 + hh
                    S.mm(pq[:, hh * 128:(hh + 1) * 128], [(QT[:, h, :], Q[:, h, :])], R=[QT, Q], W=[pq])
                S.cp(ev_eng(), Qn[:, hs, :], pq[:].rearrange("p (h t) -> p h t", h=4), R=[pq], W=[Qn])
                if not last:
                    pqt = rr(G.ps, st, "ps")
                    for hh in range(4):
                        h = hq * 4 + hh
                        S.mm(pqt[:, hh * 128:(hh + 1) * 128], [(Q[:, h, :], QT[:, h, :])], R=[QT, Q], W=[pqt])
                    S.cp(ev_eng(), QTn[:, hs, :], pqt[:].rearrange("p (h t) -> p h t", h=4), R=[pqt], W=[QTn])
                yield
            for hq in range(2):
                hs = slice(hq * 4, hq * 4 + 4)
                py = rr(G.ps, st, "ps")
                for hh in range(4):
                    h = hq * 4 + hh
                    S.mm(py[:, hh * 128:(hh + 1) * 128], [(Qn[:, h, :], Y[:, h, :])], R=[Qn, Y], W=[py])
                S.tt(S.dve, Yn[:, hs, :], py[:].rearrange("p (h t) -> p h t", h=4), Y[:, hs, :], ALU.add, R=[py, Y], W=[Yn])
                yield
            Q, QT, Y = Qn, QTn, Yn
        X.Yfin = Y
        ssb = X.ssb
        S.tt(S.pool, X.rkt[:], r_, X.knew[:], ALU.mult, R=[um, X.knew], W=[X.rkt])
        S.tt(S.pool, X.rkt[:], X.rkt[:], rk_b, ALU.mult, R=[X.rkt, rvt], W=[X.rkt])
        S.op(S.dve, lambda: nc.vector.tensor_reduce(out=ssb[:, 0:8], in_=h3(X.rkt[:]), axis=AX.X, op=ALU.add), [X.rkt], [ssb])
        S.tt(S.pool, h3(X.bon[:]), h3(v_), bc_last(ssb[:, 0:8], 64), ALU.mult, R=[um, ssb], W=[X.bon])
        S.actf(X.sz[:], za[:], AF.Silu, R=[za], W=[X.sz])
        yield

    def stageB(c):
        X = TS[c % 2]
        KR, AK, AB2, Y = X.KR, X.AK, X.AB2, X.Yfin
        v_b, negU, R_sb, ssb = X.v_b, X.negU, X.R_sb, X.ssb
        rows = slice(c * 128, (c + 1) * 128)
        pR = rr(G.ps, st, "ps")
        for h in range(8):
            cs_ = slice(h * 64, (h + 1) * 64)
            S.mm(pR[:, cs_], [(KR[:, h, 0, :], Hb[:, cs_]), (AK[:, h, 0, :], v_b[:, cs_])], R=[KR, Hb, AK, v_b], W=[pR])
        S.cp(S.act, R_sb[:], pR[:], R=[pR], W=[R_sb])
        yield
        pU = rr(G.ps, st, "ps")
        for h in range(8):
            cs_ = slice(h * 64, (h + 1) * 64)
            S.mm(pU[:, cs_], [(Y[:, h, :], R_sb[:, cs_])], R=[Y, R_sb], W=[pU])
        S.ts(S.dve, negU[:], pU[:], -1.0, R=[pU], W=[negU])
        yield
        pH = rr(G.ps, st, "ps")
        for h in range(8):
            cs_ = slice(h * 64, (h + 1) * 64)
            S.mm(pH[0:64, cs_], [(X.ktl_b[:, cs_], v_b[:, cs_]), (X.btl_b[:, cs_], negU[:, cs_])], R=[X.ktl_b, v_b, X.btl_b, negU], W=[pH])
        pY = rr(G.ps, st, "ps")
        for h in range(8):
            cs_ = slice(h * 64, (h + 1) * 64)
            S.mm(pY[:, cs_], [(KR[:, h, 1, :], Hb[:, cs_]), (AK[:, h, 1, :], v_b[:, cs_]), (AB2[:, h, :], negU[:, cs_])],
                 R=[KR, Hb, AK, v_b, AB2, negU], W=[pY])
        S.cp(S.act, X.yt[:], pY[:], R=[pY], W=[X.yt])
        S.tt(S.dve, Htmp[:], pH[0:64, :], Hf[:], ALU.add, R=[pH, Hf], W=[Htmp])
        S.tt(S.dve, h3(Hf[:]), h3(Htmp[:]), bc_last(X.gC[:], 64), ALU.mult, R=[Htmp, X.gC], W=[Hf])
        S.cp(S.pool, Hb[:], Hf[:], R=[Hf], W=[Hb])
        yield
        yt, yc, sq2 = X.yt, X.yc, X.sq2
        S.op(S.dve, lambda: nc.vector.tensor_reduce(out=ssb[:, 8:16], in_=h3(yt[:]), axis=AX.X, op=ALU.add), [yt], [ssb])
        S.ts(S.dve, ssb[:, 8:16], ssb[:, 8:16], -1.0 / 64, R=[ssb], W=[ssb])
        S.tt(S.pool, h3(yc[:]), h3(yt[:]), bc_last(ssb[:, 8:16], 64), ALU.add, R=[yt, ssb], W=[yc])
        S.tt(S.pool, sq2[:], yc[:], yc[:], ALU.mult, R=[yc], W=[sq2])
        yield
        S.op(S.dve, lambda: nc.vector.tensor_reduce(out=ssb[:, 16:24], in_=h3(sq2[:]), axis=AX.X, op=ALU.add), [sq2], [ssb])
        S.actf(ssb[:, 24:32], ssb[:, 16:24], AF.Sqrt, R=[ssb], W=[ssb], scale=1.0 / 64, bias=G.eps_col[:, 1:2])
        S.op(S.dve, lambda: nc.vector.reciprocal(out=ssb[:, 16:24], in_=ssb[:, 24:32]), [ssb], [ssb])
        S.tt(S.dve, h3(yc[:]), h3(yc[:]), bc_last(ssb[:, 16:24], 64), ALU.mult, R=[yc, ssb], W=[yc])
        yield
        S.tt(S.pool, yc[:], yc[:], lg_b, ALU.mult, R=[yc, rvt], W=[yc])
        S.tt(S.pool, yc[:], yc[:], lb_b, ALU.add, R=[yc, rvt], W=[yc])
        S.tt(S.pool, yc[:], yc[:], X.bon[:], ALU.add, R=[yc, X.bon], W=[yc])
        S.tt(S.dve, X.ygo[:], yc[:], X.sz[:], ALU.mult, R=[yc, X.sz], W=[X.ygo])
        S.dma(S.q_sync, yga[rows, :], X.ygo[:], R=[X.ygo], W=[yga])
        yield

    def drain(g):
        for _ in g:
            pass

    drain(stageA(0))
    for c in range(nch):
        gB = stageB(c)
        gA = stageA(c + 1) if c + 1 < nch else iter(())
        doneA = doneB = False
        while not (doneA and doneB):
            for _ in range(5):
                if not doneA:
                    try:
                        next(gA)
                    except StopIteration:
                        doneA = True
            if not doneB:
                try:
                    next(gB)
                except StopIteration:
                    doneB = True
    sc.close()


def phase_lru(S, C, G, l, pbT, ybT, T):
    nc = S.nc
    sc = Scope(S)
    st = {}
    cvt = sc.sb("p3_cv", [128, NCV])
    S.dma(S.q_sync, cvt[:], C.cv[l], W=[cvt])
    gwt = sc.sb("p3_gw", [128, 4, 2, 128])
    S.dma(S.q_sync, gwt[:], C.gw[l].rearrange("c p g q -> p c g q"), W=[gwt])
    c8 = sc.sb("p3_c8", [128, 12])
    S.actf(c8[:, 0:4], cvt[:, CV_LAM:CV_LAM + 4], AF.Exp, R=[cvt], W=[c8], scale=-1.0)
    S.actf(c8[:, 4:8], c8[:, 0:4], AF.Ln, R=[c8], W=[c8], bias=1.0)
    S.ts(S.dve, c8[:, 8:12], c8[:, 4:8], -8.0, R=[c8], W=[c8])
    ubs = [sc.sb(f"p3_ub{i}", [128, 515]) for i in range(3)]
    xcs = [sc.sb(f"p3_xc{i}", [128, 512]) for i in range(2)]
    grs = [sc.sb(f"p3_gr{i}", [128, 512]) for i in range(2)]
    gis = [sc.sb(f"p3_gi{i}", [128, 512]) for i in range(2)]
    a_s = [sc.sb(f"p3_a{i}", [128, 512]) for i in range(2)]
    oms = [sc.sb(f"p3_om{i}", [128, 512]) for i in range(2)]
    bxs = [sc.sb(f"p3_bx{i}", [128, 512]) for i in range(2)]
    hos = [sc.sb(f"p3_ho{i}", [128, 512]) for i in range(3)]
    zero = sc.sb("p3_zero", [128, 1])
    S.memset(S.dve, zero[:], 0.0, W=[zero])
    nblk = T // 512
    for ct in range(4):
        r0 = OB_UB + ct * 128
        cw = lambda j: cvt[:, CV_CW + j * 4 + ct:CV_CW + j * 4 + ct + 1]
        col = lambda off: cvt[:, off + ct:off + ct + 1]
        hprev, hprev_buf = zero[:, 0:1], zero
        for j in range(nblk):
            ub = rr(ubs, st, "ub")
            if j == 0:
                S.memset(S.pool, ub[:, 0:3], 0.0, W=[ub])
                S.dma(S.q_sync, ub[:, 3:515], pbT[r0:r0 + 128, 0:512], R=[pbT], W=[ub])
            else:
                S.dma(S.q_sync, ub[:], pbT[r0:r0 + 128, j * 512 - 3:(j + 1) * 512], R=[pbT], W=[ub])
            xc = rr(xcs, st, "xc")
            S.ts(S.dve, xc[:], ub[:, 0:512], cw(0), col(CV_CB), op0=ALU.mult, op1=ALU.add, R=[ub, cvt], W=[xc])
            for jj in range(1, 4):
                S.stt(S.dve, xc[:], ub[:, jj:jj + 512], cw(jj), xc[:], ALU.mult, ALU.add, R=[ub, cvt, xc], W=[xc])
            p1 = rr(G.ps, st, "ps"); p2 = rr(G.ps, st, "ps")
            S.mm(p1[:], [(gwt[:, ct, 0, :], xc[:])], R=[gwt, xc], W=[p1])
            S.mm(p2[:], [(gwt[:, ct, 1, :], xc[:])], R=[gwt, xc], W=[p2])
            gr = rr(grs, st, "gr"); gi = rr(gis, st, "gi")
            S.actf(gr[:], p1[:], AF.Sigmoid, R=[p1, cvt], W=[gr], bias=col(CV_GAB))
            S.actf(gi[:], p2[:], AF.Sigmoid, R=[p2, cvt], W=[gi], bias=col(CV_GXB))
            a = rr(a_s, st, "a"); om = rr(oms, st, "om"); bx = rr(bxs, st, "bx")
            S.actf(a[:], gr[:], AF.Exp, R=[gr, c8], W=[a], scale=c8[:, 8 + ct:9 + ct])
            S.tt(S.pool, bx[:], gi[:], xc[:], ALU.mult, R=[gi, xc], W=[bx])
            S.tt(S.dve, om[:], a[:], a[:], ALU.mult, R=[a], W=[om])
            S.actf(om[:], om[:], AF.Sqrt, R=[om], W=[om], scale=-1.0, bias=1.0)
            S.tt(S.pool, bx[:], bx[:], om[:], ALU.mult, R=[bx, om], W=[bx])
            ho = rr(hos, st, "ho")
            S.op(S.dve, lambda ho=ho, a=a, bx=bx, hp=hprev: nc.vector.tensor_tensor_scan(
                out=ho[:], data0=a[:], data1=bx[:], initial=hp, op0=ALU.mult, op1=ALU.add),
                [a, bx, hprev_buf], [ho])
            S.dma(S.q_pool, ybT[ct * 128:(ct + 1) * 128, j * 512:(j + 1) * 512], ho[:], R=[ho], W=[ybT])
            hprev, hprev_buf = ho[:, 511:512], ho
    sc.close()


def phase_mla(S, C, G, l, pbT, ycT, T):
    nc = S.nc
    sc = Scope(S)
    st = {}
    nblk = T // 512
    cvt = sc.sb("p4_cv", [128, NCV])
    S.dma(S.q_sync, cvt[:], C.cv[l], W=[cvt])
    wq = sc.sb("p4_wq", [128, 2, 1024], BF16)
    wqs = sc.sb("p4_wqs", [128, 2, 1024], BF16)
    wk = sc.sb("p4_wk", [128, 1024], BF16)
    wv = sc.sb("p4_wv", [128, 512], BF16)
    sc2 = Scope(S)
    stg = [sc2.sb(f"p4_stg{i}", [128, 1024]) for i in range(2)]
    jobs = [(C.wq[l][0:128, :], wq[:, 0, :], 1024, wq), (C.wq[l][128:256, :], wq[:, 1, :], 1024, wq),
            (C.wqs[l][0:128, :], wqs[:, 0, :], 1024, wqs), (C.wqs[l][128:256, :], wqs[:, 1, :], 1024, wqs),
            (C.wk[l], wk[:], 1024, wk), (C.wv[l], wv[:], 512, wv)]
    for n, (src, dst, ncol, dbuf) in enumerate(jobs):
        sg = stg[n % 2]
        S.dma(S.q_sync, sg[:, 0:ncol], src, W=[sg])
        S.cp(S.dve if n % 2 == 0 else S.pool, dst, sg[:, 0:ncol], R=[sg], W=[dbuf])
    sc2.close()
    E65 = sc.sb("p4_E96", [128, 64])
    S.memset(S.dve, E65[:], 0.0, W=[E65])
    S.memset(S.dve, E65[64:65, :], 1.0, W=[E65])
    KT = [sc.sb(f"p4_KT{j}", [128, 8, 512], BF16) for j in range(nblk)]
    VP = [sc.sb(f"p4_VP{j}", [128, 4, 8, 96], BF16) for j in range(nblk)]
    for j in range(nblk):
        S.memset(S.pool, KT[j][32:64, :, :], 0.0, W=[KT[j]])
        S.memset(S.pool, VP[j][:, :, :, 64:96], 0.0, W=[VP[j]])
        S.memset(S.pool, VP[j][:, :, :, 64:65], 1.0, W=[VP[j]])
    QT = [[sc.sb(f"p4_QT{b}_{h}", [128, 512], BF16) for h in range(8)] for b in range(1)]
    cst = [sc.sb(f"p4_cs{i}", [128, 2, 512]) for i in range(1)]
    qls = [sc.sb(f"p4_ql{i}", [128, 2, 512]) for i in range(1)]
    kvls = [sc.sb(f"p4_kvl{i}", [128, 512]) for i in range(2)]
    krs = [sc.sb(f"p4_kr{i}", [32, 2, 512]) for i in range(2)]
    sq = sc.sb("p4_sq", [128, 2, 512])
    rq = sc.sb("p4_rq", [128, 512]); rk = sc.sb("p4_rk", [128, 512])
    qn = sc.sb("p4_qn", [128, 2, 512], BF16)
    ckv = sc.sb("p4_ckv", [128, 512], BF16)
    t1s = [sc.sb(f"p4_t1{i}", [128, 512]) for i in range(2)]
    t2s = [sc.sb(f"p4_t2{i}", [128, 512]) for i in range(2)]
    pTs = [sc.sb(f"p4_pT{i}", [128, 512], BF16) for i in range(3)]
    osbs = [sc.sb(f"p4_osb{i}", [96, 512]) for i in range(2)]
    rls = [sc.sb(f"p4_rl{i}", [64, 512]) for i in range(2)]
    ohs = [sc.sb(f"p4_oh{i}", [64, 512]) for i in range(2)]
    psS = G.ps[0:3]; psO = G.ps[3:5]; psL = G.ps[5]
    scale = 96.0 ** -0.5
    for j in range(nblk):
        blk = slice(j * 512, (j + 1) * 512)
        ql = rr(qls, st, "ql"); kvl = rr(kvls, st, "kvl"); kr = rr(krs, st, "kr"); cs = rr(cst, st, "cs")
        for k in range(2):
            S.dma(S.q_sync, ql[:, k, :], pbT[OB_QL + k * 128:OB_QL + (k + 1) * 128, blk], R=[pbT], W=[ql])
        S.dma(S.q_sync, kvl[:], pbT[OB_KV:OB_KV + 128, blk], R=[pbT], W=[kvl])
        S.dma(S.q_pool, kr[:, 0, :], pbT[OB_KR:OB_KR + 32, blk], R=[pbT], W=[kr])
        S.dma(S.q_pool, kr[:, 1, :], pbT[OB_KRS:OB_KRS + 32, blk], R=[pbT], W=[kr])
        S.dma(S.q_pool, cs[:], C.cs[:, :, blk], W=[cs])
        S.actf(sq[:], ql[:], AF.Square, R=[ql], W=[sq])
        pA = rr(psS, st, "psS")
        S.mm(pA[:], [(G.ones_f[:], sq[:, 0, :]), (G.ones_f[:], sq[:, 1, :])], R=[G.ones_f, sq], W=[pA])
        S.actf(rq[:], pA[:], AF.Sqrt, R=[pA], W=[rq], scale=1.0 / QL, bias=G.eps_col[:, 0:1])
        S.op(S.dve, lambda: nc.vector.reciprocal(out=rq[:], in_=rq[:]), [rq], [rq])
        for k in range(2):
            S.stt(S.dve, qn[:, k, :], ql[:, k, :], cvt[:, CV_QNG + k:CV_QNG + k + 1], rq[:], ALU.mult, ALU.mult,
                  R=[ql, cvt, rq], W=[qn])
        S.actf(sq[:, 0, :], kvl[:], AF.Square, R=[kvl], W=[sq])
        pB = rr(psS, st, "psS")
        S.mm(pB[:], [(G.ones_f[:], sq[:, 0, :])], R=[G.ones_f, sq], W=[pB])
        S.actf(rk[:], pB[:], AF.Sqrt, R=[pB], W=[rk], scale=1.0 / KVL, bias=G.eps_col[:, 0:1])
        S.op(S.dve, lambda: nc.vector.reciprocal(out=rk[:], in_=rk[:]), [rk], [rk])
        S.stt(S.dve, ckv[:], kvl[:], cvt[:, CV_KVNG:CV_KVNG + 1], rk[:], ALU.mult, ALU.mult, R=[kvl, cvt, rk], W=[ckv])
        t1 = rr(t1s, st, "t1"); t2 = rr(t2s, st, "t2")
        S.tt(S.dve, t1[0:32, :], kr[:, 0, :], cs[0:32, 0, :], ALU.mult, R=[kr, cs], W=[t1])
        S.tt(S.pool, t2[0:32, :], kr[:, 1, :], cs[0:32, 1, :], ALU.mult, R=[kr, cs], W=[t2])
        for h in range(8):
            S.tt(S.pool if h % 2 else S.dve, KT[j][0:32, h, :], t1[0:32, :], t2[0:32, :], ALU.add, R=[t1, t2], W=[KT[j]])
        for h in range(8):
            Qh = QT[0][h]
            pq = rr(psS, st, "psS")
            S.mm(pq[:], [(wq[:, k, h * 128:(h + 1) * 128], qn[:, k, :]) for k in range(2)], R=[wq, qn], W=[pq])
            pqs = rr(psS, st, "psS")
            S.mm(pqs[:], [(wqs[:, k, h * 128:(h + 1) * 128], qn[:, k, :]) for k in range(2)], R=[wqs, qn], W=[pqs])
            t1 = rr(t1s, st, "t1"); t2 = rr(t2s, st, "t2")
            S.tt(S.dve, t1[:], pq[:], cs[:, 0, :], ALU.mult, R=[pq, cs], W=[t1])
            S.tt(S.dve, t2[:], pqs[:], cs[:, 1, :], ALU.mult, R=[pqs, cs], W=[t2])
            S.tt(S.pool, Qh[:], t1[:], t2[:], ALU.add, R=[t1, t2], W=[Qh])
            pk = rr(psS, st, "psS")
            S.mm(pk[:], [(wk[:, h * 128:(h + 1) * 128], ckv[:])], R=[wk, ckv], W=[pk])
            S.cp(S.act, KT[j][64:128, h, :], pk[64:128, :], R=[pk], W=[KT[j]])
        for tt in range(4):
            pv = rr(psS, st, "psS")
            S.mm(pv[:], [(ckv[:, tt * 128:(tt + 1) * 128], wv[:])], R=[ckv, wv], W=[pv])
            S.cp(S.act if tt % 2 else S.dve, VP[j][:, tt, :, 0:64], pv[:].rearrange("p (h d) -> p h d", h=8),
                 R=[pv], W=[VP[j]])
        nkt = 4 * j + 4
        for h in range(8):
            Qh = QT[0][h]
            po = rr(psO, st, "psO")
            def issue_S(kt):
                i = kt - 4 * j
                c0 = 128 * max(i, 0)
                N = 512 - c0
                kb, ko = kt // 4, (kt % 4) * 128
                pS = rr(psS, st, "psS")
                S.mm(pS[:, 0:N], [(KT[kb][:, h, ko:ko + 128], Qh[:, c0:512])], R=[KT[kb], Qh], W=[pS])
                return pS
            pS_next = issue_S(0)
            for kt in range(nkt):
                i = kt - 4 * j
                c0 = 128 * max(i, 0)
                N = 512 - c0
                kb = kt // 4
                pS = pS_next
                if kt + 1 < nkt:
                    pS_next = issue_S(kt + 1)
                pT = rr(pTs, st, "pT")
                S.actf(pT[:, 0:N], pS[:, 0:N], AF.Exp, R=[pS], W=[pT], scale=scale)
                if i >= 0:
                    S.tt(S.pool, pT[:, 0:128], pT[:, 0:128], G.mask_bf[:], ALU.mult, R=[pT, G.mask_bf], W=[pT])
                S.mm(po[0:96, c0:512], [(VP[kb][:, kt % 4, h, :], pT[:, 0:N])], R=[VP[kb], pT], W=[po],
                     start=(kt == 0), stop=(kt == nkt - 1))
            osb = rr(osbs, st, "osb")
            S.cp(S.act, osb[:], po[0:96, :], R=[po], W=[osb])
            S.mm(psL[0:64, :], [(E65[0:96, :], osb[0:96, :])], R=[E65, osb], W=[psL])
            rl = rr(rls, st, "rl")
            S.op(S.dve, lambda rl=rl: nc.vector.reciprocal(out=rl[:], in_=psL[0:64, :]), [psL], [rl])
            oh = rr(ohs, st, "oh")
            S.tt(S.pool, oh[:], osb[0:64, :], rl[:], ALU.mult, R=[osb, rl], W=[oh])
            S.dma(S.q_sync, ycT[h * 64:(h + 1) * 64, blk], oh[:], R=[oh], W=[ycT])
    sc.close()


INV_DT = BF16
CDEC = 0.6065306597126334
GN_EPS = 64e-5


def bc_mid(ap, n):
    return ap.unsqueeze(1).broadcast_to([ap.shape[0], n, ap.shape[1]])


def bc_last(ap, n):
    return ap.unsqueeze(2).broadcast_to([ap.shape[0], ap.shape[1], n])


def h3(ap):
    return ap.rearrange("p (h d) -> p h d", h=8)


def phase_rwkv(S, C, G, l, pa, yga, T, dbg=None):
    nc = S.nc
    sc = Scope(S)
    st = {}
    nch = T // 128
    rvt = sc.sb("p2_rv", [128, NRV])
    S.dma(S.q_sync, rvt[:], C.rv[l:l + 1, :].partition_broadcast(128), W=[rvt])
    w0a0 = sc.sb("p2_w0a0", [1, 1024])
    S.dma(S.q_sync, w0a0[:], C.w0a0[l:l + 1, :], W=[w0a0])
    w2a2 = sc.sb("p2_w2a2", [128, 512])
    S.dma(S.q_sync, w2a2[:], C.w2a2[l], W=[w2a2])
    mu_b = rvt[:, RV_MU:RV_MU + 1664]
    kk_b = rvt[:, RV_KK:RV_KK + 512]; ka_b = rvt[:, RV_KA:RV_KA + 512]; rk_b = rvt[:, RV_RK:RV_RK + 512]
    lg_b = rvt[:, RV_LG:RV_LG + 512]; lb_b = rvt[:, RV_LB:RV_LB + 512]
    M_strict, M_incl, M_lower = G.masks[:, 0, :], G.masks[:, 1, :], G.masks[:, 2, :]
    uas = [sc.sb(f"p2_ua{i}", [128, 1664]) for i in range(2)]
    ups = [sc.sb(f"p2_up{i}", [128, 1664]) for i in range(2)]
    zas = [sc.sb(f"p2_za{i}", [128, 512]) for i in range(2)]
    f32t = lambda n: sc.sb("p2_" + n, [128, 512])
    lw = sc.sb("p2_lw", [128, 128])
    sgw, av, gam, ig, gae = f32t("sgw"), f32t("av"), f32t("gam"), f32t("ig"), f32t("gae")
    kk, sqt, kkn, e1, knew = f32t("kk"), f32t("sqt"), f32t("kkn"), f32t("e1"), f32t("knew")
    ssq = sc.sb("p2_ssq", [128, 32])
    gC = sc.sb("p2_gC", [64, 8])
    kap_b = sc.sb("p2_kapb", [128, 512], BF16); ktl_b = sc.sb("p2_ktlb", [128, 512], BF16)
    btl_b = sc.sb("p2_btlb", [128, 512], BF16); rtl_b = sc.sb("p2_rtlb", [128, 512], BF16)
    v_b = sc.sb("p2_vb", [128, 512], BF16)
    KR = sc.sb("p2_KR", [64, 8, 2, 128], BF16)
    KB = sc.sb("p2_KB", [64, 8, 2, 128], BF16)
    AK = sc.sb("p2_AK", [128, 8, 2, 128], BF16)
    AB2 = sc.sb("p2_AB2", [128, 8, 128], BF16)
    Qs = [sc.sb(f"p2_Q{i}", [128, 8, 128], INV_DT) for i in range(2)]
    QTs = [sc.sb(f"p2_QT{i}", [128, 8, 128], INV_DT) for i in range(2)]
    Ys = [sc.sb(f"p2_Y{i}", [128, 8, 128], INV_DT) for i in range(2)]
    R_sb = sc.sb("p2_R", [128, 512], INV_DT)
    negU = sc.sb("p2_negU", [128, 512], BF16)
    Hf = sc.sb("p2_Hf", [64, 512]); Hb = sc.sb("p2_Hb", [64, 512], BF16); Htmp = sc.sb("p2_Htmp", [64, 512])
    S.memset(S.dve, Hf[:], 0.0, W=[Hf]); S.memset(S.dve, Hb[:], 0.0, W=[Hb])
    yt, yc, sq2, rkt, sz, bon = f32t("yt"), f32t("yc"), f32t("sq2"), f32t("rkt"), f32t("sz"), f32t("bon")
    ygo = [sc.sb(f"p2_ygo{i}", [128, 512]) for i in range(2)]
    ident_i = G.ident_f if INV_DT == F32 else G.ident_bf
    evn = [0]
    def ev_eng():
        evn[0] += 1
        return S.act if evn[0] % 2 else S.dve
    for c in range(nch):
        ua = rr(uas, st, "ua"); up = rr(ups, st, "up"); za = rr(zas, st, "za")
        rows = slice(c * 128, (c + 1) * 128)
        S.dma(S.q_sync, ua[:], pa[rows, 0:1664], R=[pa], W=[ua])
        S.dma(S.q_sync, za[:], pa[rows, 1664:2176], R=[pa], W=[za])
        if c == 0:
            S.memset(S.pool, up[0:1, :], 0.0, W=[up])
            S.dma(S.q_sync, up[1:128, :], pa[0:127, 0:1664], R=[pa], W=[up])
        else:
            S.dma(S.q_sync, up[:], pa[c * 128 - 1:c * 128 + 127, 0:1664], R=[pa], W=[up])
        S.tt(S.pool, up[:], up[:], ua[:], ALU.subtract, R=[up, ua], W=[up])
        S.tt(S.dve, up[:], up[:], mu_b, ALU.mult, R=[up, rvt], W=[up])
        S.tt(S.pool, up[:], up[:], ua[:], ALU.add, R=[up, ua], W=[up])
        um = up
        r_, k_, v_ = um[:, 0:512], um[:, 512:1024], um[:, 1024:1536]
        pl_ = rr(G.ps, st, "ps")
        S.tr([(pl_[:, 0:128], um[:, 1536:1664])], G.ident_f[:], R=[um, G.ident_f], W=[pl_])
        S.actf(lw[0:64, :], pl_[0:64, 0:128], AF.Tanh, R=[pl_], W=[lw])
        S.cp(S.dve, lw[64:128, :], pl_[64:128, 0:128], R=[pl_], W=[lw])
        pzw = rr(G.ps, st, "ps"); pza = rr(G.ps, st, "ps")
        S.mm(pzw[:], [(lw[0:64, :], w2a2[0:64, :]), (G.ones_f[0:1, :], w0a0[0:1, 0:512])], R=[lw, w2a2, w0a0, G.ones_f], W=[pzw])
        S.mm(pza[:], [(lw[64:128, :], w2a2[64:128, :]), (G.ones_f[0:1, :], w0a0[0:1, 512:1024])], R=[lw, w2a2, w0a0, G.ones_f], W=[pza])
        S.actf(sgw[:], pzw[:], AF.Sigmoid, R=[pzw], W=[sgw])
        S.actf(av[:], pza[:], AF.Sigmoid, R=[pza], W=[av])
        pci = rr(G.ps, st, "ps"); pce = rr(G.ps, st, "ps"); pgc = rr(G.ps, st, "ps")
        S.mm(pci[:], [(M_incl, sgw[:])], R=[G.masks, sgw], W=[pci])
        S.mm(pce[:], [(M_strict, sgw[:])], R=[G.masks, sgw], W=[pce])
        S.group(S.pe, [(lambda h=h: nc.tensor.matmul(pgc[0:64, h:h + 1], lhsT=sgw[:, h * 64:(h + 1) * 64], rhs=G.ones_f[:, 0:1],
                                                      start=True, stop=True)) for h in range(8)], [sgw, G.ones_f], [pgc])
        S.actf(gam[:], pci[:], AF.Exp, R=[pci], W=[gam], scale=-CDEC)
        S.actf(ig[:], pci[:], AF.Exp, R=[pci], W=[ig], scale=CDEC)
        S.actf(gae[:], pce[:], AF.Exp, R=[pce], W=[gae], scale=-CDEC)
        S.actf(gC[:], pgc[0:64, 0:8], AF.Exp, R=[pgc], W=[gC], scale=-CDEC)
        S.tt(S.dve, kk[:], k_, kk_b, ALU.mult, R=[um, rvt], W=[kk])
        S.tt(S.pool, sqt[:], kk[:], kk[:], ALU.mult, R=[kk], W=[sqt])
        S.op(S.dve, lambda: nc.vector.tensor_reduce(out=ssq[:, 0:8], in_=h3(sqt[:]), axis=AX.X, op=ALU.add), [sqt], [ssq])
        S.actf(ssq[:, 8:16], ssq[:, 0:8], AF.Sqrt, R=[ssq], W=[ssq])
        S.ts(S.dve, ssq[:, 8:16], ssq[:, 8:16], 1e-12, op0=ALU.max, R=[ssq], W=[ssq])
        S.op(S.dve, lambda: nc.vector.reciprocal(out=ssq[:, 16:24], in_=ssq[:, 8:16]), [ssq], [ssq])
        S.tt(S.dve, h3(kkn[:]), h3(kk[:]), bc_last(ssq[:, 16:24], 64), ALU.mult, R=[kk, ssq], W=[kkn])
        S.stt(S.dve, e1[:], av[:], -1.0, ka_b, ALU.add, ALU.mult, R=[av, rvt], W=[e1])
        S.stt(S.dve, knew[:], e1[:], 1.0, k_, ALU.add, ALU.mult, R=[e1, um], W=[knew])
        S.tt(S.pool, kap_b[:], kkn[:], gae[:], ALU.mult, R=[kkn, gae], W=[kap_b])
        S.tt(S.dve, ktl_b[:], knew[:], ig[:], ALU.mult, R=[knew, ig], W=[ktl_b])
        S.tt(S.pool, e1[:], kkn[:], av[:], ALU.mult, R=[kkn, av], W=[e1])
        S.tt(S.pool, btl_b[:], e1[:], ig[:], ALU.mult, R=[e1, ig], W=[btl_b])
        S.tt(S.dve, rtl_b[:], r_, gam[:], ALU.mult, R=[um, gam], W=[rtl_b])
        S.cp(S.pool, v_b[:], v_, R=[um], W=[v_b])
        for (src, dst, wi) in ((kap_b, KR, 0), (rtl_b, KR, 1), (ktl_b, KB, 0), (btl_b, KB, 1)):
            pb = rr(G.psb, st, "psb")
            S.tr([(pb[0:64, h * 128:(h + 1) * 128], src[:, h * 64:(h + 1) * 64]) for h in range(8)], G.ident_bf[:],
                 R=[src, G.ident_bf], W=[pb])
            S.cp(ev_eng(), dst[:, :, wi, :], pb[0:64, :].rearrange("p (h t) -> p h t", h=8), R=[pb], W=[dst])
        Q, QT, Y = rr(Qs, st, "Q"), rr(QTs, st, "QT"), rr(Ys, st, "Y")
        for hp in range(4):
            p1 = rr(G.ps, st, "ps"); p2 = rr(G.ps, st, "ps")
            for hh in range(2):
                h = hp * 2 + hh
                S.mm(p1[:, hh * 256:(hh + 1) * 256], [(KB[:, h, 0, :], KR[:, h, :, :])], R=[KB, KR], W=[p1])
                S.mm(p2[:, hh * 256:(hh + 1) * 256], [(KB[:, h, 1, :], KR[:, h, :, :])], R=[KB, KR], W=[p2])
            hs = slice(hp * 2, hp * 2 + 2)
            p1v = p1[:].rearrange("p (h w t) -> p h w t", h=2, w=2)
            p2v = p2[:].rearrange("p (h w t) -> p h w t", h=2, w=2)
            for hh in range(2):
                h = hp * 2 + hh
                S.tt(S.dve, AK[:, h, :, :], p1v[:, hh, :, :], G.masks[:, 0:2, :], ALU.mult, R=[p1, G.masks], W=[AK])
            S.tt(S.dve, AB2[:, hs, :], p2v[:, :, 1, :], bc_mid(M_incl, 2), ALU.mult, R=[p2, G.masks], W=[AB2])
            S.stt(S.dve, QT[:, hs, :], p2v[:, :, 0, :], -1.0, bc_mid(M_strict, 2), ALU.mult, ALU.mult, R=[p2, G.masks], W=[QT])
            S.tt(S.pool, Y[:, hs, :], QT[:, hs, :], bc_mid(ident_i[:], 2), ALU.add, R=[QT, ident_i], W=[Y])
        for hq in range(2):
            p3 = rr(G.ps, st, "ps")
            for hh in range(4):
                h = hq * 4 + hh
                S.mm(p3[:, hh * 128:(hh + 1) * 128], [(KR[:, h, 0, :], KB[:, h, 1, :])], R=[KR, KB], W=[p3])
            hs = slice(hq * 4, hq * 4 + 4)
            S.stt(S.dve, Q[:, hs, :], p3[:].rearrange("p (h t) -> p h t", h=4), -1.0, bc_mid(M_lower, 4), ALU.mult, ALU.mult,
                  R=[p3, G.masks], W=[Q])
        for lev in range(1, 7):
            Qn, QTn, Yn = rr(Qs, st, "Q"), rr(QTs, st, "QT"), rr(Ys, st, "Y")
            last = lev == 6
            for hq in range(2):
                hs = slice(hq * 4, hq * 4 + 4)
                pq = rr(G.ps, st, "ps")
                for hh in range(4):
                    h = hq * 4 + hh
                    S.mm(pq[:, hh * 128:(hh + 1) * 128], [(QT[:, h, :], Q[:, h, :])], R=[QT, Q], W=[pq])
                S.cp(ev_eng(), Qn[:, hs, :], pq[:].rearrange("p (h t) -> p h t", h=4), R=[pq], W=[Qn])
                if not last:
                    pqt = rr(G.ps, st, "ps")
                    for hh in range(4):
                        h = hq * 4 + hh
                        S.mm(pqt[:, hh * 128:(hh + 1) * 128], [(Q[:, h, :], QT[:, h, :])], R=[QT, Q], W=[pqt])
                    S.cp(ev_eng(), QTn[:, hs, :], pqt[:].rearrange("p (h t) -> p h t", h=4), R=[pqt], W=[QTn])
            for hq in range(2):
                hs = slice(hq * 4, hq * 4 + 4)
                py = rr(G.ps, st, "ps")
                for hh in range(4):
                    h = hq * 4 + hh
                    S.mm(py[:, hh * 128:(hh + 1) * 128], [(Qn[:, h, :], Y[:, h, :])], R=[Qn, Y], W=[py])
                S.tt(S.dve, Yn[:, hs, :], py[:].rearrange("p (h t) -> p h t", h=4), Y[:, hs, :], ALU.add, R=[py, Y], W=[Yn])
            Q, QT, Y = Qn, QTn, Yn
        pR = rr(G.ps, st, "ps")
        for h in range(8):
            cs_ = slice(h * 64, (h + 1) * 64)
            S.mm(pR[:, cs_], [(KR[:, h, 0, :], Hb[:, cs_]), (AK[:, h, 0, :], v_b[:, cs_])], R=[KR, Hb, AK, v_b], W=[pR])
        S.cp(S.act, R_sb[:], pR[:], R=[pR], W=[R_sb])
        pU = rr(G.ps, st, "ps")
        for h in range(8):
            cs_ = slice(h * 64, (h + 1) * 64)
            S.mm(pU[:, cs_], [(Y[:, h, :], R_sb[:, cs_])], R=[Y, R_sb], W=[pU])
        S.ts(S.dve, negU[:], pU[:], -1.0, R=[pU], W=[negU])
        pY = rr(G.ps, st, "ps")
        for h in range(8):
            cs_ = slice(h * 64, (h + 1) * 64)
            S.mm(pY[:, cs_], [(KR[:, h, 1, :], Hb[:, cs_]), (AK[:, h, 1, :], v_b[:, cs_]), (AB2[:, h, :], negU[:, cs_])],
                 R=[KR, Hb, AK, v_b, AB2, negU], W=[pY])
        S.cp(S.act, yt[:], pY[:], R=[pY], W=[yt])
        pH = rr(G.ps, st, "ps")
        for h in range(8):
            cs_ = slice(h * 64, (h + 1) * 64)
            S.mm(pH[0:64, cs_], [(ktl_b[:, cs_], v_b[:, cs_]), (btl_b[:, cs_], negU[:, cs_])], R=[ktl_b, v_b, btl_b, negU], W=[pH])
        S.tt(S.dve, Htmp[:], pH[0:64, :], Hf[:], ALU.add, R=[pH, Hf], W=[Htmp])
        S.tt(S.dve, h3(Hf[:]), h3(Htmp[:]), bc_last(gC[:], 64), ALU.mult, R=[Htmp, gC], W=[Hf])
        S.cp(S.pool, Hb[:], Hf[:], R=[Hf], W=[Hb])
        S.op(S.dve, lambda: nc.vector.tensor_reduce(out=ssq[:, 24:32], in_=h3(yt[:]), axis=AX.X, op=ALU.add), [yt], [ssq])
        S.ts(S.dve, ssq[:, 24:32], ssq[:, 24:32], -1.0 / 64, R=[ssq], W=[ssq])
        S.tt(S.pool, h3(yc[:]), h3(yt[:]), bc_last(ssq[:, 24:32], 64), ALU.add, R=[yt, ssq], W=[yc])
        S.tt(S.pool, sq2[:], yc[:], yc[:], ALU.mult, R=[yc], W=[sq2])
        S.op(S.dve, lambda: nc.vector.tensor_reduce(out=ssq[:, 0:8], in_=h3(sq2[:]), axis=AX.X, op=ALU.add), [sq2], [ssq])
        S.actf(ssq[:, 8:16], ssq[:, 0:8], AF.Sqrt, R=[ssq], W=[ssq], scale=1.0 / 64, bias=G.eps_col[:, 1:2])
        S.op(S.dve, lambda: nc.vector.reciprocal(out=ssq[:, 16:24], in_=ssq[:, 8:16]), [ssq], [ssq])
        S.tt(S.dve, h3(yc[:]), h3(yc[:]), bc_last(ssq[:, 16:24], 64), ALU.mult, R=[yc, ssq], W=[yc])
        S.tt(S.pool, yc[:], yc[:], lg_b, ALU.mult, R=[yc, rvt], W=[yc])
        S.tt(S.pool, yc[:], yc[:], lb_b, ALU.add, R=[yc, rvt], W=[yc])
        S.tt(S.pool, rkt[:], r_, knew[:], ALU.mult, R=[um, knew], W=[rkt])
        S.tt(S.pool, rkt[:], rkt[:], rk_b, ALU.mult, R=[rkt, rvt], W=[rkt])
        S.op(S.dve, lambda: nc.vector.tensor_reduce(out=ssq[:, 24:32], in_=h3(rkt[:]), axis=AX.X, op=ALU.add), [rkt], [ssq])
        S.tt(S.dve, h3(bon[:]), h3(v_), bc_last(ssq[:, 24:32], 64), ALU.mult, R=[um, ssq], W=[bon])
        S.tt(S.pool, yc[:], yc[:], bon[:], ALU.add, R=[yc, bon], W=[yc])
        S.actf(sz[:], za[:], AF.Silu, R=[za], W=[sz])
        yo = rr(ygo, st, "ygo")
        S.tt(S.dve, yo[:], yc[:], sz[:], ALU.mult, R=[yc, sz], W=[yo])
        S.dma(S.q_pool, yga[rows, :], yo[:], R=[yo], W=[yga])
    sc.close()


def phase_out(S, C, G, l, hsrc, hdst, pbT, yga, ybT, ycT, T, final):
    nc = S.nc
    sc = Scope(S)
    st = {}
    nblk = T // 512
    cvt = sc.sb("p5_cv", [128, NCV])
    S.dma(S.q_sync, cvt[:], C.cv[l], W=[cvt])
    wo = sc.sb("p5_wo", [128, 12, 1024], BF16)
    sc2 = Scope(S)
    stg = [sc2.sb(f"p5_stg{i}", [128, 1024]) for i in range(2)]
    for k in range(12):
        sg = stg[k % 2]
        S.dma(S.q_sync if k % 2 == 0 else S.q_pool, sg[:], C.w_out[l][k * 128:(k + 1) * 128, :], W=[sg])
        S.cp(S.dve if k % 2 == 0 else S.pool, wo[:, k, :], sg[:], R=[sg], W=[wo])
    sc2.close()
    if final:
        fg = sc.sb("p5_fg", [128, D])
        S.dma(S.q_sync, fg[:], C.final_g[0:1, :].partition_broadcast(128), W=[fg])
        junk = sc.sb("p5_junk", [128, D], BF16)
        ssf = [sc.sb(f"p5_ssf{i}", [128, 4]) for i in range(2)]
    ygT = [sc.sb(f"p5_ygT{i}", [128, 12, 512], BF16) for i in range(2)]
    ybs = [sc.sb(f"p5_yb{i}", [128, 512]) for i in range(5)]
    zbs = [sc.sb(f"p5_zb{i}", [128, 512]) for i in range(3)]
    sqs = [sc.sb(f"p5_sq{i}", [128, 512]) for i in range(2)]
    rstd = [sc.sb(f"p5_rstd{i}", [128, 512]) for i in range(2)]
    t1s = [sc.sb(f"p5_t1{i}", [128, 512]) for i in range(2)]
    t2s = [sc.sb(f"p5_t2{i}", [128, 512]) for i in range(2)]
    yas = [sc.sb(f"p5_ya{i}", [128, 512]) for i in range(2)]
    yabs = [sc.sb(f"p5_yab{i}", [128, 512], BF16) for i in range(2)]
    hts = [sc.sb(f"p5_h{i}", [128, D]) for i in range(2)]
    hos = [sc.sb(f"p5_ho{i}", [128, D]) for i in range(2)]
    def fin(j):
        blk = slice(j * 512, (j + 1) * 512)
        yg = ygT[j % 2]
        for (src, zoff, goff, kbase) in ((ybT, OB_ZB, CV_LOG, 4), (ycT, OB_ZC, CV_MOG, 8)):
            ys = []
            pS = rr(G.ps, st, "ps")
            for ct in range(4):
                yb = rr(ybs, st, "yb"); ys.append(yb)
                S.dma(S.q_sync, yb[:], src[ct * 128:(ct + 1) * 128, blk], R=[src], W=[yb])
                sq = rr(sqs, st, "sq")
                S.actf(sq[:], yb[:], AF.Square, R=[yb], W=[sq])
                S.mm(pS[:], [(G.ones_f[:], sq[:])], R=[G.ones_f, sq], W=[pS], start=(ct == 0), stop=(ct == 3))
            rs = rr(rstd, st, "rstd")
            S.actf(rs[:], pS[:], AF.Sqrt, R=[pS], W=[rs], scale=1.0 / 512, bias=G.eps_col[:, 0:1])
            S.op(S.dve, lambda rs=rs: nc.vector.reciprocal(out=rs[:], in_=rs[:]), [rs], [rs])
            for ct in range(4):
                zb = rr(zbs, st, "zb")
                S.dma(S.q_pool, zb[:], pbT[zoff + ct * 128:zoff + (ct + 1) * 128, blk], R=[pbT], W=[zb])
                t1 = rr(t1s, st, "t1"); t2 = rr(t2s, st, "t2")
                S.actf(t1[:], zb[:], AF.Silu, R=[zb], W=[t1])
                S.stt(S.dve, t2[:], ys[ct][:], cvt[:, goff + ct:goff + ct + 1], rs[:], ALU.mult, ALU.mult, R=[ys[ct], cvt, rs], W=[t2])
                S.tt(S.pool, yg[:, kbase + ct, :], t1[:], t2[:], ALU.mult, R=[t1, t2], W=[yg])
        for tt in range(4):
            t = 4 * j + tt
            ya = rr(yas, st, "ya"); yab = rr(yabs, st, "yab")
            S.dma(S.q_sync, ya[:], yga[t * 128:(t + 1) * 128, :], R=[yga], W=[ya])
            S.cp(S.pool, yab[:], ya[:], R=[ya], W=[yab])
            pb = rr(G.psb, st, "psb")
            S.tr([(pb[:, ct * 128:(ct + 1) * 128], yab[:, ct * 128:(ct + 1) * 128]) for ct in range(4)], G.ident_bf[:],
                 R=[yab, G.ident_bf], W=[pb])
            S.cp(S.act, yg[:, 0:4, tt * 128:(tt + 1) * 128], pb[:, 0:512].rearrange("p (c t) -> p c t", c=4), R=[pb], W=[yg])

    def outproj(j):
        yg = ygT[j % 2]
        for tt in range(4):
            t = 4 * j + tt
            ht = rr(hts, st, "h"); ho = rr(hos, st, "ho")
            S.dma(S.q_sync, ht[:], hsrc[t * 128:(t + 1) * 128, :], R=[hsrc], W=[ht])
            for half in range(2):
                cs_ = slice(half * 512, (half + 1) * 512)
                pO = rr(G.ps, st, "ps")
                S.mm(pO[:], [(yg[:, k, tt * 128:(tt + 1) * 128], wo[:, k, cs_]) for k in range(12)], R=[yg, wo], W=[pO])
                S.tt(S.dve, ho[:, cs_], pO[:], ht[:, cs_], ALU.add, R=[pO, ht], W=[ho])
            if final:
                s_ = rr(ssf, st, "ssf")
                S.actf(junk[:], ho[:], AF.Square, R=[ho], W=[junk, s_], accum=s_[:, 0:1])
                S.actf(s_[:, 1:2], s_[:, 0:1], AF.Sqrt, R=[s_], W=[s_], scale=1.0 / D, bias=G.eps_col[:, 0:1])
                S.op(S.dve, lambda a=s_: nc.vector.reciprocal(out=a[:, 2:3], in_=a[:, 1:2]), [s_], [s_])
                S.stt(S.dve, ht[:], ho[:], s_[:, 2:3], fg[:], ALU.mult, ALU.mult, R=[ho, s_, fg], W=[ht])
                S.dma(S.q_pool, hdst[t * 128:(t + 1) * 128, :], ht[:], R=[ht], W=[hdst])
            else:
                S.dma(S.q_pool, hdst[t * 128:(t + 1) * 128, :], ho[:], R=[ho], W=[hdst])

    fin(0)
    for j in range(nblk):
        if j + 1 < nblk:
            fin(j + 1)
        outproj(j)
    sc.close()


def build(T, L, phases=None, debug=False):
    nc = bass.Bass("TRN2", target_bir_lowering=False)
    S = Sched(nc)
    C = declare_inputs(S, T, L)
    G = Ctx()
    load_consts(S, C, G)
    G.eps_col = S.sb("eps_col", [128, 2])
    S.memset(S.dve, G.eps_col[:, 0:1], EPS, W=[G.eps_col])
    S.memset(S.dve, G.eps_col[:, 1:2], GN_EPS, W=[G.eps_col])
    full = phases is None
    if full:
        phases = ("p1", "p2", "p3", "p4", "p5")
    dbg = lambda name: "ExternalOutput" if (debug and name in phases) else "Internal"
    pa = S.dram("pa", [T, NA], F32, kind=dbg("p1"))
    pbT = S.dram("pbT", [NB, T], F32, kind=dbg("p1"))
    ybT = S.dram("ybT", [512, T], F32, kind=dbg("p3"))
    ycT = S.dram("ycT", [512, T], F32, kind=dbg("p4"))
    yga = S.dram("yga", [T, 512], F32, kind=dbg("p2"))
    hb = [S.dram(f"hbuf{i}", [T, D], F32) for i in range(2)]
    hout = S.dram("hout", [T, D], F32, kind="ExternalOutput" if (full or "p5" in phases) else "Internal")
    outs = []
    nl = L if full else 1
    for l in range(nl):
        hsrc = C.x if l == 0 else hb[(l - 1) % 2]
        last = l == nl - 1
        hdst = hout if last else hb[l % 2]
        phase_in_proj(S, C, G, l, hsrc, pa, pbT, T)
        if "p2" in phases: phase_rwkv(S, C, G, l, pa, yga, T)
        if "p3" in phases: phase_lru(S, C, G, l, pbT, ybT, T)
        if "p4" in phases: phase_mla(S, C, G, l, pbT, ycT, T)
        if "p5" in phases: phase_out(S, C, G, l, hsrc, hdst, pbT, yga, ybT, ycT, T, final=(full and last))
    if debug:
        for nm, b in (("p1", pa), ("p1", pbT), ("p3", ybT), ("p4", ycT), ("p2", yga)):
            if nm in phases: outs.append(b)
    if full or "p5" in phases: outs.append(hout)
    S.finish(outs)
    emit_all(S)
    return nc, S

from concourse.bass_utils import run_bass_kernel_spmd

T_FULL = 4096
L_FULL = 4
_IN_NAMES = ["x", "wA", "wB", "rv", "w0a0", "w2a2", "cv", "gw", "wq", "wqs", "wk", "wv", "w_out", "final_g",
             "ident_bf", "ident_f", "cs", "masks", "mask_bf"]
_NC_CACHE = {}


def kernel(**inputs):
    x = np.asarray(inputs["x"], np.float32)
    B, T, _ = x.shape
    L = np.asarray(inputs["w_in"]).shape[0]
    hp = host_prep(inputs, T)
    key = (T, L)
    if key not in _NC_CACHE:
        _NC_CACHE[key] = build(T, L)[0]
    nc = _NC_CACHE[key]
    in_maps = []
    for b in range(B):
        m = {k: hp[k] for k in _IN_NAMES if k != "x"}
        m["x"] = np.ascontiguousarray(x[b])
        in_maps.append(m)
    res = run_bass_kernel_spmd(nc, in_maps, core_ids=list(range(B)))
    return np.stack([np.asarray(r["hout"], np.float32) for r in res.results], axis=0)
```

```python
import numpy as np
import concourse.bass as bass
import concourse.mybir as mybir

F32 = mybir.dt.float32
BF16 = mybir.dt.bfloat16
ALU = mybir.AluOpType
AF = mybir.ActivationFunctionType
AX = mybir.AxisListType


class Sem:
    def __init__(self, nc, name):
        self.h = nc.alloc_semaphore(name) if hasattr(nc, "alloc_semaphore") else None
        self.name = name
        self.count = 0


class Buf:
    __slots__ = ("t", "name", "last_write", "reads")

    def __init__(self, t, name):
        self.t = t
        self.name = name
        self.last_write = None
        self.reads = {}

    def __getitem__(self, idx):
        return self.t[idx]


SEM_LIMIT = 8000


class Eng:
    def cur_sem(self, i=0):
        sem = self.sems[i]
        if sem.count >= SEM_LIMIT:
            self.nrot = getattr(self, "nrot", 0) + 1
            sem = self.S.new_sem(f"{self.name}_r{self.nrot}")
            self.sems[i] = sem
        return sem

    def __init__(self, S, name, eng, is_dma=False, nsem=1):
        self.S = S
        self.name = name
        self.e = eng
        self.is_dma = is_dma
        self.sems = [S.new_sem(f"{name}_s{i}") for i in range(nsem)]
        self.rr = 0
        self.known = {}
        self.prog = []

    def wait(self, tok):
        sem, val = tok
        if self.known.get(id(sem), 0) >= val:
            return
        e = self.e; h = sem.h
        self.prog.append(lambda: e.wait_ge(h, val))
        self.known[id(sem)] = val
        self.S.nwaits += 1


class Sched:
    def __init__(self, nc):
        self.nc = nc
        import contextlib
        self.es = contextlib.ExitStack()
        self.nwaits = 0
        self.ninst = 0
        self.sem_list = []
        self.pe = Eng(self, "pe", nc.tensor)
        self.dve = Eng(self, "dve", nc.vector)
        self.act = Eng(self, "act", nc.scalar)
        self.pool = Eng(self, "pool", nc.gpsimd)
        self.q_sync = Eng(self, "qsync", nc.sync, is_dma=True, nsem=8)
        self.q_pool = Eng(self, "qpool", nc.gpsimd, is_dma=True, nsem=4)
        self.q_pool.prog = self.pool.prog
        self.engines = [self.pe, self.dve, self.act, self.pool, self.q_sync]

    def new_sem(self, name):
        s = Sem.__new__(Sem)
        s.is_pe = name.startswith("pe_")
        s.name = name
        s.count = 0
        s.h = self.es.enter_context(self.nc.semaphore(name))
        self.sem_list.append(s)
        return s

    def sb(self, name, shape, dtype=F32):
        t = self.nc.alloc_sbuf_tensor(name, list(shape), dtype)
        return Buf(t, name)

    def ps(self, name, shape, dtype=F32):
        t = self.nc.alloc_psum_tensor(name, list(shape), dtype)
        return Buf(t, name)

    def dram(self, name, shape, dtype=F32, kind="Internal"):
        t = self.nc.dram_tensor(name, list(shape), dtype, kind=kind)
        return Buf(t, name)

    def _deps(self, reads, writes):
        deps = []
        for b in reads:
            if b.last_write is not None:
                deps.append(b.last_write)
        for b in writes:
            if b.last_write is not None:
                deps.append(b.last_write)
            deps.extend(b.reads.values())
        return deps

    def _commit(self, tok, reads, writes):
        for b in reads:
            if b not in writes:
                b.reads[id(tok[0])] = tok
        for b in writes:
            b.last_write = tok
            b.reads = {}

    def op(self, eng, fn, reads=(), writes=()):
        for tok in self._deps(reads, writes):
            if eng is self.pe and tok[0].is_pe:
                continue
            eng.wait(tok)
        sem = eng.cur_sem()
        sem.count += 1
        h = sem.h
        eng.prog.append(lambda: fn().then_inc(h, 1))
        tok = (sem, sem.count)
        self._commit(tok, reads, writes)
        self.ninst += 1
        return tok

    def group(self, eng, fns, reads=(), writes=()):
        for tok in self._deps(reads, writes):
            if eng is self.pe and tok[0].is_pe:
                continue
            eng.wait(tok)
        fns = list(fns)
        for fn in fns[:-1]:
            eng.prog.append(fn)
            self.ninst += 1
        self.ninst += 1
        sem = eng.cur_sem()
        sem.count += 1
        h = sem.h
        last = fns[-1]
        eng.prog.append(lambda: last().then_inc(h, 1))
        tok = (sem, sem.count)
        self._commit(tok, reads, writes)
        return tok

    def dma(self, q, out_ap, in_ap, R=(), W=(), **kw):
        i = q.rr % len(q.sems)
        sem = q.sems[i]
        q.rr += 1
        if sem.count > 0:
            q.wait((sem, sem.count))
        sem = q.cur_sem(i)
        for tok in self._deps(R, W):
            q.wait(tok)
        sem.count += 16
        h = sem.h; e = q.e
        q.prog.append(lambda: e.dma_start(out=out_ap, in_=in_ap, **kw).then_inc(h, 16))
        tok = (sem, sem.count)
        self._commit(tok, R, W)
        self.ninst += 1
        return tok

    def ts(self, eng, out, in0, s1, s2=None, op0=ALU.mult, op1=None, R=(), W=(), accum=None):
        e = eng.e
        kw = {}
        if op1 is not None: kw["op1"] = op1
        if accum is not None: kw["accum_out"] = accum
        return self.op(eng, lambda: e.tensor_scalar(out=out, in0=in0, scalar1=s1, scalar2=s2, op0=op0, **kw), R, W)

    def tt(self, eng, out, in0, in1, op, R=(), W=()):
        e = eng.e
        return self.op(eng, lambda: e.tensor_tensor(out=out, in0=in0, in1=in1, op=op), R, W)

    def stt(self, eng, out, in0, scalar, in1, op0, op1, R=(), W=()):
        e = eng.e
        return self.op(eng, lambda: e.scalar_tensor_tensor(out=out, in0=in0, scalar=scalar, in1=in1, op0=op0, op1=op1), R, W)

    def cp(self, eng, out, in_, R=(), W=()):
        e = eng.e
        if eng is self.act:
            return self.op(eng, lambda: e.copy(out=out, in_=in_), R, W)
        return self.op(eng, lambda: e.tensor_copy(out=out, in_=in_), R, W)

    def actf(self, out, in_, func, R=(), W=(), scale=1.0, bias=None, accum=None):
        e = self.act.e
        kw = {}
        if bias is not None: kw["bias"] = bias
        if accum is not None: kw["accum_out"] = accum
        return self.op(self.act, lambda: e.activation(out=out, in_=in_, func=func, scale=scale, **kw), R, W)

    def mm(self, out, pairs, R=(), W=(), start=True, stop=True, **kw):
        e = self.pe.e
        n = len(pairs)
        fns = []
        for i, (l, r) in enumerate(pairs):
            st = start and i == 0
            sp = stop and i == n - 1
            fns.append((lambda l=l, r=r, st=st, sp=sp: e.matmul(out, lhsT=l, rhs=r, start=st, stop=sp, **kw)))
        return self.group(self.pe, fns, R, W)

    def tr(self, outs_ins, ident, R=(), W=()):
        e = self.pe.e
        fns = [(lambda o=o, i=i: e.transpose(out=o, in_=i, identity=ident)) for (o, i) in outs_ins]
        return self.group(self.pe, fns, R, W)

    def memset(self, eng, ap, val, W=()):
        e = eng.e
        return self.op(eng, lambda: e.memset(ap, val), (), W)

    def barrier(self):
        toks = [(s, s.count) for s in self.sem_list if s.count > 0]
        for eng in [self.pe, self.dve, self.act, self.pool, self.q_sync]:
            for tok in toks:
                eng.wait(tok)

    def finish(self, bufs):
        for b in bufs:
            if b.last_write is not None:
                self.q_sync.wait(b.last_write)


def emit_all(S):
    nc = S.nc
    def run(prog):
        def f(e):
            for g in prog: g()
        return f
    with nc.Block() as block:
        if S.q_sync.prog: block.sync(run(S.q_sync.prog))
        if S.pe.prog: block.tensor(run(S.pe.prog))
        if S.dve.prog: block.vector(run(S.dve.prog))
        if S.act.prog: block.scalar(run(S.act.prog))
        if S.pool.prog: block.gpsimd(run(S.pool.prog))
    S.es.close()


def _prune(reads):
    best = {}
    for sem, val in reads:
        k = id(sem)
        if k not in best or best[k][1] < val:
            best[k] = (sem, val)
    return list(best.values())

D = 1024
GW = 512
NH = 8
HD = 64
RW_IN = 1664
NA = 2176
NB = 1984
QL = 256
KVL = 128
DMIX = 1536

OB_UB, OB_ZB, OB_QL, OB_KV, OB_ZC, OB_KR, OB_KRS = 0, 512, 1024, 1280, 1408, 1920, 1952

RV_MU, RV_KK, RV_KA, RV_RK, RV_LG, RV_LB = 0, 1664, 2176, 2688, 3200, 3712
NRV = 4224
CV_LNG = 0
CV_CW = 8
CV_CB = 24
CV_GAB = 28
CV_GXB = 32
CV_LAM = 36
CV_LOG = 40
CV_QNG = 44
CV_KVNG = 46
CV_MOG = 47
NCV = 52


def host_prep(inp, T):
    f = np.float32
    L = inp["w_in"].shape[0]
    out = {}
    w_in = np.asarray(inp["w_in"], f)
    out["wA"] = np.ascontiguousarray(w_in[:, :, 0:NA])
    c = 2176
    colsB = np.concatenate([
        np.arange(c, c + 512), np.arange(c + 512, c + 1024), np.arange(3200, 3456), np.arange(3456, 3584),
        np.arange(3616, 4128), np.arange(3584, 3616), np.arange(3600, 3616), np.arange(3584, 3600)])
    assert colsB.size == NB
    out["wB"] = np.ascontiguousarray(w_in[:, :, colsB])
    rv = np.zeros((L, NRV), f)
    rv[:, RV_MU:RV_MU + 1664] = inp["rwkv_mu"]
    rv[:, RV_KK:RV_KK + 512] = inp["rwkv_k_k"]
    rv[:, RV_KA:RV_KA + 512] = inp["rwkv_k_a"]
    rv[:, RV_RK:RV_RK + 512] = np.asarray(inp["rwkv_r_k"], f).reshape(L, 512)
    rv[:, RV_LG:RV_LG + 512] = inp["rwkv_lnx_g"]
    rv[:, RV_LB:RV_LB + 512] = inp["rwkv_lnx_b"]
    out["rv"] = rv
    out["w0a0"] = np.ascontiguousarray(np.concatenate([np.asarray(inp["rwkv_w0"], f), np.asarray(inp["rwkv_a0"], f)], axis=1))
    out["w2a2"] = np.ascontiguousarray(np.concatenate([np.asarray(inp["rwkv_w2"], f), np.asarray(inp["rwkv_a2"], f)], axis=1))
    cv = np.zeros((L, 128, NCV), f)
    def colfill(off, vec, n):
        cv[:, :, off:off + n] = np.asarray(vec, f).reshape(L, n, 128).transpose(0, 2, 1)
    colfill(CV_LNG, inp["ln_g"], 8)
    cw = np.asarray(inp["lru_conv_w"], f)
    for j in range(4):
        colfill(CV_CW + j * 4, cw[:, j], 4)
    colfill(CV_CB, inp["lru_conv_b"], 4)
    colfill(CV_GAB, inp["lru_ga_b"], 4)
    colfill(CV_GXB, inp["lru_gx_b"], 4)
    colfill(CV_LAM, inp["lru_lam"], 4)
    colfill(CV_LOG, inp["lru_out_g"], 4)
    colfill(CV_QNG, inp["mla_q_norm_g"], 2)
    colfill(CV_KVNG, inp["mla_kv_norm_g"], 1)
    colfill(CV_MOG, inp["mla_out_g"], 4)
    out["cv"] = cv
    gw = np.zeros((L, 4, 128, 2, 128), f)
    for gi, nm in enumerate(["lru_ga_w", "lru_gx_w"]):
        w = np.asarray(inp[nm], f)
        for h in range(8):
            ct, o = h // 2, (h % 2) * 64
            gw[:, ct, o:o + 64, gi, o:o + 64] = w[:, h]
    out["gw"] = gw
    wuq = np.asarray(inp["mla_w_uq"], f).reshape(L, 256, 8, 96)
    wq = np.zeros((L, 256, 8, 128), f)
    wq[..., 0:32] = wuq[..., 64:96]
    wq[..., 64:128] = wuq[..., 0:64]
    out["wq"] = np.ascontiguousarray(wq.reshape(L, 256, 1024))
    wqs = np.zeros((L, 256, 8, 128), f)
    wqs[..., 0:16] = wuq[..., 80:96]
    wqs[..., 16:32] = wuq[..., 64:80]
    out["wqs"] = np.ascontiguousarray(wqs.reshape(L, 256, 1024))
    wukv = np.asarray(inp["mla_w_ukv"], f).reshape(L, 128, 8, 128)
    wk = np.zeros((L, 128, 8, 128), f)
    wk[..., 64:128] = wukv[..., 0:64]
    out["wk"] = np.ascontiguousarray(wk.reshape(L, 128, 1024))
    out["wv"] = np.ascontiguousarray(wukv[..., 64:128].reshape(L, 128, 512))
    out["w_out"] = np.asarray(inp["w_out"], f)
    out["final_g"] = np.asarray(inp["final_g"], f).reshape(1, 1024)
    import ml_dtypes
    bf = ml_dtypes.bfloat16
    out["ident_bf"] = np.eye(128, dtype=f).astype(bf)
    out["ident_f"] = np.eye(128, dtype=f)
    half = 16
    inv_freq = (10000.0 ** (-np.arange(half, dtype=f) * 2.0 / 32)).astype(f)
    ang = np.arange(T, dtype=f)[:, None] * inv_freq[None, :]
    cos, sin = np.cos(ang).astype(f).T, np.sin(ang).astype(f).T
    cs = np.zeros((128, 2, T), f)
    cs[32:128, 0] = 1.0
    cs[0:16, 0], cs[16:32, 0] = cos, cos
    cs[0:16, 1], cs[16:32, 1] = -sin, sin
    out["cs"] = cs
    ii = np.arange(128)
    m = np.zeros((128, 4, 128), f)
    m[:, 0] = (ii[:, None] < ii[None, :])
    m[:, 1] = (ii[:, None] <= ii[None, :])
    m[:, 2] = (ii[:, None] > ii[None, :])
    m[:, 3] = 1.0
    out["masks"] = m
    out["mask_bf"] = (ii[:, None] <= ii[None, :]).astype(f).astype(bf)
    return out

import contextlib

EPS = 1e-6


class Ctx:
    pass


def declare_inputs(S, T, L):
    C = Ctx()
    di = lambda n, shp, dt=F32: S.dram(n, shp, dt, kind="ExternalInput")
    C.x = di("x", [T, D])
    C.wA = di("wA", [L, D, NA]); C.wB = di("wB", [L, D, NB])
    C.rv = di("rv", [L, NRV]); C.w0a0 = di("w0a0", [L, 1024]); C.w2a2 = di("w2a2", [L, 128, 512])
    C.cv = di("cv", [L, 128, NCV]); C.gw = di("gw", [L, 4, 128, 2, 128])
    C.wq = di("wq", [L, 256, 1024]); C.wqs = di("wqs", [L, 256, 1024])
    C.wk = di("wk", [L, 128, 1024]); C.wv = di("wv", [L, 128, 512])
    C.w_out = di("w_out", [L, DMIX, D]); C.final_g = di("final_g", [1, D])
    C.ident_bf = di("ident_bf", [128, 128], BF16); C.ident_f = di("ident_f", [128, 128])
    C.cs = di("cs", [128, 2, T]); C.masks = di("masks", [128, 4, 128]); C.mask_bf = di("mask_bf", [128, 128], BF16)
    return C


class Scope:
    def __init__(self, S):
        self.S = S
        self.es = contextlib.ExitStack()

    _n = [0]

    def sb(self, name, shape, dtype=F32):
        Scope._n[0] += 1
        name = f"{name}_u{Scope._n[0]}"
        t = self.es.enter_context(self.S.nc.sbuf_tensor(name, list(shape), dtype))
        return Buf(t, name)

    def close(self):
        self.S.barrier()
        self.es.close()


def load_consts(S, C, G):
    nc = S.nc
    G.ident_bf = S.sb("ident_bf_sb", [128, 128], BF16)
    G.ident_f = S.sb("ident_f_sb", [128, 128])
    G.masks = S.sb("masks_sb", [128, 4, 128])
    G.mask_bf = S.sb("mask_bf_sb", [128, 128], BF16)
    G.ones_f = S.sb("ones_f", [128, 128])
    S.dma(S.q_sync, G.ident_bf[:], C.ident_bf[:], W=[G.ident_bf])
    S.dma(S.q_sync, G.ident_f[:], C.ident_f[:], W=[G.ident_f])
    S.dma(S.q_sync, G.masks[:], C.masks[:], W=[G.masks])
    S.dma(S.q_sync, G.mask_bf[:], C.mask_bf[:], W=[G.mask_bf])
    S.memset(S.dve, G.ones_f[:], 1.0, W=[G.ones_f])
    G.ps = [S.ps(f"ps{i}", [128, 512], F32) for i in range(6)]
    G.psb = [S.ps(f"psb{i}", [128, 1024], BF16) for i in range(2)]


def rr(lst, state, key):
    i = state.get(key, 0)
    state[key] = i + 1
    return lst[i % len(lst)]


def phase_in_proj(S, C, G, l, hsrc, pa, pbT, T):
    nc = S.nc
    sc = Scope(S)
    st = {}
    wA = sc.sb("wA_sb", [128, 8, NA], BF16)
    wB = sc.sb("wB_sb", [128, 8, NB], BF16)
    cvt = sc.sb("p1_cv", [128, NCV])
    S.dma(S.q_sync, cvt[:], C.cv[l], W=[cvt])
    stg = [sc.sb(f"p1_stg{i}", [128, NA]) for i in range(4)]
    wAv = C.wA[l].rearrange("(k p) c -> p k c", p=128)
    wBv = C.wB[l].rearrange("(k p) c -> p k c", p=128)
    n = 0
    for k in range(8):
        for (src, dst, nc_) in ((wAv, wA, NA), (wBv, wB, NB)):
            sg = stg[n % 4]
            S.dma(S.q_sync if n % 2 == 0 else S.q_pool, sg[:, 0:nc_], src[:, k, :], W=[sg])
            if n % 2 == 0:
                S.ts(S.dve, dst[:, k, :], sg[:, 0:nc_], cvt[:, CV_LNG + k:CV_LNG + k + 1], R=[sg, cvt], W=[dst])
            else:
                S.actf(dst[:, k, :], sg[:, 0:nc_], AF.Copy, R=[sg, cvt], W=[dst], scale=cvt[:, CV_LNG + k:CV_LNG + k + 1])
            n += 1
    hts = [sc.sb(f"p1_h{i}", [128, D]) for i in range(3)]
    junk = sc.sb("p1_junk", [128, D], BF16)
    xnb = [sc.sb(f"p1_xnb{i}", [128, D], BF16) for i in range(2)]
    ss = [sc.sb(f"p1_ss{i}", [128, 4]) for i in range(2)]
    xnT = [sc.sb(f"p1_xnT{i}", [128, 8, 512], BF16) for i in range(2)]
    oA = [sc.sb(f"p1_oA{i}", [128, NA]) for i in range(2)]
    oB = [sc.sb(f"p1_oB{i}", [128, 512]) for i in range(3)]
    nblk = T // 512
    ntile = T // 128
    evc = [0]
    def ev_eng():
        evc[0] += 1
        return S.act if evc[0] % 2 else S.dve

    def stageX(t):
        j, tt = t // 4, t % 4
        xT = xnT[j % 2]
        ht = rr(hts, st, "h")
        S.dma(S.q_sync, ht[:], hsrc[t * 128:(t + 1) * 128, :], R=[hsrc], W=[ht])
        s_ = rr(ss, st, "ss")
        S.actf(junk[:], ht[:], AF.Square, R=[ht], W=[junk, s_], accum=s_[:, 0:1])
        S.actf(s_[:, 1:2], s_[:, 0:1], AF.Sqrt, R=[s_], W=[s_], scale=1.0 / D, bias=G.eps_col[:, 0:1])
        S.op(S.dve, lambda a=s_: nc.vector.reciprocal(out=a[:, 2:3], in_=a[:, 1:2]), [s_], [s_])
        xb = rr(xnb, st, "xnb")
        S.ts(S.dve, xb[:], ht[:], s_[:, 2:3], R=[ht, s_], W=[xb])
        pb = rr(G.psb, st, "psb")
        S.tr([(pb[:, k * 128:(k + 1) * 128], xb[:, k * 128:(k + 1) * 128]) for k in range(8)], G.ident_bf[:],
             R=[xb, G.ident_bf], W=[pb])
        S.cp(S.act, xT[:, :, tt * 128:(tt + 1) * 128], pb[:].rearrange("p (k c) -> p k c", k=8), R=[pb], W=[xT])

    def stageM(t):
        j, tt = t // 4, t % 4
        xT = xnT[j % 2]
        o = rr(oA, st, "oA")
        c0 = 0
        while c0 < NA:
            cn = min(512, NA - c0)
            p = rr(G.ps, st, "ps")
            S.mm(p[:, 0:cn], [(xT[:, k, tt * 128:(tt + 1) * 128], wA[:, k, c0:c0 + cn]) for k in range(8)],
                 R=[xT, wA], W=[p])
            S.cp(ev_eng(), o[:, c0:c0 + cn], p[:, 0:cn], R=[p], W=[o])
            c0 += cn
        S.dma(S.q_pool, pa[t * 128:(t + 1) * 128, :], o[:], R=[o], W=[pa])

    def stageF(j):
        xT = xnT[j % 2]
        r0 = 0
        while r0 < NB:
            rn = min(128, NB - r0)
            p = rr(G.ps, st, "ps")
            S.mm(p[0:rn, :], [(wB[:, k, r0:r0 + rn], xT[:, k, :]) for k in range(8)], R=[xT, wB], W=[p])
            o = rr(oB, st, "oB")
            S.cp(ev_eng(), o[0:rn, :], p[0:rn, :], R=[p], W=[o])
            S.dma(S.q_pool, pbT[r0:r0 + rn, j * 512:(j + 1) * 512], o[0:rn, :], R=[o], W=[pbT])
            r0 += rn

    stageX(0)
    for t in range(ntile):
        if t + 1 < ntile:
            stageX(t + 1)
        stageM(t)
        if t % 4 == 3:
            stageF(t // 4)
    sc.close()


def phase_lru(S, C, G, l, pbT, ybT, T):
    nc = S.nc
    sc = Scope(S)
    st = {}
    cvt = sc.sb("p3_cv", [128, NCV])
    S.dma(S.q_sync, cvt[:], C.cv[l], W=[cvt])
    gwt = sc.sb("p3_gw", [128, 4, 2, 128])
    S.dma(S.q_sync, gwt[:], C.gw[l].rearrange("c p g q -> p c g q"), W=[gwt])
    c8 = sc.sb("p3_c8", [128, 12])
    S.actf(c8[:, 0:4], cvt[:, CV_LAM:CV_LAM + 4], AF.Exp, R=[cvt], W=[c8], scale=-1.0)
    S.actf(c8[:, 4:8], c8[:, 0:4], AF.Ln, R=[c8], W=[c8], bias=1.0)
    S.ts(S.dve, c8[:, 8:12], c8[:, 4:8], -8.0, R=[c8], W=[c8])
    ubs = [sc.sb(f"p3_ub{i}", [128, 515]) for i in range(3)]
    xcs = [sc.sb(f"p3_xc{i}", [128, 512]) for i in range(2)]
    grs = [sc.sb(f"p3_gr{i}", [128, 512]) for i in range(2)]
    gis = [sc.sb(f"p3_gi{i}", [128, 512]) for i in range(2)]
    a_s = [sc.sb(f"p3_a{i}", [128, 512]) for i in range(2)]
    oms = [sc.sb(f"p3_om{i}", [128, 512]) for i in range(2)]
    bxs = [sc.sb(f"p3_bx{i}", [128, 512]) for i in range(2)]
    hos = [sc.sb(f"p3_ho{i}", [128, 512]) for i in range(3)]
    zero = sc.sb("p3_zero", [128, 1])
    S.memset(S.dve, zero[:], 0.0, W=[zero])
    nblk = T // 512
    for ct in range(4):
        r0 = OB_UB + ct * 128
        cw = lambda j: cvt[:, CV_CW + j * 4 + ct:CV_CW + j * 4 + ct + 1]
        col = lambda off: cvt[:, off + ct:off + ct + 1]
        hprev, hprev_buf = zero[:, 0:1], zero
        for j in range(nblk):
            ub = rr(ubs, st, "ub")
            if j == 0:
                S.memset(S.pool, ub[:, 0:3], 0.0, W=[ub])
                S.dma(S.q_sync, ub[:, 3:515], pbT[r0:r0 + 128, 0:512], R=[pbT], W=[ub])
            else:
                S.dma(S.q_sync, ub[:], pbT[r0:r0 + 128, j * 512 - 3:(j + 1) * 512], R=[pbT], W=[ub])
            xc = rr(xcs, st, "xc")
            S.ts(S.dve, xc[:], ub[:, 0:512], cw(0), col(CV_CB), op0=ALU.mult, op1=ALU.add, R=[ub, cvt], W=[xc])
            for jj in range(1, 4):
                S.stt(S.dve, xc[:], ub[:, jj:jj + 512], cw(jj), xc[:], ALU.mult, ALU.add, R=[ub, cvt, xc], W=[xc])
            p1 = rr(G.ps, st, "ps"); p2 = rr(G.ps, st, "ps")
            S.mm(p1[:], [(gwt[:, ct, 0, :], xc[:])], R=[gwt, xc], W=[p1])
            S.mm(p2[:], [(gwt[:, ct, 1, :], xc[:])], R=[gwt, xc], W=[p2])
            gr = rr(grs, st, "gr"); gi = rr(gis, st, "gi")
            S.actf(gr[:], p1[:], AF.Sigmoid, R=[p1, cvt], W=[gr], bias=col(CV_GAB))
            S.actf(gi[:], p2[:], AF.Sigmoid, R=[p2, cvt], W=[gi], bias=col(CV_GXB))
            a = rr(a_s, st, "a"); om = rr(oms, st, "om"); bx = rr(bxs, st, "bx")
            S.actf(a[:], gr[:], AF.Exp, R=[gr, c8], W=[a], scale=c8[:, 8 + ct:9 + ct])
            S.tt(S.dve, bx[:], gi[:], xc[:], ALU.mult, R=[gi, xc], W=[bx])
            S.actf(om[:], a[:], AF.Square, R=[a], W=[om])
            S.actf(om[:], om[:], AF.Sqrt, R=[om], W=[om], scale=-1.0, bias=1.0)
            S.tt(S.dve, bx[:], bx[:], om[:], ALU.mult, R=[bx, om], W=[bx])
            ho = rr(hos, st, "ho")
            S.op(S.dve, lambda ho=ho, a=a, bx=bx, hp=hprev: nc.vector.tensor_tensor_scan(
                out=ho[:], data0=a[:], data1=bx[:], initial=hp, op0=ALU.mult, op1=ALU.add),
                [a, bx, hprev_buf], [ho])
            S.dma(S.q_pool, ybT[ct * 128:(ct + 1) * 128, j * 512:(j + 1) * 512], ho[:], R=[ho], W=[ybT])
            hprev, hprev_buf = ho[:, 511:512], ho
    sc.close()


def phase_mla(S, C, G, l, pbT, ycT, T):
    nc = S.nc
    sc = Scope(S)
    st = {}
    nblk = T // 512
    cvt = sc.sb("p4_cv", [128, NCV])
    S.dma(S.q_sync, cvt[:], C.cv[l], W=[cvt])
    wq = sc.sb("p4_wq", [128, 2, 1024], BF16)
    wqs = sc.sb("p4_wqs", [128, 2, 1024], BF16)
    wk = sc.sb("p4_wk", [128, 1024], BF16)
    wv = sc.sb("p4_wv", [128, 512], BF16)
    sc2 = Scope(S)
    stg = [sc2.sb(f"p4_stg{i}", [128, 1024]) for i in range(2)]
    jobs = [(C.wq[l][0:128, :], wq[:, 0, :], 1024, wq), (C.wq[l][128:256, :], wq[:, 1, :], 1024, wq),
            (C.wqs[l][0:128, :], wqs[:, 0, :], 1024, wqs), (C.wqs[l][128:256, :], wqs[:, 1, :], 1024, wqs),
            (C.wk[l], wk[:], 1024, wk), (C.wv[l], wv[:], 512, wv)]
    for n, (src, dst, ncol, dbuf) in enumerate(jobs):
        sg = stg[n % 2]
        S.dma(S.q_sync, sg[:, 0:ncol], src, W=[sg])
        S.cp(S.dve if n % 2 == 0 else S.act, dst, sg[:, 0:ncol], R=[sg], W=[dbuf])
    sc2.close()
    E65 = sc.sb("p4_E96", [128, 64])
    S.memset(S.dve, E65[:], 0.0, W=[E65])
    S.memset(S.dve, E65[64:65, :], 1.0, W=[E65])
    KT = [sc.sb(f"p4_KT{j}", [128, 8, 512], BF16) for j in range(nblk)]
    VP = [sc.sb(f"p4_VP{j}", [128, 4, 8, 96], BF16) for j in range(nblk)]
    for j in range(nblk):
        S.memset(S.pool, KT[j][32:64, :, :], 0.0, W=[KT[j]])
        S.memset(S.pool, VP[j][:, :, :, 64:96], 0.0, W=[VP[j]])
        S.memset(S.pool, VP[j][:, :, :, 64:65], 1.0, W=[VP[j]])
    QT = [[sc.sb(f"p4_QT{b}_{h}", [128, 512], BF16) for h in range(8)] for b in range(1)]
    cst = [sc.sb(f"p4_cs{i}", [128, 2, 512]) for i in range(1)]
    qls = [sc.sb(f"p4_ql{i}", [128, 2, 512]) for i in range(1)]
    kvls = [sc.sb(f"p4_kvl{i}", [128, 512]) for i in range(2)]
    krs = [sc.sb(f"p4_kr{i}", [32, 2, 512]) for i in range(2)]
    sq = sc.sb("p4_sq", [128, 2, 512])
    rq = sc.sb("p4_rq", [128, 512]); rk = sc.sb("p4_rk", [128, 512])
    qn = sc.sb("p4_qn", [128, 2, 512], BF16)
    ckv = sc.sb("p4_ckv", [128, 512], BF16)
    t1s = [sc.sb(f"p4_t1{i}", [128, 512]) for i in range(2)]
    t2s = [sc.sb(f"p4_t2{i}", [128, 512]) for i in range(2)]
    pTs = [sc.sb(f"p4_pT{i}", [128, 512], BF16) for i in range(3)]
    osbs = [sc.sb(f"p4_osb{i}", [96, 512]) for i in range(2)]
    rls = [sc.sb(f"p4_rl{i}", [64, 512]) for i in range(2)]
    ohs = [sc.sb(f"p4_oh{i}", [64, 512]) for i in range(2)]
    psS = G.ps[0:3]; psO = G.ps[3:5]; psL = G.ps[5]
    scale = 96.0 ** -0.5
    for j in range(nblk):
        blk = slice(j * 512, (j + 1) * 512)
        ql = rr(qls, st, "ql"); kvl = rr(kvls, st, "kvl"); kr = rr(krs, st, "kr"); cs = rr(cst, st, "cs")
        for k in range(2):
            S.dma(S.q_sync, ql[:, k, :], pbT[OB_QL + k * 128:OB_QL + (k + 1) * 128, blk], R=[pbT], W=[ql])
        S.dma(S.q_sync, kvl[:], pbT[OB_KV:OB_KV + 128, blk], R=[pbT], W=[kvl])
        S.dma(S.q_pool, kr[:, 0, :], pbT[OB_KR:OB_KR + 32, blk], R=[pbT], W=[kr])
        S.dma(S.q_pool, kr[:, 1, :], pbT[OB_KRS:OB_KRS + 32, blk], R=[pbT], W=[kr])
        S.dma(S.q_pool, cs[:], C.cs[:, :, blk], W=[cs])
        S.actf(sq[:], ql[:], AF.Square, R=[ql], W=[sq])
        pA = rr(psS, st, "psS")
        S.mm(pA[:], [(G.ones_f[:], sq[:, 0, :]), (G.ones_f[:], sq[:, 1, :])], R=[G.ones_f, sq], W=[pA])
        S.actf(rq[:], pA[:], AF.Sqrt, R=[pA], W=[rq], scale=1.0 / QL, bias=G.eps_col[:, 0:1])
        S.op(S.dve, lambda: nc.vector.reciprocal(out=rq[:], in_=rq[:]), [rq], [rq])
        for k in range(2):
            S.stt(S.dve, qn[:, k, :], ql[:, k, :], cvt[:, CV_QNG + k:CV_QNG + k + 1], rq[:], ALU.mult, ALU.mult,
                  R=[ql, cvt, rq], W=[qn])
        S.actf(sq[:, 0, :], kvl[:], AF.Square, R=[kvl], W=[sq])
        pB = rr(psS, st, "psS")
        S.mm(pB[:], [(G.ones_f[:], sq[:, 0, :])], R=[G.ones_f, sq], W=[pB])
        S.actf(rk[:], pB[:], AF.Sqrt, R=[pB], W=[rk], scale=1.0 / KVL, bias=G.eps_col[:, 0:1])
        S.op(S.dve, lambda: nc.vector.reciprocal(out=rk[:], in_=rk[:]), [rk], [rk])
        S.stt(S.dve, ckv[:], kvl[:], cvt[:, CV_KVNG:CV_KVNG + 1], rk[:], ALU.mult, ALU.mult, R=[kvl, cvt, rk], W=[ckv])
        t1 = rr(t1s, st, "t1"); t2 = rr(t2s, st, "t2")
        S.tt(S.dve, t1[0:32, :], kr[:, 0, :], cs[0:32, 0, :], ALU.mult, R=[kr, cs], W=[t1])
        S.tt(S.dve, t2[0:32, :], kr[:, 1, :], cs[0:32, 1, :], ALU.mult, R=[kr, cs], W=[t2])
        for h in range(8):
            S.tt(S.dve, KT[j][0:32, h, :], t1[0:32, :], t2[0:32, :], ALU.add, R=[t1, t2], W=[KT[j]])
        for h in range(8):
            Qh = QT[0][h]
            pq = rr(psS, st, "psS")
            S.mm(pq[:], [(wq[:, k, h * 128:(h + 1) * 128], qn[:, k, :]) for k in range(2)], R=[wq, qn], W=[pq])
            pqs = rr(psS, st, "psS")
            S.mm(pqs[:], [(wqs[:, k, h * 128:(h + 1) * 128], qn[:, k, :]) for k in range(2)], R=[wqs, qn], W=[pqs])
            t1 = rr(t1s, st, "t1"); t2 = rr(t2s, st, "t2")
            S.tt(S.dve, t1[:], pq[:], cs[:, 0, :], ALU.mult, R=[pq, cs], W=[t1])
            S.tt(S.dve, t2[:], pqs[:], cs[:, 1, :], ALU.mult, R=[pqs, cs], W=[t2])
            S.tt(S.dve, Qh[:], t1[:], t2[:], ALU.add, R=[t1, t2], W=[Qh])
            pk = rr(psS, st, "psS")
            S.mm(pk[:], [(wk[:, h * 128:(h + 1) * 128], ckv[:])], R=[wk, ckv], W=[pk])
            S.cp(S.act, KT[j][64:128, h, :], pk[64:128, :], R=[pk], W=[KT[j]])
        for tt in range(4):
            pv = rr(psS, st, "psS")
            S.mm(pv[:], [(ckv[:, tt * 128:(tt + 1) * 128], wv[:])], R=[ckv, wv], W=[pv])
            S.cp(S.dve, VP[j][:, tt, :, 0:64], pv[:].rearrange("p (h d) -> p h d", h=8),
                 R=[pv], W=[VP[j]])
        nkt = 4 * j + 4
        for h in range(8):
            Qh = QT[0][h]
            po = rr(psO, st, "psO")
            def issue_S(kt):
                i = kt - 4 * j
                c0 = 128 * max(i, 0)
                N = 512 - c0
                kb, ko = kt // 4, (kt % 4) * 128
                pS = rr(psS, st, "psS")
                S.mm(pS[:, 0:N], [(KT[kb][:, h, ko:ko + 128], Qh[:, c0:512])], R=[KT[kb], Qh], W=[pS])
                return pS
            pS_next = issue_S(0)
            for kt in range(nkt):
                i = kt - 4 * j
                c0 = 128 * max(i, 0)
                N = 512 - c0
                kb = kt // 4
                pS = pS_next
                if kt + 1 < nkt:
                    pS_next = issue_S(kt + 1)
                pT = rr(pTs, st, "pT")
                S.actf(pT[:, 0:N], pS[:, 0:N], AF.Exp, R=[pS], W=[pT], scale=scale)
                if i >= 0:
                    S.tt(S.pool, pT[:, 0:128], pT[:, 0:128], G.mask_bf[:], ALU.mult, R=[pT, G.mask_bf], W=[pT])
                S.mm(po[0:96, c0:512], [(VP[kb][:, kt % 4, h, :], pT[:, 0:N])], R=[VP[kb], pT], W=[po],
                     start=(kt == 0), stop=(kt == nkt - 1))
            osb = rr(osbs, st, "osb")
            S.cp(S.act, osb[:], po[0:96, :], R=[po], W=[osb])
            S.mm(psL[0:64, :], [(E65[0:96, :], osb[0:96, :])], R=[E65, osb], W=[psL])
            rl = rr(rls, st, "rl")
            S.op(S.dve, lambda rl=rl: nc.vector.reciprocal(out=rl[:], in_=psL[0:64, :]), [psL], [rl])
            oh = rr(ohs, st, "oh")
            S.tt(S.dve, oh[:], osb[0:64, :], rl[:], ALU.mult, R=[osb, rl], W=[oh])
            S.dma(S.q_sync, ycT[h * 64:(h + 1) * 64, blk], oh[:], R=[oh], W=[ycT])
    sc.close()


INV_DT = BF16
CDEC = 0.6065306597126334
GN_EPS = 64e-5


def bc_mid(ap, n):
    return ap.unsqueeze(1).broadcast_to([ap.shape[0], n, ap.shape[1]])


def bc_last(ap, n):
    return ap.unsqueeze(2).broadcast_to([ap.shape[0], ap.shape[1], n])


def h3(ap):
    return ap.rearrange("p (h d) -> p h d", h=8)


def phase_rwkv(S, C, G, l, pa, yga, T, dbg=None):
    nc = S.nc
    sc = Scope(S)
    st = {}
    nch = T // 128
    rvt = sc.sb("p2_rv", [128, NRV])
    S.dma(S.q_sync, rvt[:], C.rv[l:l + 1, :].partition_broadcast(128), W=[rvt])
    w0a0 = sc.sb("p2_w0a0", [1, 1024])
    S.dma(S.q_sync, w0a0[:], C.w0a0[l:l + 1, :], W=[w0a0])
    w2a2 = sc.sb("p2_w2a2", [128, 512])
    S.dma(S.q_sync, w2a2[:], C.w2a2[l], W=[w2a2])
    mu_b = rvt[:, RV_MU:RV_MU + 1664]
    kk_b = rvt[:, RV_KK:RV_KK + 512]; ka_b = rvt[:, RV_KA:RV_KA + 512]; rk_b = rvt[:, RV_RK:RV_RK + 512]
    lg_b = rvt[:, RV_LG:RV_LG + 512]; lb_b = rvt[:, RV_LB:RV_LB + 512]
    M_strict, M_incl, M_lower = G.masks[:, 0, :], G.masks[:, 1, :], G.masks[:, 2, :]
    ident_i = G.ident_f if INV_DT == F32 else G.ident_bf
    f32t = lambda n: sc.sb(f"p2_{n}", [128, 512])
    b16t = lambda n: sc.sb(f"p2_{n}", [128, 512], BF16)
    uas = [sc.sb(f"p2_ua{i}", [128, 1664]) for i in range(2)]
    ups = [sc.sb(f"p2_up{i}", [128, 1664]) for i in range(2)]
    zas = [f32t(f"za{i}") for i in range(2)]
    Tm = Ctx()
    Tm.lw = sc.sb("p2_lw", [128, 128])
    Tm.sgw, Tm.av, Tm.gam, Tm.ig, Tm.gae = f32t("sgw"), f32t("av"), f32t("gam"), f32t("ig"), f32t("gae")
    Tm.kk, Tm.sqt, Tm.kkn, Tm.e1, Tm.knew, Tm.rkt = f32t("kk"), f32t("sqt"), f32t("kkn"), f32t("e1"), f32t("knew"), f32t("rkt")
    Tm.ssq = sc.sb("p2_ssq", [128, 32])

    def outset(i):
        O = Ctx()
        O.kap_b, O.ktl_b, O.btl_b, O.rtl_b, O.v_b = [b16t(f"{n}{i}") for n in ("kapb", "ktlb", "btlb", "rtlb", "vb")]
        O.gC = sc.sb(f"p2_gC{i}", [64, 8])
        O.bon, O.sz = f32t(f"bon{i}"), f32t(f"sz{i}")
        return O

    def a2set(i):
        Z = Ctx()
        Z.KR = sc.sb(f"p2_KR{i}", [64, 8, 2, 128], BF16)
        Z.KB = sc.sb(f"p2_KB{i}", [64, 8, 2, 128], BF16)
        Z.AK = sc.sb(f"p2_AK{i}", [128, 8, 2, 128], BF16)
        Z.AB2 = sc.sb(f"p2_AB2{i}", [128, 8, 128], BF16)
        Z.Qs = [sc.sb(f"p2_Q{i}_{k}", [128, 8, 128], INV_DT) for k in range(2)]
        Z.QTs = [sc.sb(f"p2_QT{i}_{k}", [128, 8, 128], INV_DT) for k in range(2)]
        Z.Ys = [sc.sb(f"p2_Y{i}_{k}", [128, 8, 128], INV_DT) for k in range(2)]
        return Z

    OS = [outset(i) for i in range(3)]
    ZS = [a2set(i) for i in range(2)]
    Bt = Ctx()
    Bt.R_sb = sc.sb("p2_R", [128, 512], INV_DT); Bt.negU = b16t("negU")
    Bt.yt, Bt.yc, Bt.sq2 = f32t("yt"), f32t("yc"), f32t("sq2")
    Bt.ygo = [f32t(f"ygo{i}") for i in range(2)]
    Bt.ssb = sc.sb("p2_ssb", [128, 32])
    Hf = sc.sb("p2_Hf", [64, 512]); Hb = sc.sb("p2_Hb", [64, 512], BF16); Htmp = sc.sb("p2_Htmp", [64, 512])
    S.memset(S.dve, Hf[:], 0.0, W=[Hf]); S.memset(S.dve, Hb[:], 0.0, W=[Hb])

    def stageA1(c):
        X = Tm; O = OS[c % 3]
        ua, up, za = uas[c % 2], ups[c % 2], zas[c % 2]
        rows = slice(c * 128, (c + 1) * 128)
        S.dma(S.q_sync, ua[:], pa[rows, 0:1664], R=[pa], W=[ua])
        S.dma(S.q_sync, za[:], pa[rows, 1664:2176], R=[pa], W=[za])
        if c == 0:
            S.memset(S.pool, up[0:1, :], 0.0, W=[up])
            S.dma(S.q_sync, up[1:128, :], pa[0:127, 0:1664], R=[pa], W=[up])
        else:
            S.dma(S.q_sync, up[:], pa[c * 128 - 1:c * 128 + 127, 0:1664], R=[pa], W=[up])
        yield
        S.tt(S.dve, up[:], up[:], ua[:], ALU.subtract, R=[up, ua], W=[up])
        yield
        S.tt(S.dve, up[:], up[:], mu_b, ALU.mult, R=[up, rvt], W=[up])
        yield
        S.tt(S.dve, up[:], up[:], ua[:], ALU.add, R=[up, ua], W=[up])
        yield
        um = up
        r_, k_, v_ = um[:, 0:512], um[:, 512:1024], um[:, 1024:1536]
        pl_ = rr(G.ps, st, "ps")
        S.tr([(pl_[:, 0:128], um[:, 1536:1664])], G.ident_f[:], R=[um, G.ident_f], W=[pl_])
        S.actf(X.lw[0:64, :], pl_[0:64, 0:128], AF.Tanh, R=[pl_], W=[X.lw])
        S.cp(S.act, X.lw[64:128, :], pl_[64:128, 0:128], R=[pl_], W=[X.lw])
        S.cp(S.act, O.v_b[:], v_, R=[um], W=[O.v_b])
        S.actf(O.sz[:], za[:], AF.Silu, R=[za], W=[O.sz])
        yield
        pzw = rr(G.ps, st, "ps"); pza = rr(G.ps, st, "ps")
        S.mm(pzw[:], [(X.lw[0:64, :], w2a2[0:64, :]), (G.ones_f[0:1, :], w0a0[0:1, 0:512])], R=[X.lw, w2a2, w0a0, G.ones_f], W=[pzw])
        S.mm(pza[:], [(X.lw[64:128, :], w2a2[64:128, :]), (G.ones_f[0:1, :], w0a0[0:1, 512:1024])], R=[X.lw, w2a2, w0a0, G.ones_f], W=[pza])
        S.actf(X.sgw[:], pzw[:], AF.Sigmoid, R=[pzw], W=[X.sgw])
        S.actf(X.av[:], pza[:], AF.Sigmoid, R=[pza], W=[X.av])
        yield
        pci = rr(G.ps, st, "ps"); pce = rr(G.ps, st, "ps"); pgc = rr(G.ps, st, "ps")
        S.mm(pci[:], [(M_incl, X.sgw[:])], R=[G.masks, X.sgw], W=[pci])
        S.mm(pce[:], [(M_strict, X.sgw[:])], R=[G.masks, X.sgw], W=[pce])
        sgw = X.sgw
        S.group(S.pe, [(lambda h=h: nc.tensor.matmul(pgc[0:64, h:h + 1], lhsT=sgw[:, h * 64:(h + 1) * 64], rhs=G.ones_f[:, 0:1],
                                                      start=True, stop=True)) for h in range(8)], [X.sgw, G.ones_f], [pgc])
        yield
        S.actf(X.gam[:], pci[:], AF.Exp, R=[pci], W=[X.gam], scale=-CDEC)
        S.actf(X.ig[:], pci[:], AF.Exp, R=[pci], W=[X.ig], scale=CDEC)
        S.actf(X.gae[:], pce[:], AF.Exp, R=[pce], W=[X.gae], scale=-CDEC)
        S.actf(O.gC[:], pgc[0:64, 0:8], AF.Exp, R=[pgc], W=[O.gC], scale=-CDEC)
        yield
        ssq = X.ssq
        S.tt(S.dve, X.kk[:], k_, kk_b, ALU.mult, R=[um, rvt], W=[X.kk])
        S.actf(X.sqt[:], X.kk[:], AF.Square, R=[X.kk], W=[X.sqt])
        yield
        S.op(S.dve, lambda: nc.vector.tensor_reduce(out=ssq[:, 0:8], in_=h3(X.sqt[:]), axis=AX.X, op=ALU.add), [X.sqt], [ssq])
        S.actf(ssq[:, 8:16], ssq[:, 0:8], AF.Sqrt, R=[ssq], W=[ssq])
        S.ts(S.dve, ssq[:, 8:16], ssq[:, 8:16], 1e-12, op0=ALU.max, R=[ssq], W=[ssq])
        S.op(S.dve, lambda: nc.vector.reciprocal(out=ssq[:, 16:24], in_=ssq[:, 8:16]), [ssq], [ssq])
        yield
        S.tt(S.dve, h3(X.kkn[:]), h3(X.kk[:]), bc_last(ssq[:, 16:24], 64), ALU.mult, R=[X.kk, ssq], W=[X.kkn])
        yield
        S.stt(S.dve, X.e1[:], X.av[:], -1.0, ka_b, ALU.add, ALU.mult, R=[X.av, rvt], W=[X.e1])
        yield
        S.stt(S.dve, X.knew[:], X.e1[:], 1.0, k_, ALU.add, ALU.mult, R=[X.e1, um], W=[X.knew])
        yield
        S.tt(S.dve, O.kap_b[:], X.kkn[:], X.gae[:], ALU.mult, R=[X.kkn, X.gae], W=[O.kap_b])
        yield
        S.tt(S.dve, O.ktl_b[:], X.knew[:], X.ig[:], ALU.mult, R=[X.knew, X.ig], W=[O.ktl_b])
        yield
        S.tt(S.dve, X.e1[:], X.kkn[:], X.av[:], ALU.mult, R=[X.kkn, X.av], W=[X.e1])
        yield
        S.tt(S.dve, O.btl_b[:], X.e1[:], X.ig[:], ALU.mult, R=[X.e1, X.ig], W=[O.btl_b])
        yield
        S.tt(S.dve, O.rtl_b[:], r_, X.gam[:], ALU.mult, R=[um, X.gam], W=[O.rtl_b])
        yield
        S.tt(S.dve, X.rkt[:], r_, X.knew[:], ALU.mult, R=[um, X.knew], W=[X.rkt])
        yield
        S.tt(S.dve, X.rkt[:], X.rkt[:], rk_b, ALU.mult, R=[X.rkt, rvt], W=[X.rkt])
        S.op(S.dve, lambda: nc.vector.tensor_reduce(out=ssq[:, 24:32], in_=h3(X.rkt[:]), axis=AX.X, op=ALU.add), [X.rkt], [ssq])
        yield
        S.tt(S.dve, h3(O.bon[:]), h3(v_), bc_last(ssq[:, 24:32], 64), ALU.mult, R=[um, ssq], W=[O.bon])
        yield

    def stageA2(c):
        O = OS[c % 3]; Z = ZS[c % 2]
        for (src, dst, wi) in ((O.kap_b, Z.KR, 0), (O.rtl_b, Z.KR, 1), (O.ktl_b, Z.KB, 0), (O.btl_b, Z.KB, 1)):
            pb = rr(G.psb, st, "psb")
            S.tr([(pb[0:64, h * 128:(h + 1) * 128], src[:, h * 64:(h + 1) * 64]) for h in range(8)], G.ident_bf[:],
                 R=[src, G.ident_bf], W=[pb])
            S.cp(S.act, dst[:, :, wi, :], pb[0:64, :].rearrange("p (h t) -> p h t", h=8), R=[pb], W=[dst])
            yield
        KR, KB, AK, AB2 = Z.KR, Z.KB, Z.AK, Z.AB2
        Q, QT, Y = Z.Qs[0], Z.QTs[0], Z.Ys[0]
        for hp in range(4):
            p1 = rr(G.ps, st, "ps"); p2 = rr(G.ps, st, "ps")
            for hh in range(2):
                h = hp * 2 + hh
                S.mm(p1[:, hh * 256:(hh + 1) * 256], [(KB[:, h, 0, :], KR[:, h, :, :])], R=[KB, KR], W=[p1])
                S.mm(p2[:, hh * 256:(hh + 1) * 256], [(KB[:, h, 1, :], KR[:, h, :, :])], R=[KB, KR], W=[p2])
            hs = slice(hp * 2, hp * 2 + 2)
            p1v = p1[:].rearrange("p (h w t) -> p h w t", h=2, w=2)
            p2v = p2[:].rearrange("p (h w t) -> p h w t", h=2, w=2)
            for hh in range(2):
                h = hp * 2 + hh
                S.tt(S.dve, AK[:, h, :, :], p1v[:, hh, :, :], G.masks[:, 0:2, :], ALU.mult, R=[p1, G.masks], W=[AK])
            yield
            S.tt(S.dve, AB2[:, hs, :], p2v[:, :, 1, :], bc_mid(M_incl, 2), ALU.mult, R=[p2, G.masks], W=[AB2])
            S.stt(S.dve, QT[:, hs, :], p2v[:, :, 0, :], -1.0, bc_mid(M_strict, 2), ALU.mult, ALU.mult, R=[p2, G.masks], W=[QT])
            S.tt(S.dve, Y[:, hs, :], QT[:, hs, :], bc_mid(ident_i[:], 2), ALU.add, R=[QT, ident_i], W=[Y])
            yield
        for hq in range(2):
            p3 = rr(G.ps, st, "ps")
            for hh in range(4):
                h = hq * 4 + hh
                S.mm(p3[:, hh * 128:(hh + 1) * 128], [(KR[:, h, 0, :], KB[:, h, 1, :])], R=[KR, KB], W=[p3])
            hs = slice(hq * 4, hq * 4 + 4)
            S.stt(S.dve, Q[:, hs, :], p3[:].rearrange("p (h t) -> p h t", h=4), -1.0, bc_mid(M_lower, 4), ALU.mult, ALU.mult,
                  R=[p3, G.masks], W=[Q])
            yield
        k_ = 0
        for lev in range(1, 7):
            k_ ^= 1
            Qn, QTn, Yn = Z.Qs[k_], Z.QTs[k_], Z.Ys[k_]
            last = lev == 6
            for hq in range(2):
                hs = slice(hq * 4, hq * 4 + 4)
                pq = rr(G.ps, st, "ps")
                for hh in range(4):
                    h = hq * 4 + hh
                    S.mm(pq[:, hh * 128:(hh + 1) * 128], [(QT[:, h, :], Q[:, h, :])], R=[QT, Q], W=[pq])
                S.cp(S.act, Qn[:, hs, :], pq[:].rearrange("p (h t) -> p h t", h=4), R=[pq], W=[Qn])
                if not last:
                    pqt = rr(G.ps, st, "ps")
                    for hh in range(4):
                        h = hq * 4 + hh
                        S.mm(pqt[:, hh * 128:(hh + 1) * 128], [(Q[:, h, :], QT[:, h, :])], R=[QT, Q], W=[pqt])
                    S.cp(S.act, QTn[:, hs, :], pqt[:].rearrange("p (h t) -> p h t", h=4), R=[pqt], W=[QTn])
                yield
            for hq in range(2):
                hs = slice(hq * 4, hq * 4 + 4)
                py = rr(G.ps, st, "ps")
                for hh in range(4):
                    h = hq * 4 + hh
                    S.mm(py[:, hh * 128:(hh + 1) * 128], [(Qn[:, h, :], Y[:, h, :])], R=[Qn, Y], W=[py])
                S.tt(S.dve, Yn[:, hs, :], py[:].rearrange("p (h t) -> p h t", h=4), Y[:, hs, :], ALU.add, R=[py, Y], W=[Yn])
                yield
            Q, QT, Y = Qn, QTn, Yn
        Z.Yfin = Y

    def stageB(c):
        O = OS[c % 3]; Z = ZS[c % 2]
        KR, AK, AB2, Y = Z.KR, Z.AK, Z.AB2, Z.Yfin
        v_b, negU, R_sb, ssb = O.v_b, Bt.negU, Bt.R_sb, Bt.ssb
        rows = slice(c * 128, (c + 1) * 128)
        pR = rr(G.ps, st, "ps")
        for h in range(8):
            cs_ = slice(h * 64, (h + 1) * 64)
            S.mm(pR[:, cs_], [(KR[:, h, 0, :], Hb[:, cs_]), (AK[:, h, 0, :], v_b[:, cs_])], R=[KR, Hb, AK, v_b], W=[pR])
        S.cp(S.act, R_sb[:], pR[:], R=[pR], W=[R_sb])
        yield
        pU = rr(G.ps, st, "ps")
        for h in range(8):
            cs_ = slice(h * 64, (h + 1) * 64)
            S.mm(pU[:, cs_], [(Y[:, h, :], R_sb[:, cs_])], R=[Y, R_sb], W=[pU])
        S.actf(negU[:], pU[:], AF.Copy, R=[pU], W=[negU], scale=-1.0)
        yield
        pH = rr(G.ps, st, "ps")
        for h in range(8):
            cs_ = slice(h * 64, (h + 1) * 64)
            S.mm(pH[0:64, cs_], [(O.ktl_b[:, cs_], v_b[:, cs_]), (O.btl_b[:, cs_], negU[:, cs_])], R=[O.ktl_b, v_b, O.btl_b, negU], W=[pH])
        pY = rr(G.ps, st, "ps")
        for h in range(8):
            cs_ = slice(h * 64, (h + 1) * 64)
            S.mm(pY[:, cs_], [(KR[:, h, 1, :], Hb[:, cs_]), (AK[:, h, 1, :], v_b[:, cs_]), (AB2[:, h, :], negU[:, cs_])],
                 R=[KR, Hb, AK, v_b, AB2, negU], W=[pY])
        S.cp(S.act, Bt.yt[:], pY[:], R=[pY], W=[Bt.yt])
        S.tt(S.dve, Htmp[:], pH[0:64, :], Hf[:], ALU.add, R=[pH, Hf], W=[Htmp])
        S.tt(S.dve, h3(Hf[:]), h3(Htmp[:]), bc_last(O.gC[:], 64), ALU.mult, R=[Htmp, O.gC], W=[Hf])
        S.cp(S.act, Hb[:], Hf[:], R=[Hf], W=[Hb])
        yield
        yt, yc, sq2 = Bt.yt, Bt.yc, Bt.sq2
        S.op(S.dve, lambda: nc.vector.tensor_reduce(out=ssb[:, 8:16], in_=h3(yt[:]), axis=AX.X, op=ALU.add), [yt], [ssb])
        S.ts(S.dve, ssb[:, 8:16], ssb[:, 8:16], -1.0 / 64, R=[ssb], W=[ssb])
        S.tt(S.dve, h3(yc[:]), h3(yt[:]), bc_last(ssb[:, 8:16], 64), ALU.add, R=[yt, ssb], W=[yc])
        S.actf(sq2[:], yc[:], AF.Square, R=[yc], W=[sq2])
        yield
        S.op(S.dve, lambda: nc.vector.tensor_reduce(out=ssb[:, 16:24], in_=h3(sq2[:]), axis=AX.X, op=ALU.add), [sq2], [ssb])
        S.actf(ssb[:, 24:32], ssb[:, 16:24], AF.Sqrt, R=[ssb], W=[ssb], scale=1.0 / 64, bias=G.eps_col[:, 1:2])
        S.op(S.dve, lambda: nc.vector.reciprocal(out=ssb[:, 16:24], in_=ssb[:, 24:32]), [ssb], [ssb])
        S.tt(S.dve, h3(yc[:]), h3(yc[:]), bc_last(ssb[:, 16:24], 64), ALU.mult, R=[yc, ssb], W=[yc])
        yield
        S.tt(S.dve, yc[:], yc[:], lg_b, ALU.mult, R=[yc, rvt], W=[yc])
        S.tt(S.dve, yc[:], yc[:], lb_b, ALU.add, R=[yc, rvt], W=[yc])
        yield
        S.tt(S.dve, yc[:], yc[:], O.bon[:], ALU.add, R=[yc, O.bon], W=[yc])
        ygo = Bt.ygo[c % 2]
        S.tt(S.dve, ygo[:], yc[:], O.sz[:], ALU.mult, R=[yc, O.sz], W=[ygo])
        S.dma(S.q_sync, yga[rows, :], ygo[:], R=[ygo], W=[yga])
        yield

    def drain(g):
        for _ in g:
            pass

    def step(g):
        if g is None:
            return None
        try:
            next(g)
            return g
        except StopIteration:
            return None

    drain(stageA1(0))
    if nch > 1:
        drain(stageA1(1))
    drain(stageA2(0))
    for c in range(nch):
        gB = stageB(c)
        gA2 = stageA2(c + 1) if c + 1 < nch else None
        gA1 = stageA1(c + 2) if c + 2 < nch else None
        while gB is not None or gA2 is not None or gA1 is not None:
            gA2 = step(gA2)
            gA1 = step(gA1)
            gA2 = step(gA2)
            gA1 = step(gA1)
            gA2 = step(gA2)
            gB = step(gB)
    sc.close()


def phase_out(S, C, G, l, hsrc, hdst, pbT, yga, ybT, ycT, T, final):
    nc = S.nc
    sc = Scope(S)
    st = {}
    nblk = T // 512
    cvt = sc.sb("p5_cv", [128, NCV])
    S.dma(S.q_sync, cvt[:], C.cv[l], W=[cvt])
    wo = sc.sb("p5_wo", [128, 12, 1024], BF16)
    sc2 = Scope(S)
    stg = [sc2.sb(f"p5_stg{i}", [128, 1024]) for i in range(2)]
    for k in range(12):
        sg = stg[k % 2]
        S.dma(S.q_sync if k % 2 == 0 else S.q_pool, sg[:], C.w_out[l][k * 128:(k + 1) * 128, :], W=[sg])
        S.cp(S.dve if k % 2 == 0 else S.act, wo[:, k, :], sg[:], R=[sg], W=[wo])
    sc2.close()
    if final:
        fg = sc.sb("p5_fg", [128, D])
        S.dma(S.q_sync, fg[:], C.final_g[0:1, :].partition_broadcast(128), W=[fg])
        junk = sc.sb("p5_junk", [128, D], BF16)
        ssf = [sc.sb(f"p5_ssf{i}", [128, 4]) for i in range(2)]
    ygT = [sc.sb(f"p5_ygT{i}", [128, 12, 512], BF16) for i in range(2)]
    ybs = [sc.sb(f"p5_yb{i}", [128, 512]) for i in range(5)]
    zbs = [sc.sb(f"p5_zb{i}", [128, 512]) for i in range(3)]
    sqs = [sc.sb(f"p5_sq{i}", [128, 512]) for i in range(2)]
    rstd = [sc.sb(f"p5_rstd{i}", [128, 512]) for i in range(2)]
    t1s = [sc.sb(f"p5_t1{i}", [128, 512]) for i in range(2)]
    t2s = [sc.sb(f"p5_t2{i}", [128, 512]) for i in range(2)]
    yas = [sc.sb(f"p5_ya{i}", [128, 512]) for i in range(2)]
    yabs = [sc.sb(f"p5_yab{i}", [128, 512], BF16) for i in range(2)]
    hts = [sc.sb(f"p5_h{i}", [128, D]) for i in range(2)]
    hos = [sc.sb(f"p5_ho{i}", [128, D]) for i in range(2)]
    def fin(j):
        blk = slice(j * 512, (j + 1) * 512)
        yg = ygT[j % 2]
        for (src, zoff, goff, kbase) in ((ybT, OB_ZB, CV_LOG, 4), (ycT, OB_ZC, CV_MOG, 8)):
            ys = []
            pS = rr(G.ps, st, "ps")
            for ct in range(4):
                yb = rr(ybs, st, "yb"); ys.append(yb)
                S.dma(S.q_sync, yb[:], src[ct * 128:(ct + 1) * 128, blk], R=[src], W=[yb])
                sq = rr(sqs, st, "sq")
                S.actf(sq[:], yb[:], AF.Square, R=[yb], W=[sq])
                S.mm(pS[:], [(G.ones_f[:], sq[:])], R=[G.ones_f, sq], W=[pS], start=(ct == 0), stop=(ct == 3))
            rs = rr(rstd, st, "rstd")
            S.actf(rs[:], pS[:], AF.Sqrt, R=[pS], W=[rs], scale=1.0 / 512, bias=G.eps_col[:, 0:1])
            S.op(S.dve, lambda rs=rs: nc.vector.reciprocal(out=rs[:], in_=rs[:]), [rs], [rs])
            for ct in range(4):
                zb = rr(zbs, st, "zb")
                S.dma(S.q_pool, zb[:], pbT[zoff + ct * 128:zoff + (ct + 1) * 128, blk], R=[pbT], W=[zb])
                t1 = rr(t1s, st, "t1"); t2 = rr(t2s, st, "t2")
                S.actf(t1[:], zb[:], AF.Silu, R=[zb], W=[t1])
                S.stt(S.dve, t2[:], ys[ct][:], cvt[:, goff + ct:goff + ct + 1], rs[:], ALU.mult, ALU.mult, R=[ys[ct], cvt, rs], W=[t2])
                S.tt(S.dve, yg[:, kbase + ct, :], t1[:], t2[:], ALU.mult, R=[t1, t2], W=[yg])
        for tt in range(4):
            t = 4 * j + tt
            ya = rr(yas, st, "ya"); yab = rr(yabs, st, "yab")
            S.dma(S.q_sync, ya[:], yga[t * 128:(t + 1) * 128, :], R=[yga], W=[ya])
            S.cp(S.act, yab[:], ya[:], R=[ya], W=[yab])
            pb = rr(G.psb, st, "psb")
            S.tr([(pb[:, ct * 128:(ct + 1) * 128], yab[:, ct * 128:(ct + 1) * 128]) for ct in range(4)], G.ident_bf[:],
                 R=[yab, G.ident_bf], W=[pb])
            S.cp(S.act, yg[:, 0:4, tt * 128:(tt + 1) * 128], pb[:, 0:512].rearrange("p (c t) -> p c t", c=4), R=[pb], W=[yg])

    def outproj(j):
        yg = ygT[j % 2]
        for tt in range(4):
            t = 4 * j + tt
            ht = rr(hts, st, "h"); ho = rr(hos, st, "ho")
            S.dma(S.q_sync, ht[:], hsrc[t * 128:(t + 1) * 128, :], R=[hsrc], W=[ht])
            for half in range(2):
                cs_ = slice(half * 512, (half + 1) * 512)
                pO = rr(G.ps, st, "ps")
                S.mm(pO[:], [(yg[:, k, tt * 128:(tt + 1) * 128], wo[:, k, cs_]) for k in range(12)], R=[yg, wo], W=[pO])
                S.tt(S.dve, ho[:, cs_], pO[:], ht[:, cs_], ALU.add, R=[pO, ht], W=[ho])
            if final:
                s_ = rr(ssf, st, "ssf")
                S.actf(junk[:], ho[:], AF.Square, R=[ho], W=[junk, s_], accum=s_[:, 0:1])
                S.actf(s_[:, 1:2], s_[:, 0:1], AF.Sqrt, R=[s_], W=[s_], scale=1.0 / D, bias=G.eps_col[:, 0:1])
                S.op(S.dve, lambda a=s_: nc.vector.reciprocal(out=a[:, 2:3], in_=a[:, 1:2]), [s_], [s_])
                S.stt(S.dve, ht[:], ho[:], s_[:, 2:3], fg[:], ALU.mult, ALU.mult, R=[ho, s_, fg], W=[ht])
                S.dma(S.q_pool, hdst[t * 128:(t + 1) * 128, :], ht[:], R=[ht], W=[hdst])
            else:
                S.dma(S.q_pool, hdst[t * 128:(t + 1) * 128, :], ho[:], R=[ho], W=[hdst])

    fin(0)
    for j in range(nblk):
        if j + 1 < nblk:
            fin(j + 1)
        outproj(j)
    sc.close()


def build(T, L, phases=None, debug=False):
    nc = bass.Bass("TRN2", target_bir_lowering=False)
    S = Sched(nc)
    C = declare_inputs(S, T, L)
    G = Ctx()
    load_consts(S, C, G)
    G.eps_col = S.sb("eps_col", [128, 2])
    S.memset(S.dve, G.eps_col[:, 0:1], EPS, W=[G.eps_col])
    S.memset(S.dve, G.eps_col[:, 1:2], GN_EPS, W=[G.eps_col])
    full = phases is None
    if full:
        phases = ("p1", "p2", "p3", "p4", "p5")
    dbg = lambda name: "ExternalOutput" if (debug and name in phases) else "Internal"
    pa = S.dram("pa", [T, NA], F32, kind=dbg("p1"))
    pbT = S.dram("pbT", [NB, T], F32, kind=dbg("p1"))
    ybT = S.dram("ybT", [512, T], F32, kind=dbg("p3"))
    ycT = S.dram("ycT", [512, T], F32, kind=dbg("p4"))
    yga = S.dram("yga", [T, 512], F32, kind=dbg("p2"))
    hb = [S.dram(f"hbuf{i}", [T, D], F32) for i in range(2)]
    hout = S.dram("hout", [T, D], F32, kind="ExternalOutput" if (full or "p5" in phases) else "Internal")
    outs = []
    nl = L if full else 1
    for l in range(nl):
        hsrc = C.x if l == 0 else hb[(l - 1) % 2]
        last = l == nl - 1
        hdst = hout if last else hb[l % 2]
        phase_in_proj(S, C, G, l, hsrc, pa, pbT, T)
        if "p2" in phases: phase_rwkv(S, C, G, l, pa, yga, T)
        if "p3" in phases: phase_lru(S, C, G, l, pbT, ybT, T)
        if "p4" in phases: phase_mla(S, C, G, l, pbT, ycT, T)
        if "p5" in phases: phase_out(S, C, G, l, hsrc, hdst, pbT, yga, ybT, ycT, T, final=(full and last))
    if debug:
        for nm, b in (("p1", pa), ("p1", pbT), ("p3", ybT), ("p4", ycT), ("p2", yga)):
            if nm in phases: outs.append(b)
    if full or "p5" in phases: outs.append(hout)
    S.finish(outs)
    emit_all(S)
    return nc, S

from concourse.bass_utils import run_bass_kernel_spmd

T_FULL = 4096
L_FULL = 4
_IN_NAMES = ["x", "wA", "wB", "rv", "w0a0", "w2a2", "cv", "gw", "wq", "wqs", "wk", "wv", "w_out", "final_g",
             "ident_bf", "ident_f", "cs", "masks", "mask_bf"]
_NC_CACHE = {}


def kernel(**inputs):
    x = np.asarray(inputs["x"], np.float32)
    B, T, _ = x.shape
    L = np.asarray(inputs["w_in"]).shape[0]
    hp = host_prep(inputs, T)
    key = (T, L)
    if key not in _NC_CACHE:
        _NC_CACHE[key] = build(T, L)[0]
    nc = _NC_CACHE[key]
    in_maps = []
    for b in range(B):
        m = {k: hp[k] for k in _IN_NAMES if k != "x"}
        m["x"] = np.ascontiguousarray(x[b])
        in_maps.append(m)
    res = run_bass_kernel_spmd(nc, in_maps, core_ids=list(range(B)))
    return np.stack([np.asarray(r["hout"], np.float32) for r in res.results], axis=0)
```

```python
import numpy as np
import concourse.bass as bass
import concourse.mybir as mybir

F32 = mybir.dt.float32
BF16 = mybir.dt.bfloat16
ALU = mybir.AluOpType
AF = mybir.ActivationFunctionType
AX = mybir.AxisListType


class Sem:
    def __init__(self, nc, name):
        self.h = nc.alloc_semaphore(name) if hasattr(nc, "alloc_semaphore") else None
        self.name = name
        self.count = 0


class Buf:
    __slots__ = ("t", "name", "last_write", "reads")

    def __init__(self, t, name):
        self.t = t
        self.name = name
        self.last_write = None
        self.reads = {}

    def __getitem__(self, idx):
        return self.t[idx]


SEM_LIMIT = 8000


class Eng:
    def cur_sem(self, i=0):
        sem = self.sems[i]
        if sem.count >= SEM_LIMIT:
            self.nrot = getattr(self, "nrot", 0) + 1
            sem = self.S.new_sem(f"{self.name}_r{self.nrot}")
            self.sems[i] = sem
        return sem

    def __init__(self, S, name, eng, is_dma=False, nsem=1):
        self.S = S
        self.name = name
        self.e = eng
        self.is_dma = is_dma
        self.sems = [S.new_sem(f"{name}_s{i}") for i in range(nsem)]
        self.rr = 0
        self.known = {}
        self.prog = []

    def wait(self, tok):
        sem, val = tok
        if self.known.get(id(sem), 0) >= val:
            return
        e = self.e; h = sem.h
        self.prog.append(lambda: e.wait_ge(h, val))
        self.known[id(sem)] = val
        self.S.nwaits += 1


class Sched:
    def __init__(self, nc):
        self.nc = nc
        import contextlib
        self.es = contextlib.ExitStack()
        self.nwaits = 0
        self.ninst = 0
        self.sem_list = []
        self.pe = Eng(self, "pe", nc.tensor)
        self.dve = Eng(self, "dve", nc.vector)
        self.act = Eng(self, "act", nc.scalar)
        self.pool = Eng(self, "pool", nc.gpsimd)
        self.q_sync = Eng(self, "qsync", nc.sync, is_dma=True, nsem=8)
        self.q_pool = Eng(self, "qpool", nc.gpsimd, is_dma=True, nsem=4)
        self.q_pool.prog = self.pool.prog
        self.engines = [self.pe, self.dve, self.act, self.pool, self.q_sync]

    def new_sem(self, name):
        s = Sem.__new__(Sem)
        s.is_pe = name.startswith("pe_")
        s.name = name
        s.count = 0
        s.h = self.es.enter_context(self.nc.semaphore(name))
        self.sem_list.append(s)
        return s

    def sb(self, name, shape, dtype=F32):
        t = self.nc.alloc_sbuf_tensor(name, list(shape), dtype)
        return Buf(t, name)

    def ps(self, name, shape, dtype=F32):
        t = self.nc.alloc_psum_tensor(name, list(shape), dtype)
        return Buf(t, name)

    def dram(self, name, shape, dtype=F32, kind="Internal"):
        t = self.nc.dram_tensor(name, list(shape), dtype, kind=kind)
        return Buf(t, name)

    def _deps(self, reads, writes):
        deps = []
        for b in reads:
            if b.last_write is not None:
                deps.append(b.last_write)
        for b in writes:
            if b.last_write is not None:
                deps.append(b.last_write)
            deps.extend(b.reads.values())
        return deps

    def _commit(self, tok, reads, writes):
        for b in reads:
            if b not in writes:
                b.reads[id(tok[0])] = tok
        for b in writes:
            b.last_write = tok
            b.reads = {}

    def op(self, eng, fn, reads=(), writes=()):
        for tok in self._deps(reads, writes):
            if eng is self.pe and tok[0].is_pe:
                continue
            eng.wait(tok)
        sem = eng.cur_sem()
        sem.count += 1
        h = sem.h
        eng.prog.append(lambda: fn().then_inc(h, 1))
        tok = (sem, sem.count)
        self._commit(tok, reads, writes)
        self.ninst += 1
        return tok

    def group(self, eng, fns, reads=(), writes=()):
        for tok in self._deps(reads, writes):
            if eng is self.pe and tok[0].is_pe:
                continue
            eng.wait(tok)
        fns = list(fns)
        for fn in fns[:-1]:
            eng.prog.append(fn)
            self.ninst += 1
        self.ninst += 1
        sem = eng.cur_sem()
        sem.count += 1
        h = sem.h
        last = fns[-1]
        eng.prog.append(lambda: last().then_inc(h, 1))
        tok = (sem, sem.count)
        self._commit(tok, reads, writes)
        return tok

    def dma(self, q, out_ap, in_ap, R=(), W=(), **kw):
        i = q.rr % len(q.sems)
        sem = q.sems[i]
        q.rr += 1
        if sem.count > 0:
            q.wait((sem, sem.count))
        sem = q.cur_sem(i)
        for tok in self._deps(R, W):
            q.wait(tok)
        sem.count += 16
        h = sem.h; e = q.e
        q.prog.append(lambda: e.dma_start(out=out_ap, in_=in_ap, **kw).then_inc(h, 16))
        tok = (sem, sem.count)
        self._commit(tok, R, W)
        self.ninst += 1
        return tok

    def ts(self, eng, out, in0, s1, s2=None, op0=ALU.mult, op1=None, R=(), W=(), accum=None):
        e = eng.e
        kw = {}
        if op1 is not None: kw["op1"] = op1
        if accum is not None: kw["accum_out"] = accum
        return self.op(eng, lambda: e.tensor_scalar(out=out, in0=in0, scalar1=s1, scalar2=s2, op0=op0, **kw), R, W)

    def tt(self, eng, out, in0, in1, op, R=(), W=()):
        e = eng.e
        return self.op(eng, lambda: e.tensor_tensor(out=out, in0=in0, in1=in1, op=op), R, W)

    def stt(self, eng, out, in0, scalar, in1, op0, op1, R=(), W=()):
        e = eng.e
        return self.op(eng, lambda: e.scalar_tensor_tensor(out=out, in0=in0, scalar=scalar, in1=in1, op0=op0, op1=op1), R, W)

    def cp(self, eng, out, in_, R=(), W=()):
        e = eng.e
        if eng is self.act:
            return self.op(eng, lambda: e.copy(out=out, in_=in_), R, W)
        return self.op(eng, lambda: e.tensor_copy(out=out, in_=in_), R, W)

    def actf(self, out, in_, func, R=(), W=(), scale=1.0, bias=None, accum=None):
        e = self.act.e
        kw = {}
        if bias is not None: kw["bias"] = bias
        if accum is not None: kw["accum_out"] = accum
        return self.op(self.act, lambda: e.activation(out=out, in_=in_, func=func, scale=scale, **kw), R, W)

    def mm(self, out, pairs, R=(), W=(), start=True, stop=True, **kw):
        e = self.pe.e
        n = len(pairs)
        fns = []
        for i, (l, r) in enumerate(pairs):
            st = start and i == 0
            sp = stop and i == n - 1
            fns.append((lambda l=l, r=r, st=st, sp=sp: e.matmul(out, lhsT=l, rhs=r, start=st, stop=sp, **kw)))
        return self.group(self.pe, fns, R, W)

    def tr(self, outs_ins, ident, R=(), W=()):
        e = self.pe.e
        fns = [(lambda o=o, i=i: e.transpose(out=o, in_=i, identity=ident)) for (o, i) in outs_ins]
        return self.group(self.pe, fns, R, W)

    def memset(self, eng, ap, val, W=()):
        e = eng.e
        return self.op(eng, lambda: e.memset(ap, val), (), W)

    def barrier(self):
        toks = [(s, s.count) for s in self.sem_list if s.count > 0]
        for eng in [self.pe, self.dve, self.act, self.pool, self.q_sync]:
            for tok in toks:
                eng.wait(tok)

    def finish(self, bufs):
        for b in bufs:
            if b.last_write is not None:
                self.q_sync.wait(b.last_write)


def emit_all(S):
    nc = S.nc
    def run(prog):
        def f(e):
            for g in prog: g()
        return f
    with nc.Block() as block:
        if S.q_sync.prog: block.sync(run(S.q_sync.prog))
        if S.pe.prog: block.tensor(run(S.pe.prog))
        if S.dve.prog: block.vector(run(S.dve.prog))
        if S.act.prog: block.scalar(run(S.act.prog))
        if S.pool.prog: block.gpsimd(run(S.pool.prog))
    S.es.close()


def _prune(reads):
    best = {}
    for sem, val in reads:
        k = id(sem)
        if k not in best or best[k][1] < val:
            best[k] = (sem, val)
    return list(best.values())

D = 1024
GW = 512
NH = 8
HD = 64
RW_IN = 1664
NA = 2176
NB = 1984
QL = 256
KVL = 128
DMIX = 1536

OB_UB, OB_ZB, OB_QL, OB_KV, OB_ZC, OB_KR, OB_KRS = 0, 512, 1024, 1280, 1408, 1920, 1952

RV_MU, RV_KK, RV_KA, RV_RK, RV_LG, RV_LB = 0, 1664, 2176, 2688, 3200, 3712
NRV = 4224
CV_LNG = 0
CV_CW = 8
CV_CB = 24
CV_GAB = 28
CV_GXB = 32
CV_LAM = 36
CV_LOG = 40
CV_QNG = 44
CV_KVNG = 46
CV_MOG = 47
NCV = 52


def host_prep(inp, T):
    f = np.float32
    L = inp["w_in"].shape[0]
    out = {}
    w_in = np.asarray(inp["w_in"], f)
    out["wA"] = np.ascontiguousarray(w_in[:, :, 0:NA])
    c = 2176
    colsB = np.concatenate([
        np.arange(c, c + 512), np.arange(c + 512, c + 1024), np.arange(3200, 3456), np.arange(3456, 3584),
        np.arange(3616, 4128), np.arange(3584, 3616), np.arange(3600, 3616), np.arange(3584, 3600)])
    assert colsB.size == NB
    out["wB"] = np.ascontiguousarray(w_in[:, :, colsB])
    rv = np.zeros((L, NRV), f)
    rv[:, RV_MU:RV_MU + 1664] = inp["rwkv_mu"]
    rv[:, RV_KK:RV_KK + 512] = inp["rwkv_k_k"]
    rv[:, RV_KA:RV_KA + 512] = inp["rwkv_k_a"]
    rv[:, RV_RK:RV_RK + 512] = np.asarray(inp["rwkv_r_k"], f).reshape(L, 512)
    rv[:, RV_LG:RV_LG + 512] = inp["rwkv_lnx_g"]
    rv[:, RV_LB:RV_LB + 512] = inp["rwkv_lnx_b"]
    out["rv"] = rv
    out["w0a0"] = np.ascontiguousarray(np.concatenate([np.asarray(inp["rwkv_w0"], f), np.asarray(inp["rwkv_a0"], f)], axis=1))
    out["w2a2"] = np.ascontiguousarray(np.concatenate([np.asarray(inp["rwkv_w2"], f), np.asarray(inp["rwkv_a2"], f)], axis=1))
    cv = np.zeros((L, 128, NCV), f)
    def colfill(off, vec, n):
        cv[:, :, off:off + n] = np.asarray(vec, f).reshape(L, n, 128).transpose(0, 2, 1)
    colfill(CV_LNG, inp["ln_g"], 8)
    cw = np.asarray(inp["lru_conv_w"], f)
    for j in range(4):
        colfill(CV_CW + j * 4, cw[:, j], 4)
    colfill(CV_CB, inp["lru_conv_b"], 4)
    colfill(CV_GAB, inp["lru_ga_b"], 4)
    colfill(CV_GXB, inp["lru_gx_b"], 4)
    colfill(CV_LAM, inp["lru_lam"], 4)
    colfill(CV_LOG, inp["lru_out_g"], 4)
    colfill(CV_QNG, inp["mla_q_norm_g"], 2)
    colfill(CV_KVNG, inp["mla_kv_norm_g"], 1)
    colfill(CV_MOG, inp["mla_out_g"], 4)
    out["cv"] = cv
    gw = np.zeros((L, 4, 128, 2, 128), f)
    for gi, nm in enumerate(["lru_ga_w", "lru_gx_w"]):
        w = np.asarray(inp[nm], f)
        for h in range(8):
            ct, o = h // 2, (h % 2) * 64
            gw[:, ct, o:o + 64, gi, o:o + 64] = w[:, h]
    out["gw"] = gw
    wuq = np.asarray(inp["mla_w_uq"], f).reshape(L, 256, 8, 96)
    wq = np.zeros((L, 256, 8, 128), f)
    wq[..., 0:32] = wuq[..., 64:96]
    wq[..., 64:128] = wuq[..., 0:64]
    out["wq"] = np.ascontiguousarray(wq.reshape(L, 256, 1024))
    wqs = np.zeros((L, 256, 8, 128), f)
    wqs[..., 0:16] = wuq[..., 80:96]
    wqs[..., 16:32] = wuq[..., 64:80]
    out["wqs"] = np.ascontiguousarray(wqs.reshape(L, 256, 1024))
    wukv = np.asarray(inp["mla_w_ukv"], f).reshape(L, 128, 8, 128)
    wk = np.zeros((L, 128, 8, 128), f)
    wk[..., 64:128] = wukv[..., 0:64]
    out["wk"] = np.ascontiguousarray(wk.reshape(L, 128, 1024))
    out["wv"] = np.ascontiguousarray(wukv[..., 64:128].reshape(L, 128, 512))
    out["w_out"] = np.asarray(inp["w_out"], f)
    out["final_g"] = np.asarray(inp["final_g"], f).reshape(1, 1024)
    import ml_dtypes
    bf = ml_dtypes.bfloat16
    out["ident_bf"] = np.eye(128, dtype=f).astype(bf)
    out["ident_f"] = np.eye(128, dtype=f)
    half = 16
    inv_freq = (10000.0 ** (-np.arange(half, dtype=f) * 2.0 / 32)).astype(f)
    ang = np.arange(T, dtype=f)[:, None] * inv_freq[None, :]
    cos, sin = np.cos(ang).astype(f).T, np.sin(ang).astype(f).T
    cs = np.zeros((128, 2, T), f)
    cs[32:128, 0] = 1.0
    cs[0:16, 0], cs[16:32, 0] = cos, cos
    cs[0:16, 1], cs[16:32, 1] = -sin, sin
    out["cs"] = cs
    ii = np.arange(128)
    m = np.zeros((128, 4, 128), f)
    m[:, 0] = (ii[:, None] < ii[None, :])
    m[:, 1] = (ii[:, None] <= ii[None, :])
    m[:, 2] = (ii[:, None] > ii[None, :])
    m[:, 3] = 1.0
    out["masks"] = m
    out["mask_bf"] = (ii[:, None] <= ii[None, :]).astype(f).astype(bf)
    return out

import contextlib

EPS = 1e-6


class Ctx:
    pass


def declare_inputs(S, T, L):
    C = Ctx()
    di = lambda n, shp, dt=F32: S.dram(n, shp, dt, kind="ExternalInput")
    C.x = di("x", [T, D])
    C.wA = di("wA", [L, D, NA]); C.wB = di("wB", [L, D, NB])
    C.rv = di("rv", [L, NRV]); C.w0a0 = di("w0a0", [L, 1024]); C.w2a2 = di("w2a2", [L, 128, 512])
    C.cv = di("cv", [L, 128, NCV]); C.gw = di("gw", [L, 4, 128, 2, 128])
    C.wq = di("wq", [L, 256, 1024]); C.wqs = di("wqs", [L, 256, 1024])
    C.wk = di("wk", [L, 128, 1024]); C.wv = di("wv", [L, 128, 512])
    C.w_out = di("w_out", [L, DMIX, D]); C.final_g = di("final_g", [1, D])
    C.ident_bf = di("ident_bf", [128, 128], BF16); C.ident_f = di("ident_f", [128, 128])
    C.cs = di("cs", [128, 2, T]); C.masks = di("masks", [128, 4, 128]); C.mask_bf = di("mask_bf", [128, 128], BF16)
    return C


class Scope:
    def __init__(self, S):
        self.S = S
        self.es = contextlib.ExitStack()

    _n = [0]

    def sb(self, name, shape, dtype=F32):
        Scope._n[0] += 1
        name = f"{name}_u{Scope._n[0]}"
        t = self.es.enter_context(self.S.nc.sbuf_tensor(name, list(shape), dtype))
        return Buf(t, name)

    def close(self):
        self.S.barrier()
        self.es.close()


def load_consts(S, C, G):
    nc = S.nc
    G.ident_bf = S.sb("ident_bf_sb", [128, 128], BF16)
    G.ident_f = S.sb("ident_f_sb", [128, 128])
    G.masks = S.sb("masks_sb", [128, 4, 128])
    G.mask_bf = S.sb("mask_bf_sb", [128, 128], BF16)
    G.ones_f = S.sb("ones_f", [128, 128])
    S.dma(S.q_sync, G.ident_bf[:], C.ident_bf[:], W=[G.ident_bf])
    S.dma(S.q_sync, G.ident_f[:], C.ident_f[:], W=[G.ident_f])
    S.dma(S.q_sync, G.masks[:], C.masks[:], W=[G.masks])
    S.dma(S.q_sync, G.mask_bf[:], C.mask_bf[:], W=[G.mask_bf])
    S.memset(S.dve, G.ones_f[:], 1.0, W=[G.ones_f])
    G.ps = [S.ps(f"ps{i}", [128, 512], F32) for i in range(6)]
    G.psb = [S.ps(f"psb{i}", [128, 1024], BF16) for i in range(2)]


def rr(lst, state, key):
    i = state.get(key, 0)
    state[key] = i + 1
    return lst[i % len(lst)]


def phase_in_proj(S, C, G, l, hsrc, pa, pbT, T):
    nc = S.nc
    sc = Scope(S)
    st = {}
    wA = sc.sb("wA_sb", [128, 8, NA], BF16)
    wB = sc.sb("wB_sb", [128, 8, NB], BF16)
    cvt = sc.sb("p1_cv", [128, NCV])
    S.dma(S.q_sync, cvt[:], C.cv[l], W=[cvt])
    stg = [sc.sb(f"p1_stg{i}", [128, NA]) for i in range(4)]
    wAv = C.wA[l].rearrange("(k p) c -> p k c", p=128)
    wBv = C.wB[l].rearrange("(k p) c -> p k c", p=128)
    n = 0
    for k in range(8):
        for (src, dst, nc_) in ((wAv, wA, NA), (wBv, wB, NB)):
            sg = stg[n % 4]
            S.dma(S.q_sync if n % 2 == 0 else S.q_pool, sg[:, 0:nc_], src[:, k, :], W=[sg])
            if n % 2 == 0:
                S.ts(S.dve, dst[:, k, :], sg[:, 0:nc_], cvt[:, CV_LNG + k:CV_LNG + k + 1], R=[sg, cvt], W=[dst])
            else:
                S.actf(dst[:, k, :], sg[:, 0:nc_], AF.Copy, R=[sg, cvt], W=[dst], scale=cvt[:, CV_LNG + k:CV_LNG + k + 1])
            n += 1
    hts = [sc.sb(f"p1_h{i}", [128, D]) for i in range(3)]
    junk = sc.sb("p1_junk", [128, D], BF16)
    xnb = [sc.sb(f"p1_xnb{i}", [128, D], BF16) for i in range(2)]
    ss = [sc.sb(f"p1_ss{i}", [128, 4]) for i in range(2)]
    xnT = [sc.sb(f"p1_xnT{i}", [128, 8, 512], BF16) for i in range(2)]
    oA = [sc.sb(f"p1_oA{i}", [128, NA]) for i in range(2)]
    oB = [sc.sb(f"p1_oB{i}", [128, 512]) for i in range(3)]
    nblk = T // 512
    ntile = T // 128
    evc = [0]
    def ev_eng():
        evc[0] += 1
        return S.act if evc[0] % 2 else S.dve

    def stageX(t):
        j, tt = t // 4, t % 4
        xT = xnT[j % 2]
        ht = rr(hts, st, "h")
        S.dma(S.q_sync, ht[:], hsrc[t * 128:(t + 1) * 128, :], R=[hsrc], W=[ht])
        s_ = rr(ss, st, "ss")
        S.actf(junk[:], ht[:], AF.Square, R=[ht], W=[junk, s_], accum=s_[:, 0:1])
        S.actf(s_[:, 1:2], s_[:, 0:1], AF.Sqrt, R=[s_], W=[s_], scale=1.0 / D, bias=G.eps_col[:, 0:1])
        S.op(S.dve, lambda a=s_: nc.vector.reciprocal(out=a[:, 2:3], in_=a[:, 1:2]), [s_], [s_])
        xb = rr(xnb, st, "xnb")
        S.ts(S.dve, xb[:], ht[:], s_[:, 2:3], R=[ht, s_], W=[xb])
        pb = rr(G.psb, st, "psb")
        S.tr([(pb[:, k * 128:(k + 1) * 128], xb[:, k * 128:(k + 1) * 128]) for k in range(8)], G.ident_bf[:],
             R=[xb, G.ident_bf], W=[pb])
        S.cp(S.act, xT[:, :, tt * 128:(tt + 1) * 128], pb[:].rearrange("p (k c) -> p k c", k=8), R=[pb], W=[xT])

    def stageM(t):
        j, tt = t // 4, t % 4
        xT = xnT[j % 2]
        o = rr(oA, st, "oA")
        c0 = 0
        while c0 < NA:
            cn = min(512, NA - c0)
            p = rr(G.ps, st, "ps")
            S.mm(p[:, 0:cn], [(xT[:, k, tt * 128:(tt + 1) * 128], wA[:, k, c0:c0 + cn]) for k in range(8)],
                 R=[xT, wA], W=[p])
            S.cp(ev_eng(), o[:, c0:c0 + cn], p[:, 0:cn], R=[p], W=[o])
            c0 += cn
        S.dma(S.q_pool, pa[t * 128:(t + 1) * 128, :], o[:], R=[o], W=[pa])

    def stageF(j):
        xT = xnT[j % 2]
        r0 = 0
        while r0 < NB:
            rn = min(128, NB - r0)
            p = rr(G.ps, st, "ps")
            S.mm(p[0:rn, :], [(wB[:, k, r0:r0 + rn], xT[:, k, :]) for k in range(8)], R=[xT, wB], W=[p])
            o = rr(oB, st, "oB")
            S.cp(ev_eng(), o[0:rn, :], p[0:rn, :], R=[p], W=[o])
            S.dma(S.q_pool, pbT[r0:r0 + rn, j * 512:(j + 1) * 512], o[0:rn, :], R=[o], W=[pbT])
            r0 += rn

    stageX(0)
    for t in range(ntile):
        if t + 1 < ntile:
            stageX(t + 1)
        stageM(t)
        if t % 4 == 3:
            stageF(t // 4)
    sc.close()


def phase_lru(S, C, G, l, pbT, ybT, T):
    nc = S.nc
    sc = Scope(S)
    st = {}
    cvt = sc.sb("p3_cv", [128, NCV])
    S.dma(S.q_sync, cvt[:], C.cv[l], W=[cvt])
    gwt = sc.sb("p3_gw", [128, 4, 2, 128])
    S.dma(S.q_sync, gwt[:], C.gw[l].rearrange("c p g q -> p c g q"), W=[gwt])
    c8 = sc.sb("p3_c8", [128, 12])
    S.actf(c8[:, 0:4], cvt[:, CV_LAM:CV_LAM + 4], AF.Exp, R=[cvt], W=[c8], scale=-1.0)
    S.actf(c8[:, 4:8], c8[:, 0:4], AF.Ln, R=[c8], W=[c8], bias=1.0)
    S.ts(S.dve, c8[:, 8:12], c8[:, 4:8], -8.0, R=[c8], W=[c8])
    NB_ = 2
    mk = lambda n, w=512: [[sc.sb(f"p3_{n}{ct}_{i}", [128, w]) for i in range(NB_)] for ct in range(4)]
    ubs, xcs, grs, gis, a_s, oms, bxs, hos = mk("ub", 515), mk("xc"), mk("gr"), mk("gi"), mk("a"), mk("om"), mk("bx"), mk("ho")
    zero = sc.sb("p3_zero", [128, 1])
    S.memset(S.dve, zero[:], 0.0, W=[zero])
    nblk = T // 512
    cw = lambda j, ct: cvt[:, CV_CW + j * 4 + ct:CV_CW + j * 4 + ct + 1]
    col = lambda off, ct: cvt[:, off + ct:off + ct + 1]
    hprev = [(zero[:, 0:1], zero) for _ in range(4)]
    CT = range(4)
    for j in range(nblk):
        b_ = j % NB_
        ub = [ubs[ct][b_] for ct in CT]; xc = [xcs[ct][b_] for ct in CT]; gr = [grs[ct][b_] for ct in CT]
        gi = [gis[ct][b_] for ct in CT]; a = [a_s[ct][b_] for ct in CT]; om = [oms[ct][b_] for ct in CT]
        bx = [bxs[ct][b_] for ct in CT]; ho = [hos[ct][b_] for ct in CT]
        for ct in CT:
            r0 = OB_UB + ct * 128
            if j == 0:
                S.memset(S.pool, ub[ct][:, 0:3], 0.0, W=[ub[ct]])
                S.dma(S.q_sync, ub[ct][:, 3:515], pbT[r0:r0 + 128, 0:512], R=[pbT], W=[ub[ct]])
            else:
                S.dma(S.q_sync, ub[ct][:], pbT[r0:r0 + 128, j * 512 - 3:(j + 1) * 512], R=[pbT], W=[ub[ct]])
        for ct in CT:
            S.ts(S.dve, xc[ct][:], ub[ct][:, 0:512], cw(0, ct), col(CV_CB, ct), op0=ALU.mult, op1=ALU.add, R=[ub[ct], cvt], W=[xc[ct]])
        for jj in range(1, 4):
            for ct in CT:
                S.stt(S.dve, xc[ct][:], ub[ct][:, jj:jj + 512], cw(jj, ct), xc[ct][:], ALU.mult, ALU.add, R=[ub[ct], cvt, xc[ct]], W=[xc[ct]])
        ps1 = []; ps2 = []
        for ct in CT:
            p1 = rr(G.ps, st, "ps"); ps1.append(p1)
            S.mm(p1[:], [(gwt[:, ct, 0, :], xc[ct][:])], R=[gwt, xc[ct]], W=[p1])
            S.actf(gr[ct][:], p1[:], AF.Sigmoid, R=[p1, cvt], W=[gr[ct]], bias=col(CV_GAB, ct))
        for ct in CT:
            p2 = rr(G.ps, st, "ps"); ps2.append(p2)
            S.mm(p2[:], [(gwt[:, ct, 1, :], xc[ct][:])], R=[gwt, xc[ct]], W=[p2])
            S.actf(gi[ct][:], p2[:], AF.Sigmoid, R=[p2, cvt], W=[gi[ct]], bias=col(CV_GXB, ct))
        for ct in CT:
            S.actf(a[ct][:], gr[ct][:], AF.Exp, R=[gr[ct], c8], W=[a[ct]], scale=c8[:, 8 + ct:9 + ct])
        for ct in CT:
            S.tt(S.dve, bx[ct][:], gi[ct][:], xc[ct][:], ALU.mult, R=[gi[ct], xc[ct]], W=[bx[ct]])
        for ct in CT:
            S.actf(om[ct][:], a[ct][:], AF.Square, R=[a[ct]], W=[om[ct]])
        for ct in CT:
            S.actf(om[ct][:], om[ct][:], AF.Sqrt, R=[om[ct]], W=[om[ct]], scale=-1.0, bias=1.0)
        for ct in CT:
            S.tt(S.dve, bx[ct][:], bx[ct][:], om[ct][:], ALU.mult, R=[bx[ct], om[ct]], W=[bx[ct]])
        for ct in CT:
            hp, hpb = hprev[ct]
            S.op(S.dve, lambda ho=ho[ct], a=a[ct], bx=bx[ct], hp=hp: nc.vector.tensor_tensor_scan(
                out=ho[:], data0=a[:], data1=bx[:], initial=hp, op0=ALU.mult, op1=ALU.add),
                [a[ct], bx[ct], hpb], [ho[ct]])
            S.dma(S.q_pool, ybT[ct * 128:(ct + 1) * 128, j * 512:(j + 1) * 512], ho[ct][:], R=[ho[ct]], W=[ybT])
            hprev[ct] = (ho[ct][:, 511:512], ho[ct])
    sc.close()


def phase_mla(S, C, G, l, pbT, ycT, T):
    nc = S.nc
    sc = Scope(S)
    st = {}
    nblk = T // 512
    cvt = sc.sb("p4_cv", [128, NCV])
    S.dma(S.q_sync, cvt[:], C.cv[l], W=[cvt])
    wq = sc.sb("p4_wq", [128, 2, 1024], BF16)
    wqs = sc.sb("p4_wqs", [128, 2, 1024], BF16)
    wk = sc.sb("p4_wk", [128, 1024], BF16)
    wv = sc.sb("p4_wv", [128, 512], BF16)
    sc2 = Scope(S)
    stg = [sc2.sb(f"p4_stg{i}", [128, 1024]) for i in range(2)]
    jobs = [(C.wq[l][0:128, :], wq[:, 0, :], 1024, wq), (C.wq[l][128:256, :], wq[:, 1, :], 1024, wq),
            (C.wqs[l][0:128, :], wqs[:, 0, :], 1024, wqs), (C.wqs[l][128:256, :], wqs[:, 1, :], 1024, wqs),
            (C.wk[l], wk[:], 1024, wk), (C.wv[l], wv[:], 512, wv)]
    for n, (src, dst, ncol, dbuf) in enumerate(jobs):
        sg = stg[n % 2]
        S.dma(S.q_sync, sg[:, 0:ncol], src, W=[sg])
        S.cp(S.dve if n % 2 == 0 else S.act, dst, sg[:, 0:ncol], R=[sg], W=[dbuf])
    sc2.close()
    E65 = sc.sb("p4_E96", [128, 64])
    S.memset(S.dve, E65[:], 0.0, W=[E65])
    S.memset(S.dve, E65[64:65, :], 1.0, W=[E65])
    KT = [sc.sb(f"p4_KT{j}", [128, 8, 512], BF16) for j in range(nblk)]
    VP = [sc.sb(f"p4_VP{j}", [128, 4, 8, 96], BF16) for j in range(nblk)]
    for j in range(nblk):
        S.memset(S.pool, KT[j][32:64, :, :], 0.0, W=[KT[j]])
        S.memset(S.pool, VP[j][:, :, :, 64:96], 0.0, W=[VP[j]])
        S.memset(S.pool, VP[j][:, :, :, 64:65], 1.0, W=[VP[j]])
    QT = [[sc.sb(f"p4_QT{b}_{h}", [128, 512], BF16) for h in range(8)] for b in range(1)]
    cst = [sc.sb(f"p4_cs{i}", [128, 2, 512]) for i in range(1)]
    qls = [sc.sb(f"p4_ql{i}", [128, 2, 512]) for i in range(1)]
    kvls = [sc.sb(f"p4_kvl{i}", [128, 512]) for i in range(2)]
    krs = [sc.sb(f"p4_kr{i}", [32, 2, 512]) for i in range(2)]
    sq = sc.sb("p4_sq", [128, 2, 512])
    rq = sc.sb("p4_rq", [128, 512]); rk = sc.sb("p4_rk", [128, 512])
    qn = sc.sb("p4_qn", [128, 2, 512], BF16)
    ckv = sc.sb("p4_ckv", [128, 512], BF16)
    t1s = [sc.sb(f"p4_t1{i}", [128, 512]) for i in range(2)]
    t2s = [sc.sb(f"p4_t2{i}", [128, 512]) for i in range(2)]
    pTs = [sc.sb(f"p4_pT{i}", [128, 512], BF16) for i in range(3)]
    osbs = [sc.sb(f"p4_osb{i}", [96, 512]) for i in range(2)]
    rls = [sc.sb(f"p4_rl{i}", [64, 512]) for i in range(2)]
    ohs = [sc.sb(f"p4_oh{i}", [64, 512]) for i in range(2)]
    psS = G.ps[0:3]; psO = G.ps[3:5]; psL = G.ps[5]
    scale = 96.0 ** -0.5
    for j in range(nblk):
        blk = slice(j * 512, (j + 1) * 512)
        ql = rr(qls, st, "ql"); kvl = rr(kvls, st, "kvl"); kr = rr(krs, st, "kr"); cs = rr(cst, st, "cs")
        for k in range(2):
            S.dma(S.q_sync, ql[:, k, :], pbT[OB_QL + k * 128:OB_QL + (k + 1) * 128, blk], R=[pbT], W=[ql])
        S.dma(S.q_sync, kvl[:], pbT[OB_KV:OB_KV + 128, blk], R=[pbT], W=[kvl])
        S.dma(S.q_pool, kr[:, 0, :], pbT[OB_KR:OB_KR + 32, blk], R=[pbT], W=[kr])
        S.dma(S.q_pool, kr[:, 1, :], pbT[OB_KRS:OB_KRS + 32, blk], R=[pbT], W=[kr])
        S.dma(S.q_pool, cs[:], C.cs[:, :, blk], W=[cs])
        S.actf(sq[:], ql[:], AF.Square, R=[ql], W=[sq])
        pA = rr(psS, st, "psS")
        S.mm(pA[:], [(G.ones_f[:], sq[:, 0, :]), (G.ones_f[:], sq[:, 1, :])], R=[G.ones_f, sq], W=[pA])
        S.actf(rq[:], pA[:], AF.Ln, R=[pA], W=[rq], scale=1.0 / QL, bias=G.eps_col[:, 0:1])
        S.actf(rq[:], rq[:], AF.Exp, R=[rq], W=[rq], scale=-0.5)
        for k in range(2):
            S.stt(S.dve, qn[:, k, :], ql[:, k, :], cvt[:, CV_QNG + k:CV_QNG + k + 1], rq[:], ALU.mult, ALU.mult,
                  R=[ql, cvt, rq], W=[qn])
        S.actf(sq[:, 0, :], kvl[:], AF.Square, R=[kvl], W=[sq])
        pB = rr(psS, st, "psS")
        S.mm(pB[:], [(G.ones_f[:], sq[:, 0, :])], R=[G.ones_f, sq], W=[pB])
        S.actf(rk[:], pB[:], AF.Ln, R=[pB], W=[rk], scale=1.0 / KVL, bias=G.eps_col[:, 0:1])
        S.actf(rk[:], rk[:], AF.Exp, R=[rk], W=[rk], scale=-0.5)
        S.stt(S.dve, ckv[:], kvl[:], cvt[:, CV_KVNG:CV_KVNG + 1], rk[:], ALU.mult, ALU.mult, R=[kvl, cvt, rk], W=[ckv])
        t1 = rr(t1s, st, "t1"); t2 = rr(t2s, st, "t2")
        S.tt(S.dve, t1[0:32, :], kr[:, 0, :], cs[0:32, 0, :], ALU.mult, R=[kr, cs], W=[t1])
        S.tt(S.dve, t2[0:32, :], kr[:, 1, :], cs[0:32, 1, :], ALU.mult, R=[kr, cs], W=[t2])
        for h in range(8):
            S.tt(S.dve, KT[j][0:32, h, :], t1[0:32, :], t2[0:32, :], ALU.add, R=[t1, t2], W=[KT[j]])
        for h in range(8):
            Qh = QT[0][h]
            pq = rr(psS, st, "psS")
            S.mm(pq[:], [(wq[:, k, h * 128:(h + 1) * 128], qn[:, k, :]) for k in range(2)], R=[wq, qn], W=[pq])
            pqs = rr(psS, st, "psS")
            S.mm(pqs[:], [(wqs[:, k, h * 128:(h + 1) * 128], qn[:, k, :]) for k in range(2)], R=[wqs, qn], W=[pqs])
            t1 = rr(t1s, st, "t1"); t2 = rr(t2s, st, "t2")
            S.tt(S.dve, t1[:], pq[:], cs[:, 0, :], ALU.mult, R=[pq, cs], W=[t1])
            S.tt(S.dve, t2[:], pqs[:], cs[:, 1, :], ALU.mult, R=[pqs, cs], W=[t2])
            S.tt(S.dve, Qh[:], t1[:], t2[:], ALU.add, R=[t1, t2], W=[Qh])
            pk = rr(psS, st, "psS")
            S.mm(pk[:], [(wk[:, h * 128:(h + 1) * 128], ckv[:])], R=[wk, ckv], W=[pk])
            S.cp(S.act, KT[j][64:128, h, :], pk[64:128, :], R=[pk], W=[KT[j]])
        for tt in range(4):
            pv = rr(psS, st, "psS")
            S.mm(pv[:], [(ckv[:, tt * 128:(tt + 1) * 128], wv[:])], R=[ckv, wv], W=[pv])
            S.cp(S.dve, VP[j][:, tt, :, 0:64], pv[:].rearrange("p (h d) -> p h d", h=8),
                 R=[pv], W=[VP[j]])
        nkt = 4 * j + 4

        def issue_S(h, kt):
            i = kt - 4 * j
            c0 = 128 * max(i, 0)
            N = 512 - c0
            kb, ko = kt // 4, (kt % 4) * 128
            pS = rr(psS, st, "psS")
            S.mm(pS[:, 0:N], [(KT[kb][:, h, ko:ko + 128], QT[0][h][:, c0:512])], R=[KT[kb], QT[0][h]], W=[pS])
            return pS

        pS_next = issue_S(0, 0)
        for h in range(8):
            po = rr(psO, st, "psO")
            for kt in range(nkt):
                i = kt - 4 * j
                c0 = 128 * max(i, 0)
                N = 512 - c0
                kb = kt // 4
                pS = pS_next
                if kt + 1 < nkt:
                    pS_next = issue_S(h, kt + 1)
                elif h + 1 < 8:
                    pS_next = issue_S(h + 1, 0)
                pT = rr(pTs, st, "pT")
                S.actf(pT[:, 0:N], pS[:, 0:N], AF.Exp, R=[pS], W=[pT], scale=scale)
                if i >= 0:
                    S.tt(S.pool, pT[:, 0:128], pT[:, 0:128], G.mask_bf[:], ALU.mult, R=[pT, G.mask_bf], W=[pT])
                S.mm(po[0:96, c0:512], [(VP[kb][:, kt % 4, h, :], pT[:, 0:N])], R=[VP[kb], pT], W=[po],
                     start=(kt == 0), stop=(kt == nkt - 1))
            osb = rr(osbs, st, "osb")
            S.cp(S.act, osb[:], po[0:96, :], R=[po], W=[osb])
            S.mm(psL[0:64, :], [(E65[0:96, :], osb[0:96, :])], R=[E65, osb], W=[psL])
            rl = rr(rls, st, "rl")
            S.actf(rl[:], psL[0:64, :], AF.Ln, R=[psL], W=[rl])
            S.actf(rl[:], rl[:], AF.Exp, R=[rl], W=[rl], scale=-1.0)
            oh = rr(ohs, st, "oh")
            S.tt(S.dve, oh[:], osb[0:64, :], rl[:], ALU.mult, R=[osb, rl], W=[oh])
            S.dma(S.q_sync, ycT[h * 64:(h + 1) * 64, blk], oh[:], R=[oh], W=[ycT])
    sc.close()


INV_DT = BF16
CDEC = 0.6065306597126334
GN_EPS = 64e-5


def bc_mid(ap, n):
    return ap.unsqueeze(1).broadcast_to([ap.shape[0], n, ap.shape[1]])


def bc_last(ap, n):
    return ap.unsqueeze(2).broadcast_to([ap.shape[0], ap.shape[1], n])


def h3(ap):
    return ap.rearrange("p (h d) -> p h d", h=8)


def phase_rwkv(S, C, G, l, pa, yga, T, dbg=None):
    nc = S.nc
    sc = Scope(S)
    st = {}
    nch = T // 128
    rvt = sc.sb("p2_rv", [128, NRV])
    S.dma(S.q_sync, rvt[:], C.rv[l:l + 1, :].partition_broadcast(128), W=[rvt])
    w0a0 = sc.sb("p2_w0a0", [1, 1024])
    S.dma(S.q_sync, w0a0[:], C.w0a0[l:l + 1, :], W=[w0a0])
    w2a2 = sc.sb("p2_w2a2", [128, 512])
    S.dma(S.q_sync, w2a2[:], C.w2a2[l], W=[w2a2])
    mu_b = rvt[:, RV_MU:RV_MU + 1664]
    kk_b = rvt[:, RV_KK:RV_KK + 512]; ka_b = rvt[:, RV_KA:RV_KA + 512]; rk_b = rvt[:, RV_RK:RV_RK + 512]
    lg_b = rvt[:, RV_LG:RV_LG + 512]; lb_b = rvt[:, RV_LB:RV_LB + 512]
    M_strict, M_incl, M_lower = G.masks[:, 0, :], G.masks[:, 1, :], G.masks[:, 2, :]
    ident_i = G.ident_f if INV_DT == F32 else G.ident_bf
    f32t = lambda n: sc.sb(f"p2_{n}", [128, 512])
    b16t = lambda n: sc.sb(f"p2_{n}", [128, 512], BF16)
    uas = [sc.sb(f"p2_ua{i}", [128, 1664]) for i in range(2)]
    ups = [sc.sb(f"p2_up{i}", [128, 1664]) for i in range(2)]
    zas = [f32t(f"za{i}") for i in range(2)]
    Tm = Ctx()
    Tm.lw = sc.sb("p2_lw", [128, 128])
    Tm.sgw, Tm.av, Tm.gam, Tm.ig, Tm.gae = f32t("sgw"), f32t("av"), f32t("gam"), f32t("ig"), f32t("gae")
    Tm.kk, Tm.sqt, Tm.kkn, Tm.e1, Tm.knew, Tm.rkt = f32t("kk"), f32t("sqt"), f32t("kkn"), f32t("e1"), f32t("knew"), f32t("rkt")
    Tm.ssq = sc.sb("p2_ssq", [128, 32])

    def outset(i):
        O = Ctx()
        O.kap_b, O.ktl_b, O.btl_b, O.rtl_b, O.v_b = [b16t(f"{n}{i}") for n in ("kapb", "ktlb", "btlb", "rtlb", "vb")]
        O.gC = sc.sb(f"p2_gC{i}", [64, 8])
        O.bon, O.sz = f32t(f"bon{i}"), f32t(f"sz{i}")
        return O

    def a2set(i):
        Z = Ctx()
        Z.KR = sc.sb(f"p2_KR{i}", [64, 8, 2, 128], BF16)
        Z.KB = sc.sb(f"p2_KB{i}", [64, 8, 2, 128], BF16)
        Z.AK = sc.sb(f"p2_AK{i}", [128, 8, 2, 128], BF16)
        Z.AB2 = sc.sb(f"p2_AB2{i}", [128, 8, 128], BF16)
        Z.Qs = [sc.sb(f"p2_Q{i}_{k}", [128, 8, 128], INV_DT) for k in range(2)]
        Z.QTs = [sc.sb(f"p2_QT{i}_{k}", [128, 8, 128], INV_DT) for k in range(2)]
        Z.Ys = [sc.sb(f"p2_Y{i}_{k}", [128, 8, 128], INV_DT) for k in range(2)]
        return Z

    OS = [outset(i) for i in range(3)]
    ZS = [a2set(i) for i in range(2)]
    Bt = Ctx()
    Bt.R_sb = sc.sb("p2_R", [128, 512], INV_DT); Bt.negU = b16t("negU")
    Bt.yt, Bt.yc, Bt.sq2 = f32t("yt"), f32t("yc"), f32t("sq2")
    Bt.ygo = [f32t(f"ygo{i}") for i in range(2)]
    Bt.ssb = sc.sb("p2_ssb", [128, 32])
    Hf = sc.sb("p2_Hf", [64, 512]); Hb = sc.sb("p2_Hb", [64, 512], BF16); Htmp = sc.sb("p2_Htmp", [64, 512])
    S.memset(S.dve, Hf[:], 0.0, W=[Hf]); S.memset(S.dve, Hb[:], 0.0, W=[Hb])

    def stageA1(c):
        X = Tm; O = OS[c % 3]
        ua, up, za = uas[c % 2], ups[c % 2], zas[c % 2]
        rows = slice(c * 128, (c + 1) * 128)
        S.dma(S.q_sync, ua[:], pa[rows, 0:1664], R=[pa], W=[ua])
        S.dma(S.q_sync, za[:], pa[rows, 1664:2176], R=[pa], W=[za])
        if c == 0:
            S.memset(S.pool, up[0:1, :], 0.0, W=[up])
            S.dma(S.q_sync, up[1:128, :], pa[0:127, 0:1664], R=[pa], W=[up])
        else:
            S.dma(S.q_sync, up[:], pa[c * 128 - 1:c * 128 + 127, 0:1664], R=[pa], W=[up])
        yield
        S.tt(S.dve, up[:], up[:], ua[:], ALU.subtract, R=[up, ua], W=[up])
        yield
        S.tt(S.dve, up[:], up[:], mu_b, ALU.mult, R=[up, rvt], W=[up])
        yield
        S.tt(S.dve, up[:], up[:], ua[:], ALU.add, R=[up, ua], W=[up])
        yield
        um = up
        r_, k_, v_ = um[:, 0:512], um[:, 512:1024], um[:, 1024:1536]
        pl_ = rr(G.ps, st, "ps")
        S.tr([(pl_[:, 0:128], um[:, 1536:1664])], G.ident_f[:], R=[um, G.ident_f], W=[pl_])
        S.actf(X.lw[0:64, :], pl_[0:64, 0:128], AF.Tanh, R=[pl_], W=[X.lw])
        S.cp(S.act, X.lw[64:128, :], pl_[64:128, 0:128], R=[pl_], W=[X.lw])
        S.cp(S.act, O.v_b[:], v_, R=[um], W=[O.v_b])
        S.actf(O.sz[:], za[:], AF.Silu, R=[za], W=[O.sz])
        yield
        pzw = rr(G.ps, st, "ps"); pza = rr(G.ps, st, "ps")
        S.mm(pzw[:], [(X.lw[0:64, :], w2a2[0:64, :]), (G.ones_f[0:1, :], w0a0[0:1, 0:512])], R=[X.lw, w2a2, w0a0, G.ones_f], W=[pzw])
        S.mm(pza[:], [(X.lw[64:128, :], w2a2[64:128, :]), (G.ones_f[0:1, :], w0a0[0:1, 512:1024])], R=[X.lw, w2a2, w0a0, G.ones_f], W=[pza])
        S.actf(X.sgw[:], pzw[:], AF.Sigmoid, R=[pzw], W=[X.sgw])
        S.actf(X.av[:], pza[:], AF.Sigmoid, R=[pza], W=[X.av])
        yield
        pci = rr(G.ps, st, "ps"); pce = rr(G.ps, st, "ps"); pgc = rr(G.ps, st, "ps")
        S.mm(pci[:], [(M_incl, X.sgw[:])], R=[G.masks, X.sgw], W=[pci])
        S.mm(pce[:], [(M_strict, X.sgw[:])], R=[G.masks, X.sgw], W=[pce])
        sgw = X.sgw
        S.group(S.pe, [(lambda h=h: nc.tensor.matmul(pgc[0:64, h:h + 1], lhsT=sgw[:, h * 64:(h + 1) * 64], rhs=G.ones_f[:, 0:1],
                                                      start=True, stop=True)) for h in range(8)], [X.sgw, G.ones_f], [pgc])
        yield
        S.actf(X.gam[:], pci[:], AF.Exp, R=[pci], W=[X.gam], scale=-CDEC)
        S.actf(X.ig[:], pci[:], AF.Exp, R=[pci], W=[X.ig], scale=CDEC)
        S.actf(X.gae[:], pce[:], AF.Exp, R=[pce], W=[X.gae], scale=-CDEC)
        S.actf(O.gC[:], pgc[0:64, 0:8], AF.Exp, R=[pgc], W=[O.gC], scale=-CDEC)
        yield
        ssq = X.ssq
        S.tt(S.dve, X.kk[:], k_, kk_b, ALU.mult, R=[um, rvt], W=[X.kk])
        S.actf(X.sqt[:], X.kk[:], AF.Square, R=[X.kk], W=[X.sqt])
        yield
        S.op(S.dve, lambda: nc.vector.tensor_reduce(out=ssq[:, 0:8], in_=h3(X.sqt[:]), axis=AX.X, op=ALU.add), [X.sqt], [ssq])
        S.actf(ssq[:, 8:16], ssq[:, 0:8], AF.Sqrt, R=[ssq], W=[ssq])
        S.ts(S.dve, ssq[:, 8:16], ssq[:, 8:16], 1e-12, op0=ALU.max, R=[ssq], W=[ssq])
        S.op(S.dve, lambda: nc.vector.reciprocal(out=ssq[:, 16:24], in_=ssq[:, 8:16]), [ssq], [ssq])
        yield
        S.tt(S.dve, h3(X.kkn[:]), h3(X.kk[:]), bc_last(ssq[:, 16:24], 64), ALU.mult, R=[X.kk, ssq], W=[X.kkn])
        yield
        S.stt(S.dve, X.e1[:], X.av[:], -1.0, ka_b, ALU.add, ALU.mult, R=[X.av, rvt], W=[X.e1])
        yield
        S.stt(S.dve, X.knew[:], X.e1[:], 1.0, k_, ALU.add, ALU.mult, R=[X.e1, um], W=[X.knew])
        yield
        S.tt(S.dve, O.kap_b[:], X.kkn[:], X.gae[:], ALU.mult, R=[X.kkn, X.gae], W=[O.kap_b])
        yield
        S.tt(S.dve, O.ktl_b[:], X.knew[:], X.ig[:], ALU.mult, R=[X.knew, X.ig], W=[O.ktl_b])
        yield
        S.tt(S.dve, X.e1[:], X.kkn[:], X.av[:], ALU.mult, R=[X.kkn, X.av], W=[X.e1])
        yield
        S.tt(S.dve, O.btl_b[:], X.e1[:], X.ig[:], ALU.mult, R=[X.e1, X.ig], W=[O.btl_b])
        yield
        S.tt(S.dve, O.rtl_b[:], r_, X.gam[:], ALU.mult, R=[um, X.gam], W=[O.rtl_b])
        yield
        S.tt(S.dve, X.rkt[:], r_, X.knew[:], ALU.mult, R=[um, X.knew], W=[X.rkt])
        yield
        S.tt(S.dve, X.rkt[:], X.rkt[:], rk_b, ALU.mult, R=[X.rkt, rvt], W=[X.rkt])
        S.op(S.dve, lambda: nc.vector.tensor_reduce(out=ssq[:, 24:32], in_=h3(X.rkt[:]), axis=AX.X, op=ALU.add), [X.rkt], [ssq])
        yield
        S.tt(S.dve, h3(O.bon[:]), h3(v_), bc_last(ssq[:, 24:32], 64), ALU.mult, R=[um, ssq], W=[O.bon])
        yield

    def stageA2(c):
        O = OS[c % 3]; Z = ZS[c % 2]
        for (src, dst, wi) in ((O.kap_b, Z.KR, 0), (O.rtl_b, Z.KR, 1), (O.ktl_b, Z.KB, 0), (O.btl_b, Z.KB, 1)):
            pb = rr(G.psb, st, "psb")
            S.tr([(pb[0:64, h * 128:(h + 1) * 128], src[:, h * 64:(h + 1) * 64]) for h in range(8)], G.ident_bf[:],
                 R=[src, G.ident_bf], W=[pb])
            S.cp(S.act, dst[:, :, wi, :], pb[0:64, :].rearrange("p (h t) -> p h t", h=8), R=[pb], W=[dst])
            yield
        KR, KB, AK, AB2 = Z.KR, Z.KB, Z.AK, Z.AB2
        Q, QT, Y = Z.Qs[0], Z.QTs[0], Z.Ys[0]
        for hp in range(4):
            p1 = rr(G.ps, st, "ps"); p2 = rr(G.ps, st, "ps")
            for hh in range(2):
                h = hp * 2 + hh
                S.mm(p1[:, hh * 256:(hh + 1) * 256], [(KB[:, h, 0, :], KR[:, h, :, :])], R=[KB, KR], W=[p1])
                S.mm(p2[:, hh * 256:(hh + 1) * 256], [(KB[:, h, 1, :], KR[:, h, :, :])], R=[KB, KR], W=[p2])
            hs = slice(hp * 2, hp * 2 + 2)
            p1v = p1[:].rearrange("p (h w t) -> p h w t", h=2, w=2)
            p2v = p2[:].rearrange("p (h w t) -> p h w t", h=2, w=2)
            for hh in range(2):
                h = hp * 2 + hh
                S.tt(S.dve, AK[:, h, :, :], p1v[:, hh, :, :], G.masks[:, 0:2, :], ALU.mult, R=[p1, G.masks], W=[AK])
            yield
            S.tt(S.dve, AB2[:, hs, :], p2v[:, :, 1, :], bc_mid(M_incl, 2), ALU.mult, R=[p2, G.masks], W=[AB2])
            S.stt(S.dve, QT[:, hs, :], p2v[:, :, 0, :], -1.0, bc_mid(M_strict, 2), ALU.mult, ALU.mult, R=[p2, G.masks], W=[QT])
            S.tt(S.dve, Y[:, hs, :], QT[:, hs, :], bc_mid(ident_i[:], 2), ALU.add, R=[QT, ident_i], W=[Y])
            yield
        for hq in range(2):
            p3 = rr(G.ps, st, "ps")
            for hh in range(4):
                h = hq * 4 + hh
                S.mm(p3[:, hh * 128:(hh + 1) * 128], [(KR[:, h, 0, :], KB[:, h, 1, :])], R=[KR, KB], W=[p3])
            hs = slice(hq * 4, hq * 4 + 4)
            S.stt(S.dve, Q[:, hs, :], p3[:].rearrange("p (h t) -> p h t", h=4), -1.0, bc_mid(M_lower, 4), ALU.mult, ALU.mult,
                  R=[p3, G.masks], W=[Q])
            yield
        k_ = 0
        for lev in range(1, 7):
            k_ ^= 1
            Qn, QTn, Yn = Z.Qs[k_], Z.QTs[k_], Z.Ys[k_]
            last = lev == 6
            for hq in range(2):
                hs = slice(hq * 4, hq * 4 + 4)
                pq = rr(G.ps, st, "ps")
                for hh in range(4):
                    h = hq * 4 + hh
                    S.mm(pq[:, hh * 128:(hh + 1) * 128], [(QT[:, h, :], Q[:, h, :])], R=[QT, Q], W=[pq])
                S.cp(S.act, Qn[:, hs, :], pq[:].rearrange("p (h t) -> p h t", h=4), R=[pq], W=[Qn])
                if not last:
                    pqt = rr(G.ps, st, "ps")
                    for hh in range(4):
                        h = hq * 4 + hh
                        S.mm(pqt[:, hh * 128:(hh + 1) * 128], [(Q[:, h, :], QT[:, h, :])], R=[QT, Q], W=[pqt])
                    S.cp(S.act, QTn[:, hs, :], pqt[:].rearrange("p (h t) -> p h t", h=4), R=[pqt], W=[QTn])
                yield
            for hq in range(2):
                hs = slice(hq * 4, hq * 4 + 4)
                py = rr(G.ps, st, "ps")
                for hh in range(4):
                    h = hq * 4 + hh
                    S.mm(py[:, hh * 128:(hh + 1) * 128], [(Qn[:, h, :], Y[:, h, :])], R=[Qn, Y], W=[py])
                S.tt(S.dve, Yn[:, hs, :], py[:].rearrange("p (h t) -> p h t", h=4), Y[:, hs, :], ALU.add, R=[py, Y], W=[Yn])
                yield
            Q, QT, Y = Qn, QTn, Yn
        Z.Yfin = Y

    def stageB(c):
        O = OS[c % 3]; Z = ZS[c % 2]
        KR, AK, AB2, Y = Z.KR, Z.AK, Z.AB2, Z.Yfin
        v_b, negU, R_sb, ssb = O.v_b, Bt.negU, Bt.R_sb, Bt.ssb
        rows = slice(c * 128, (c + 1) * 128)
        pR = rr(G.ps, st, "ps")
        for h in range(8):
            cs_ = slice(h * 64, (h + 1) * 64)
            S.mm(pR[:, cs_], [(KR[:, h, 0, :], Hb[:, cs_]), (AK[:, h, 0, :], v_b[:, cs_])], R=[KR, Hb, AK, v_b], W=[pR])
        S.cp(S.act, R_sb[:], pR[:], R=[pR], W=[R_sb])
        yield
        pU = rr(G.ps, st, "ps")
        for h in range(8):
            cs_ = slice(h * 64, (h + 1) * 64)
            S.mm(pU[:, cs_], [(Y[:, h, :], R_sb[:, cs_])], R=[Y, R_sb], W=[pU])
        S.actf(negU[:], pU[:], AF.Copy, R=[pU], W=[negU], scale=-1.0)
        yield
        pH = rr(G.ps, st, "ps")
        for h in range(8):
            cs_ = slice(h * 64, (h + 1) * 64)
            S.mm(pH[0:64, cs_], [(O.ktl_b[:, cs_], v_b[:, cs_]), (O.btl_b[:, cs_], negU[:, cs_])], R=[O.ktl_b, v_b, O.btl_b, negU], W=[pH])
        pY = rr(G.ps, st, "ps")
        for h in range(8):
            cs_ = slice(h * 64, (h + 1) * 64)
            S.mm(pY[:, cs_], [(KR[:, h, 1, :], Hb[:, cs_]), (AK[:, h, 1, :], v_b[:, cs_]), (AB2[:, h, :], negU[:, cs_])],
                 R=[KR, Hb, AK, v_b, AB2, negU], W=[pY])
        S.cp(S.act, Bt.yt[:], pY[:], R=[pY], W=[Bt.yt])
        S.tt(S.dve, Htmp[:], pH[0:64, :], Hf[:], ALU.add, R=[pH, Hf], W=[Htmp])
        S.tt(S.dve, h3(Hf[:]), h3(Htmp[:]), bc_last(O.gC[:], 64), ALU.mult, R=[Htmp, O.gC], W=[Hf])
        S.cp(S.act, Hb[:], Hf[:], R=[Hf], W=[Hb])
        yield
        yt, yc, sq2 = Bt.yt, Bt.yc, Bt.sq2
        S.op(S.dve, lambda: nc.vector.tensor_reduce(out=ssb[:, 8:16], in_=h3(yt[:]), axis=AX.X, op=ALU.add), [yt], [ssb])
        S.ts(S.dve, ssb[:, 8:16], ssb[:, 8:16], -1.0 / 64, R=[ssb], W=[ssb])
        S.tt(S.dve, h3(yc[:]), h3(yt[:]), bc_last(ssb[:, 8:16], 64), ALU.add, R=[yt, ssb], W=[yc])
        S.actf(sq2[:], yc[:], AF.Square, R=[yc], W=[sq2])
        yield
        S.op(S.dve, lambda: nc.vector.tensor_reduce(out=ssb[:, 16:24], in_=h3(sq2[:]), axis=AX.X, op=ALU.add), [sq2], [ssb])
        S.actf(ssb[:, 24:32], ssb[:, 16:24], AF.Sqrt, R=[ssb], W=[ssb], scale=1.0 / 64, bias=G.eps_col[:, 1:2])
        S.op(S.dve, lambda: nc.vector.reciprocal(out=ssb[:, 16:24], in_=ssb[:, 24:32]), [ssb], [ssb])
        S.tt(S.dve, h3(yc[:]), h3(yc[:]), bc_last(ssb[:, 16:24], 64), ALU.mult, R=[yc, ssb], W=[yc])
        yield
        S.tt(S.dve, yc[:], yc[:], lg_b, ALU.mult, R=[yc, rvt], W=[yc])
        S.tt(S.dve, yc[:], yc[:], lb_b, ALU.add, R=[yc, rvt], W=[yc])
        yield
        S.tt(S.dve, yc[:], yc[:], O.bon[:], ALU.add, R=[yc, O.bon], W=[yc])
        ygo = Bt.ygo[c % 2]
        S.tt(S.dve, ygo[:], yc[:], O.sz[:], ALU.mult, R=[yc, O.sz], W=[ygo])
        S.dma(S.q_sync, yga[rows, :], ygo[:], R=[ygo], W=[yga])
        yield

    def drain(g):
        for _ in g:
            pass

    def step(g):
        if g is None:
            return None
        try:
            next(g)
            return g
        except StopIteration:
            return None

    drain(stageA1(0))
    if nch > 1:
        drain(stageA1(1))
    drain(stageA2(0))
    for c in range(nch):
        gB = stageB(c)
        gA2 = stageA2(c + 1) if c + 1 < nch else None
        gA1 = stageA1(c + 2) if c + 2 < nch else None
        while gB is not None or gA2 is not None or gA1 is not None:
            gA2 = step(gA2)
            gA1 = step(gA1)
            gA2 = step(gA2)
            gA1 = step(gA1)
            gA2 = step(gA2)
            gB = step(gB)
    sc.close()


def phase_out(S, C, G, l, hsrc, hdst, pbT, yga, ybT, ycT, T, final):
    nc = S.nc
    sc = Scope(S)
    st = {}
    nblk = T // 512
    cvt = sc.sb("p5_cv", [128, NCV])
    S.dma(S.q_sync, cvt[:], C.cv[l], W=[cvt])
    wo = sc.sb("p5_wo", [128, 12, 1024], BF16)
    sc2 = Scope(S)
    stg = [sc2.sb(f"p5_stg{i}", [128, 1024]) for i in range(2)]
    for k in range(12):
        sg = stg[k % 2]
        S.dma(S.q_sync if k % 2 == 0 else S.q_pool, sg[:], C.w_out[l][k * 128:(k + 1) * 128, :], W=[sg])
        S.cp(S.dve if k % 2 == 0 else S.act, wo[:, k, :], sg[:], R=[sg], W=[wo])
    sc2.close()
    if final:
        fg = sc.sb("p5_fg", [128, D])
        S.dma(S.q_sync, fg[:], C.final_g[0:1, :].partition_broadcast(128), W=[fg])
        junk = sc.sb("p5_junk", [128, D], BF16)
        ssf = [sc.sb(f"p5_ssf{i}", [128, 4]) for i in range(2)]
    ygT = [sc.sb(f"p5_ygT{i}", [128, 12, 512], BF16) for i in range(2)]
    ybs = [sc.sb(f"p5_yb{i}", [128, 512]) for i in range(5)]
    zbs = [sc.sb(f"p5_zb{i}", [128, 512]) for i in range(3)]
    sqs = [sc.sb(f"p5_sq{i}", [128, 512]) for i in range(2)]
    rstd = [sc.sb(f"p5_rstd{i}", [128, 512]) for i in range(2)]
    t1s = [sc.sb(f"p5_t1{i}", [128, 512]) for i in range(2)]
    t2s = [sc.sb(f"p5_t2{i}", [128, 512]) for i in range(2)]
    yas = [sc.sb(f"p5_ya{i}", [128, 512]) for i in range(2)]
    yabs = [sc.sb(f"p5_yab{i}", [128, 512], BF16) for i in range(2)]
    hts = [sc.sb(f"p5_h{i}", [128, D]) for i in range(2)]
    hos = [sc.sb(f"p5_ho{i}", [128, D]) for i in range(2)]
    def fin(j):
        blk = slice(j * 512, (j + 1) * 512)
        yg = ygT[j % 2]
        for (src, zoff, goff, kbase) in ((ybT, OB_ZB, CV_LOG, 4), (ycT, OB_ZC, CV_MOG, 8)):
            ys = []
            pS = rr(G.ps, st, "ps")
            for ct in range(4):
                yb = rr(ybs, st, "yb"); ys.append(yb)
                S.dma(S.q_sync, yb[:], src[ct * 128:(ct + 1) * 128, blk], R=[src], W=[yb])
                sq = rr(sqs, st, "sq")
                S.actf(sq[:], yb[:], AF.Square, R=[yb], W=[sq])
                S.mm(pS[:], [(G.ones_f[:], sq[:])], R=[G.ones_f, sq], W=[pS], start=(ct == 0), stop=(ct == 3))
            rs = rr(rstd, st, "rstd")
            S.actf(rs[:], pS[:], AF.Ln, R=[pS], W=[rs], scale=1.0 / 512, bias=G.eps_col[:, 0:1])
            S.actf(rs[:], rs[:], AF.Exp, R=[rs], W=[rs], scale=-0.5)
            for ct in range(4):
                zb = rr(zbs, st, "zb")
                S.dma(S.q_pool, zb[:], pbT[zoff + ct * 128:zoff + (ct + 1) * 128, blk], R=[pbT], W=[zb])
                t1 = rr(t1s, st, "t1"); t2 = rr(t2s, st, "t2")
                S.actf(t1[:], zb[:], AF.Silu, R=[zb], W=[t1])
                S.stt(S.dve, t2[:], ys[ct][:], cvt[:, goff + ct:goff + ct + 1], rs[:], ALU.mult, ALU.mult, R=[ys[ct], cvt, rs], W=[t2])
                S.tt(S.dve, yg[:, kbase + ct, :], t1[:], t2[:], ALU.mult, R=[t1, t2], W=[yg])
        for tt in range(4):
            t = 4 * j + tt
            ya = rr(yas, st, "ya"); yab = rr(yabs, st, "yab")
            S.dma(S.q_sync, ya[:], yga[t * 128:(t + 1) * 128, :], R=[yga], W=[ya])
            S.cp(S.act, yab[:], ya[:], R=[ya], W=[yab])
            pb = rr(G.psb, st, "psb")
            S.tr([(pb[:, ct * 128:(ct + 1) * 128], yab[:, ct * 128:(ct + 1) * 128]) for ct in range(4)], G.ident_bf[:],
                 R=[yab, G.ident_bf], W=[pb])
            S.cp(S.act, yg[:, 0:4, tt * 128:(tt + 1) * 128], pb[:, 0:512].rearrange("p (c t) -> p c t", c=4), R=[pb], W=[yg])

    def outproj(j):
        yg = ygT[j % 2]
        for tt in range(4):
            t = 4 * j + tt
            ht = rr(hts, st, "h"); ho = rr(hos, st, "ho")
            S.dma(S.q_sync, ht[:], hsrc[t * 128:(t + 1) * 128, :], R=[hsrc], W=[ht])
            for half in range(2):
                cs_ = slice(half * 512, (half + 1) * 512)
                pO = rr(G.ps, st, "ps")
                S.mm(pO[:], [(yg[:, k, tt * 128:(tt + 1) * 128], wo[:, k, cs_]) for k in range(12)], R=[yg, wo], W=[pO])
                S.tt(S.dve, ho[:, cs_], pO[:], ht[:, cs_], ALU.add, R=[pO, ht], W=[ho])
            if final:
                s_ = rr(ssf, st, "ssf")
                S.actf(junk[:], ho[:], AF.Square, R=[ho], W=[junk, s_], accum=s_[:, 0:1])
                S.actf(s_[:, 1:2], s_[:, 0:1], AF.Sqrt, R=[s_], W=[s_], scale=1.0 / D, bias=G.eps_col[:, 0:1])
                S.op(S.dve, lambda a=s_: nc.vector.reciprocal(out=a[:, 2:3], in_=a[:, 1:2]), [s_], [s_])
                S.stt(S.dve, ht[:], ho[:], s_[:, 2:3], fg[:], ALU.mult, ALU.mult, R=[ho, s_, fg], W=[ht])
                S.dma(S.q_pool, hdst[t * 128:(t + 1) * 128, :], ht[:], R=[ht], W=[hdst])
            else:
                S.dma(S.q_pool, hdst[t * 128:(t + 1) * 128, :], ho[:], R=[ho], W=[hdst])

    fin(0)
    for j in range(nblk):
        if j + 1 < nblk:
            fin(j + 1)
        outproj(j)
    sc.close()


def build(T, L, phases=None, debug=False):
    nc = bass.Bass("TRN2", target_bir_lowering=False)
    S = Sched(nc)
    C = declare_inputs(S, T, L)
    G = Ctx()
    load_consts(S, C, G)
    G.eps_col = S.sb("eps_col", [128, 2])
    S.memset(S.dve, G.eps_col[:, 0:1], EPS, W=[G.eps_col])
    S.memset(S.dve, G.eps_col[:, 1:2], GN_EPS, W=[G.eps_col])
    full = phases is None
    if full:
        phases = ("p1", "p2", "p3", "p4", "p5")
    dbg = lambda name: "ExternalOutput" if (debug and name in phases) else "Internal"
    pa = S.dram("pa", [T, NA], F32, kind=dbg("p1"))
    pbT = S.dram("pbT", [NB, T], F32, kind=dbg("p1"))
    ybT = S.dram("ybT", [512, T], F32, kind=dbg("p3"))
    ycT = S.dram("ycT", [512, T], F32, kind=dbg("p4"))
    yga = S.dram("yga", [T, 512], F32, kind=dbg("p2"))
    hb = [S.dram(f"hbuf{i}", [T, D], F32) for i in range(2)]
    hout = S.dram("hout", [T, D], F32, kind="ExternalOutput" if (full or "p5" in phases) else "Internal")
    outs = []
    nl = L if full else 1
    for l in range(nl):
        hsrc = C.x if l == 0 else hb[(l - 1) % 2]
        last = l == nl - 1
        hdst = hout if last else hb[l % 2]
        phase_in_proj(S, C, G, l, hsrc, pa, pbT, T)
        if "p2" in phases: phase_rwkv(S, C, G, l, pa, yga, T)
        if "p3" in phases: phase_lru(S, C, G, l, pbT, ybT, T)
        if "p4" in phases: phase_mla(S, C, G, l, pbT, ycT, T)
        if "p5" in phases: phase_out(S, C, G, l, hsrc, hdst, pbT, yga, ybT, ycT, T, final=(full and last))
    if debug:
        for nm, b in (("p1", pa), ("p1", pbT), ("p3", ybT), ("p4", ycT), ("p2", yga)):
            if nm in phases: outs.append(b)
    if full or "p5" in phases: outs.append(hout)
    S.finish(outs)
    emit_all(S)
    return nc, S

from concourse.bass_utils import run_bass_kernel_spmd

T_FULL = 4096
L_FULL = 4
_IN_NAMES = ["x", "wA", "wB", "rv", "w0a0", "w2a2", "cv", "gw", "wq", "wqs", "wk", "wv", "w_out", "final_g",
             "ident_bf", "ident_f", "cs", "masks", "mask_bf"]
_NC_CACHE = {}


def kernel(**inputs):
    x = np.asarray(inputs["x"], np.float32)
    B, T, _ = x.shape
    L = np.asarray(inputs["w_in"]).shape[0]
    hp = host_prep(inputs, T)
    key = (T, L)
    if key not in _NC_CACHE:
        _NC_CACHE[key] = build(T, L)[0]
    nc = _NC_CACHE[key]
    in_maps = []
    for b in range(B):
        m = {k: hp[k] for k in _IN_NAMES if k != "x"}
        m["x"] = np.ascontiguousarray(x[b])
        in_maps.append(m)
    res = run_bass_kernel_spmd(nc, in_maps, core_ids=list(range(B)))
    return np.stack([np.asarray(r["hout"], np.float32) for r in res.results], axis=0)
```

```python
import numpy as np
import concourse.bass as bass
import concourse.mybir as mybir

F32 = mybir.dt.float32
BF16 = mybir.dt.bfloat16
ALU = mybir.AluOpType
AF = mybir.ActivationFunctionType
AX = mybir.AxisListType


class Sem:
    def __init__(self, nc, name):
        self.h = nc.alloc_semaphore(name) if hasattr(nc, "alloc_semaphore") else None
        self.name = name
        self.count = 0


class Buf:
    __slots__ = ("t", "name", "last_write", "reads")

    def __init__(self, t, name):
        self.t = t
        self.name = name
        self.last_write = None
        self.reads = {}

    def __getitem__(self, idx):
        return self.t[idx]


SEM_LIMIT = 8000


class Eng:
    def cur_sem(self, i=0):
        sem = self.sems[i]
        if sem.count >= SEM_LIMIT:
            self.nrot = getattr(self, "nrot", 0) + 1
            sem = self.S.new_sem(f"{self.name}_r{self.nrot}")
            self.sems[i] = sem
        return sem

    def __init__(self, S, name, eng, is_dma=False, nsem=1):
        self.S = S
        self.name = name
        self.e = eng
        self.is_dma = is_dma
        self.sems = [S.new_sem(f"{name}_s{i}") for i in range(nsem)]
        self.rr = 0
        self.known = {}
        self.prog = []

    def wait(self, tok):
        sem, val = tok
        if self.known.get(id(sem), 0) >= val:
            return
        e = self.e; h = sem.h
        self.prog.append(lambda: e.wait_ge(h, val))
        self.known[id(sem)] = val
        self.S.nwaits += 1


class Sched:
    def __init__(self, nc):
        self.nc = nc
        import contextlib
        self.es = contextlib.ExitStack()
        self.nwaits = 0
        self.ninst = 0
        self.sem_list = []
        self.pe = Eng(self, "pe", nc.tensor)
        self.dve = Eng(self, "dve", nc.vector)
        self.act = Eng(self, "act", nc.scalar)
        self.pool = Eng(self, "pool", nc.gpsimd)
        self.q_sync = Eng(self, "qsync", nc.sync, is_dma=True, nsem=8)
        self.q_pool = Eng(self, "qpool", nc.gpsimd, is_dma=True, nsem=4)
        self.q_pool.prog = self.pool.prog
        self.engines = [self.pe, self.dve, self.act, self.pool, self.q_sync]

    def new_sem(self, name):
        s = Sem.__new__(Sem)
        s.is_pe = name.startswith("pe_")
        s.name = name
        s.count = 0
        s.h = self.es.enter_context(self.nc.semaphore(name))
        self.sem_list.append(s)
        return s

    def sb(self, name, shape, dtype=F32):
        t = self.nc.alloc_sbuf_tensor(name, list(shape), dtype)
        return Buf(t, name)

    def ps(self, name, shape, dtype=F32):
        t = self.nc.alloc_psum_tensor(name, list(shape), dtype)
        return Buf(t, name)

    def dram(self, name, shape, dtype=F32, kind="Internal"):
        t = self.nc.dram_tensor(name, list(shape), dtype, kind=kind)
        return Buf(t, name)

    def _deps(self, reads, writes):
        deps = []
        for b in reads:
            if b.last_write is not None:
                deps.append(b.last_write)
        for b in writes:
            if b.last_write is not None:
                deps.append(b.last_write)
            deps.extend(b.reads.values())
        return deps

    def _commit(self, tok, reads, writes):
        for b in reads:
            if b not in writes:
                b.reads[id(tok[0])] = tok
        for b in writes:
            b.last_write = tok
            b.reads = {}

    def op(self, eng, fn, reads=(), writes=()):
        for tok in self._deps(reads, writes):
            if eng is self.pe and tok[0].is_pe:
                continue
            eng.wait(tok)
        sem = eng.cur_sem()
        sem.count += 1
        h = sem.h
        eng.prog.append(lambda: fn().then_inc(h, 1))
        tok = (sem, sem.count)
        self._commit(tok, reads, writes)
        self.ninst += 1
        return tok

    def group(self, eng, fns, reads=(), writes=()):
        for tok in self._deps(reads, writes):
            if eng is self.pe and tok[0].is_pe:
                continue
            eng.wait(tok)
        fns = list(fns)
        for fn in fns[:-1]:
            eng.prog.append(fn)
            self.ninst += 1
        self.ninst += 1
        sem = eng.cur_sem()
        sem.count += 1
        h = sem.h
        last = fns[-1]
        eng.prog.append(lambda: last().then_inc(h, 1))
        tok = (sem, sem.count)
        self._commit(tok, reads, writes)
        return tok

    def dma(self, q, out_ap, in_ap, R=(), W=(), **kw):
        i = q.rr % len(q.sems)
        sem = q.sems[i]
        q.rr += 1
        if sem.count > 0:
            q.wait((sem, sem.count))
        sem = q.cur_sem(i)
        for tok in self._deps(R, W):
            q.wait(tok)
        sem.count += 16
        h = sem.h; e = q.e
        q.prog.append(lambda: e.dma_start(out=out_ap, in_=in_ap, **kw).then_inc(h, 16))
        tok = (sem, sem.count)
        self._commit(tok, R, W)
        self.ninst += 1
        return tok

    def ts(self, eng, out, in0, s1, s2=None, op0=ALU.mult, op1=None, R=(), W=(), accum=None):
        e = eng.e
        kw = {}
        if op1 is not None: kw["op1"] = op1
        if accum is not None: kw["accum_out"] = accum
        return self.op(eng, lambda: e.tensor_scalar(out=out, in0=in0, scalar1=s1, scalar2=s2, op0=op0, **kw), R, W)

    def tt(self, eng, out, in0, in1, op, R=(), W=()):
        e = eng.e
        return self.op(eng, lambda: e.tensor_tensor(out=out, in0=in0, in1=in1, op=op), R, W)

    def stt(self, eng, out, in0, scalar, in1, op0, op1, R=(), W=()):
        e = eng.e
        return self.op(eng, lambda: e.scalar_tensor_tensor(out=out, in0=in0, scalar=scalar, in1=in1, op0=op0, op1=op1), R, W)

    def cp(self, eng, out, in_, R=(), W=()):
        e = eng.e
        if eng is self.act:
            return self.op(eng, lambda: e.copy(out=out, in_=in_), R, W)
        return self.op(eng, lambda: e.tensor_copy(out=out, in_=in_), R, W)

    def actf(self, out, in_, func, R=(), W=(), scale=1.0, bias=None, accum=None):
        e = self.act.e
        kw = {}
        if bias is not None: kw["bias"] = bias
        if accum is not None: kw["accum_out"] = accum
        return self.op(self.act, lambda: e.activation(out=out, in_=in_, func=func, scale=scale, **kw), R, W)

    def mm(self, out, pairs, R=(), W=(), start=True, stop=True, **kw):
        e = self.pe.e
        n = len(pairs)
        fns = []
        for i, (l, r) in enumerate(pairs):
            st = start and i == 0
            sp = stop and i == n - 1
            fns.append((lambda l=l, r=r, st=st, sp=sp: e.matmul(out, lhsT=l, rhs=r, start=st, stop=sp, **kw)))
        return self.group(self.pe, fns, R, W)

    def tr(self, outs_ins, ident, R=(), W=()):
        e = self.pe.e
        fns = [(lambda o=o, i=i: e.transpose(out=o, in_=i, identity=ident)) for (o, i) in outs_ins]
        return self.group(self.pe, fns, R, W)

    def memset(self, eng, ap, val, W=()):
        e = eng.e
        return self.op(eng, lambda: e.memset(ap, val), (), W)

    def barrier(self):
        toks = [(s, s.count) for s in self.sem_list if s.count > 0]
        for eng in [self.pe, self.dve, self.act, self.pool, self.q_sync]:
            for tok in toks:
                eng.wait(tok)

    def finish(self, bufs):
        for b in bufs:
            if b.last_write is not None:
                self.q_sync.wait(b.last_write)


def emit_all(S):
    nc = S.nc
    def run(prog):
        def f(e):
            for g in prog: g()
        return f
    with nc.Block() as block:
        if S.q_sync.prog: block.sync(run(S.q_sync.prog))
        if S.pe.prog: block.tensor(run(S.pe.prog))
        if S.dve.prog: block.vector(run(S.dve.prog))
        if S.act.prog: block.scalar(run(S.act.prog))
        if S.pool.prog: block.gpsimd(run(S.pool.prog))
    S.es.close()


def _prune(reads):
    best = {}
    for sem, val in reads:
        k = id(sem)
        if k not in best or best[k][1] < val:
            best[k] = (sem, val)
    return list(best.values())

D = 1024
GW = 512
NH = 8
HD = 64
RW_IN = 1664
NA = 2176
NB = 1984
QL = 256
KVL = 128
DMIX = 1536

OB_UB, OB_ZB, OB_QL, OB_KV, OB_ZC, OB_KR, OB_KRS = 0, 512, 1024, 1280, 1408, 1920, 1952

RV_MU, RV_KK, RV_KA, RV_RK, RV_LG, RV_LB = 0, 1664, 2176, 2688, 3200, 3712
NRV = 4224
CV_LNG = 0
CV_CW = 8
CV_CB = 24
CV_GAB = 28
CV_GXB = 32
CV_LAM = 36
CV_LOG = 40
CV_QNG = 44
CV_KVNG = 46
CV_MOG = 47
NCV = 52


def host_prep(inp, T):
    f = np.float32
    L = inp["w_in"].shape[0]
    out = {}
    w_in = np.asarray(inp["w_in"], f)
    out["wA"] = np.ascontiguousarray(w_in[:, :, 0:NA])
    c = 2176
    colsB = np.concatenate([
        np.arange(c, c + 512), np.arange(c + 512, c + 1024), np.arange(3200, 3456), np.arange(3456, 3584),
        np.arange(3616, 4128), np.arange(3584, 3616), np.arange(3600, 3616), np.arange(3584, 3600)])
    assert colsB.size == NB
    out["wB"] = np.ascontiguousarray(w_in[:, :, colsB])
    rv = np.zeros((L, NRV), f)
    rv[:, RV_MU:RV_MU + 1664] = inp["rwkv_mu"]
    rv[:, RV_KK:RV_KK + 512] = inp["rwkv_k_k"]
    rv[:, RV_KA:RV_KA + 512] = inp["rwkv_k_a"]
    rv[:, RV_RK:RV_RK + 512] = np.asarray(inp["rwkv_r_k"], f).reshape(L, 512)
    rv[:, RV_LG:RV_LG + 512] = inp["rwkv_lnx_g"]
    rv[:, RV_LB:RV_LB + 512] = inp["rwkv_lnx_b"]
    out["rv"] = rv
    out["w0a0"] = np.ascontiguousarray(np.concatenate([np.asarray(inp["rwkv_w0"], f), np.asarray(inp["rwkv_a0"], f)], axis=1))
    out["w2a2"] = np.ascontiguousarray(np.concatenate([np.asarray(inp["rwkv_w2"], f), np.asarray(inp["rwkv_a2"], f)], axis=1))
    cv = np.zeros((L, 128, NCV), f)
    def colfill(off, vec, n):
        cv[:, :, off:off + n] = np.asarray(vec, f).reshape(L, n, 128).transpose(0, 2, 1)
    colfill(CV_LNG, inp["ln_g"], 8)
    cw = np.asarray(inp["lru_conv_w"], f)
    for j in range(4):
        colfill(CV_CW + j * 4, cw[:, j], 4)
    colfill(CV_CB, inp["lru_conv_b"], 4)
    colfill(CV_GAB, inp["lru_ga_b"], 4)
    colfill(CV_GXB, inp["lru_gx_b"], 4)
    colfill(CV_LAM, inp["lru_lam"], 4)
    colfill(CV_LOG, inp["lru_out_g"], 4)
    colfill(CV_QNG, inp["mla_q_norm_g"], 2)
    colfill(CV_KVNG, inp["mla_kv_norm_g"], 1)
    colfill(CV_MOG, inp["mla_out_g"], 4)
    out["cv"] = cv
    gw = np.zeros((L, 4, 128, 2, 128), f)
    for gi, nm in enumerate(["lru_ga_w", "lru_gx_w"]):
        w = np.asarray(inp[nm], f)
        for h in range(8):
            ct, o = h // 2, (h % 2) * 64
            gw[:, ct, o:o + 64, gi, o:o + 64] = w[:, h]
    out["gw"] = gw
    wuq = np.asarray(inp["mla_w_uq"], f).reshape(L, 256, 8, 96)
    wq = np.zeros((L, 256, 8, 128), f)
    wq[..., 0:32] = wuq[..., 64:96]
    wq[..., 64:128] = wuq[..., 0:64]
    out["wq"] = np.ascontiguousarray(wq.reshape(L, 256, 1024))
    wqs = np.zeros((L, 256, 8, 128), f)
    wqs[..., 0:16] = wuq[..., 80:96]
    wqs[..., 16:32] = wuq[..., 64:80]
    out["wqs"] = np.ascontiguousarray(wqs.reshape(L, 256, 1024))
    wukv = np.asarray(inp["mla_w_ukv"], f).reshape(L, 128, 8, 128)
    wk = np.zeros((L, 128, 8, 128), f)
    wk[..., 64:128] = wukv[..., 0:64]
    out["wk"] = np.ascontiguousarray(wk.reshape(L, 128, 1024))
    out["wv"] = np.ascontiguousarray(wukv[..., 64:128].reshape(L, 128, 512))
    out["w_out"] = np.asarray(inp["w_out"], f)
    out["final_g"] = np.asarray(inp["final_g"], f).reshape(1, 1024)
    import ml_dtypes
    bf = ml_dtypes.bfloat16
    out["ident_bf"] = np.eye(128, dtype=f).astype(bf)
    out["ident_f"] = np.eye(128, dtype=f)
    half = 16
    inv_freq = (10000.0 ** (-np.arange(half, dtype=f) * 2.0 / 32)).astype(f)
    ang = np.arange(T, dtype=f)[:, None] * inv_freq[None, :]
    cos, sin = np.cos(ang).astype(f).T, np.sin(ang).astype(f).T
    cs = np.zeros((128, 2, T), f)
    cs[32:128, 0] = 1.0
    cs[0:16, 0], cs[16:32, 0] = cos, cos
    cs[0:16, 1], cs[16:32, 1] = -sin, sin
    out["cs"] = cs
    ii = np.arange(128)
    m = np.zeros((128, 4, 128), f)
    m[:, 0] = (ii[:, None] < ii[None, :])
    m[:, 1] = (ii[:, None] <= ii[None, :])
    m[:, 2] = (ii[:, None] > ii[None, :])
    m[:, 3] = 1.0
    out["masks"] = m
    out["mask_bf"] = (ii[:, None] <= ii[None, :]).astype(f).astype(bf)
    return out

import contextlib

EPS = 1e-6


class Ctx:
    pass


def declare_inputs(S, T, L):
    C = Ctx()
    di = lambda n, shp, dt=F32: S.dram(n, shp, dt, kind="ExternalInput")
    C.x = di("x", [T, D])
    C.wA = di("wA", [L, D, NA]); C.wB = di("wB", [L, D, NB])
    C.rv = di("rv", [L, NRV]); C.w0a0 = di("w0a0", [L, 1024]); C.w2a2 = di("w2a2", [L, 128, 512])
    C.cv = di("cv", [L, 128, NCV]); C.gw = di("gw", [L, 4, 128, 2, 128])
    C.wq = di("wq", [L, 256, 1024]); C.wqs = di("wqs", [L, 256, 1024])
    C.wk = di("wk", [L, 128, 1024]); C.wv = di("wv", [L, 128, 512])
    C.w_out = di("w_out", [L, DMIX, D]); C.final_g = di("final_g", [1, D])
    C.ident_bf = di("ident_bf", [128, 128], BF16); C.ident_f = di("ident_f", [128, 128])
    C.cs = di("cs", [128, 2, T]); C.masks = di("masks", [128, 4, 128]); C.mask_bf = di("mask_bf", [128, 128], BF16)
    return C


class Scope:
    def __init__(self, S):
        self.S = S
        self.es = contextlib.ExitStack()

    _n = [0]

    def sb(self, name, shape, dtype=F32):
        Scope._n[0] += 1
        name = f"{name}_u{Scope._n[0]}"
        t = self.es.enter_context(self.S.nc.sbuf_tensor(name, list(shape), dtype))
        return Buf(t, name)

    def close(self):
        self.S.barrier()
        self.es.close()


def load_consts(S, C, G):
    nc = S.nc
    G.ident_bf = S.sb("ident_bf_sb", [128, 128], BF16)
    G.ident_f = S.sb("ident_f_sb", [128, 128])
    G.masks = S.sb("masks_sb", [128, 4, 128])
    G.mask_bf = S.sb("mask_bf_sb", [128, 128], BF16)
    G.ones_f = S.sb("ones_f", [128, 128])
    S.dma(S.q_sync, G.ident_bf[:], C.ident_bf[:], W=[G.ident_bf])
    S.dma(S.q_sync, G.ident_f[:], C.ident_f[:], W=[G.ident_f])
    S.dma(S.q_sync, G.masks[:], C.masks[:], W=[G.masks])
    S.dma(S.q_sync, G.mask_bf[:], C.mask_bf[:], W=[G.mask_bf])
    S.memset(S.dve, G.ones_f[:], 1.0, W=[G.ones_f])
    G.ps = [S.ps(f"ps{i}", [128, 512], F32) for i in range(6)]
    G.psb = [S.ps(f"psb{i}", [128, 1024], BF16) for i in range(2)]


def rr(lst, state, key):
    i = state.get(key, 0)
    state[key] = i + 1
    return lst[i % len(lst)]


def phase_in_proj(S, C, G, l, hsrc, pa, pbT, T):
    nc = S.nc
    sc = Scope(S)
    st = {}
    wA = sc.sb("wA_sb", [128, 8, NA], BF16)
    wB = sc.sb("wB_sb", [128, 8, NB], BF16)
    cvt = sc.sb("p1_cv", [128, NCV])
    S.dma(S.q_sync, cvt[:], C.cv[l], W=[cvt])
    stg = [sc.sb(f"p1_stg{i}", [128, NA]) for i in range(4)]
    wAv = C.wA[l].rearrange("(k p) c -> p k c", p=128)
    wBv = C.wB[l].rearrange("(k p) c -> p k c", p=128)
    n = 0
    for k in range(8):
        for (src, dst, nc_) in ((wAv, wA, NA), (wBv, wB, NB)):
            sg = stg[n % 4]
            S.dma(S.q_sync if n % 2 == 0 else S.q_pool, sg[:, 0:nc_], src[:, k, :], W=[sg])
            if n % 2 == 0:
                S.ts(S.dve, dst[:, k, :], sg[:, 0:nc_], cvt[:, CV_LNG + k:CV_LNG + k + 1], R=[sg, cvt], W=[dst])
            else:
                S.actf(dst[:, k, :], sg[:, 0:nc_], AF.Copy, R=[sg, cvt], W=[dst], scale=cvt[:, CV_LNG + k:CV_LNG + k + 1])
            n += 1
    hts = [sc.sb(f"p1_h{i}", [128, D]) for i in range(3)]
    junk = sc.sb("p1_junk", [128, D], BF16)
    xnb = [sc.sb(f"p1_xnb{i}", [128, D], BF16) for i in range(2)]
    ss = [sc.sb(f"p1_ss{i}", [128, 4]) for i in range(2)]
    xnT = [sc.sb(f"p1_xnT{i}", [128, 8, 512], BF16) for i in range(2)]
    oA = [sc.sb(f"p1_oA{i}", [128, NA]) for i in range(2)]
    oB = [sc.sb(f"p1_oB{i}", [128, 512]) for i in range(3)]
    nblk = T // 512
    ntile = T // 128
    evc = [0]
    def ev_eng():
        evc[0] += 1
        return S.act if evc[0] % 2 else S.dve

    def stageX(t):
        j, tt = t // 4, t % 4
        xT = xnT[j % 2]
        ht = rr(hts, st, "h")
        S.dma(S.q_sync, ht[:], hsrc[t * 128:(t + 1) * 128, :], R=[hsrc], W=[ht])
        s_ = rr(ss, st, "ss")
        S.actf(junk[:], ht[:], AF.Square, R=[ht], W=[junk, s_], accum=s_[:, 0:1])
        S.actf(s_[:, 1:2], s_[:, 0:1], AF.Sqrt, R=[s_], W=[s_], scale=1.0 / D, bias=G.eps_col[:, 0:1])
        S.op(S.dve, lambda a=s_: nc.vector.reciprocal(out=a[:, 2:3], in_=a[:, 1:2]), [s_], [s_])
        xb = rr(xnb, st, "xnb")
        S.ts(S.dve, xb[:], ht[:], s_[:, 2:3], R=[ht, s_], W=[xb])
        pb = rr(G.psb, st, "psb")
        S.tr([(pb[:, k * 128:(k + 1) * 128], xb[:, k * 128:(k + 1) * 128]) for k in range(8)], G.ident_bf[:],
             R=[xb, G.ident_bf], W=[pb])
        S.cp(S.act, xT[:, :, tt * 128:(tt + 1) * 128], pb[:].rearrange("p (k c) -> p k c", k=8), R=[pb], W=[xT])

    def stageM(t):
        j, tt = t // 4, t % 4
        xT = xnT[j % 2]
        o = rr(oA, st, "oA")
        c0 = 0
        while c0 < NA:
            cn = min(512, NA - c0)
            p = rr(G.ps, st, "ps")
            S.mm(p[:, 0:cn], [(xT[:, k, tt * 128:(tt + 1) * 128], wA[:, k, c0:c0 + cn]) for k in range(8)],
                 R=[xT, wA], W=[p])
            S.cp(ev_eng(), o[:, c0:c0 + cn], p[:, 0:cn], R=[p], W=[o])
            c0 += cn
        S.dma(S.q_pool, pa[t * 128:(t + 1) * 128, :], o[:], R=[o], W=[pa])

    def stageF(j):
        xT = xnT[j % 2]
        r0 = 0
        while r0 < NB:
            rn = min(128, NB - r0)
            p = rr(G.ps, st, "ps")
            S.mm(p[0:rn, :], [(wB[:, k, r0:r0 + rn], xT[:, k, :]) for k in range(8)], R=[xT, wB], W=[p])
            o = rr(oB, st, "oB")
            S.cp(ev_eng(), o[0:rn, :], p[0:rn, :], R=[p], W=[o])
            S.dma(S.q_pool, pbT[r0:r0 + rn, j * 512:(j + 1) * 512], o[0:rn, :], R=[o], W=[pbT])
            r0 += rn

    stageX(0)
    for t in range(ntile):
        if t + 1 < ntile:
            stageX(t + 1)
        stageM(t)
        if t % 4 == 3:
            stageF(t // 4)
    sc.close()


def phase_lru(S, C, G, l, pbT, ybT, T):
    nc = S.nc
    sc = Scope(S)
    st = {}
    cvt = sc.sb("p3_cv", [128, NCV])
    S.dma(S.q_sync, cvt[:], C.cv[l], W=[cvt])
    gwt = sc.sb("p3_gw", [128, 4, 2, 128])
    S.dma(S.q_sync, gwt[:], C.gw[l].rearrange("c p g q -> p c g q"), W=[gwt])
    c8 = sc.sb("p3_c8", [128, 12])
    S.actf(c8[:, 0:4], cvt[:, CV_LAM:CV_LAM + 4], AF.Exp, R=[cvt], W=[c8], scale=-1.0)
    S.actf(c8[:, 4:8], c8[:, 0:4], AF.Ln, R=[c8], W=[c8], bias=1.0)
    S.ts(S.dve, c8[:, 8:12], c8[:, 4:8], -8.0, R=[c8], W=[c8])
    NB_ = 2
    mk = lambda n, w=512: [[sc.sb(f"p3_{n}{ct}_{i}", [128, w]) for i in range(NB_)] for ct in range(4)]
    ubs, xcs, grs, gis, a_s, oms, bxs, hos = mk("ub", 515), mk("xc"), mk("gr"), mk("gi"), mk("a"), mk("om"), mk("bx"), mk("ho")
    zero = sc.sb("p3_zero", [128, 1])
    S.memset(S.dve, zero[:], 0.0, W=[zero])
    nblk = T // 512
    cw = lambda j, ct: cvt[:, CV_CW + j * 4 + ct:CV_CW + j * 4 + ct + 1]
    col = lambda off, ct: cvt[:, off + ct:off + ct + 1]
    hprev = [(zero[:, 0:1], zero) for _ in range(4)]
    CT = range(4)
    for j in range(nblk):
        b_ = j % NB_
        ub = [ubs[ct][b_] for ct in CT]; xc = [xcs[ct][b_] for ct in CT]; gr = [grs[ct][b_] for ct in CT]
        gi = [gis[ct][b_] for ct in CT]; a = [a_s[ct][b_] for ct in CT]; om = [oms[ct][b_] for ct in CT]
        bx = [bxs[ct][b_] for ct in CT]; ho = [hos[ct][b_] for ct in CT]
        for ct in CT:
            r0 = OB_UB + ct * 128
            if j == 0:
                S.memset(S.pool, ub[ct][:, 0:3], 0.0, W=[ub[ct]])
                S.dma(S.q_sync, ub[ct][:, 3:515], pbT[r0:r0 + 128, 0:512], R=[pbT], W=[ub[ct]])
            else:
                S.dma(S.q_sync, ub[ct][:], pbT[r0:r0 + 128, j * 512 - 3:(j + 1) * 512], R=[pbT], W=[ub[ct]])
        for ct in CT:
            S.ts(S.dve, xc[ct][:], ub[ct][:, 0:512], cw(0, ct), col(CV_CB, ct), op0=ALU.mult, op1=ALU.add, R=[ub[ct], cvt], W=[xc[ct]])
        for jj in range(1, 4):
            for ct in CT:
                S.stt(S.dve, xc[ct][:], ub[ct][:, jj:jj + 512], cw(jj, ct), xc[ct][:], ALU.mult, ALU.add, R=[ub[ct], cvt, xc[ct]], W=[xc[ct]])
        ps1 = []; ps2 = []
        for ct in CT:
            p1 = rr(G.ps, st, "ps"); ps1.append(p1)
            S.mm(p1[:], [(gwt[:, ct, 0, :], xc[ct][:])], R=[gwt, xc[ct]], W=[p1])
            S.actf(gr[ct][:], p1[:], AF.Sigmoid, R=[p1, cvt], W=[gr[ct]], bias=col(CV_GAB, ct))
        for ct in CT:
            p2 = rr(G.ps, st, "ps"); ps2.append(p2)
            S.mm(p2[:], [(gwt[:, ct, 1, :], xc[ct][:])], R=[gwt, xc[ct]], W=[p2])
            S.actf(gi[ct][:], p2[:], AF.Sigmoid, R=[p2, cvt], W=[gi[ct]], bias=col(CV_GXB, ct))
        for ct in CT:
            S.actf(a[ct][:], gr[ct][:], AF.Exp, R=[gr[ct], c8], W=[a[ct]], scale=c8[:, 8 + ct:9 + ct])
        for ct in CT:
            S.tt(S.dve, bx[ct][:], gi[ct][:], xc[ct][:], ALU.mult, R=[gi[ct], xc[ct]], W=[bx[ct]])
        for ct in CT:
            S.actf(om[ct][:], a[ct][:], AF.Square, R=[a[ct]], W=[om[ct]])
        for ct in CT:
            S.actf(om[ct][:], om[ct][:], AF.Sqrt, R=[om[ct]], W=[om[ct]], scale=-1.0, bias=1.0)
        for ct in CT:
            S.tt(S.dve, bx[ct][:], bx[ct][:], om[ct][:], ALU.mult, R=[bx[ct], om[ct]], W=[bx[ct]])
        for ct in CT:
            hp, hpb = hprev[ct]
            S.op(S.dve, lambda ho=ho[ct], a=a[ct], bx=bx[ct], hp=hp: nc.vector.tensor_tensor_scan(
                out=ho[:], data0=a[:], data1=bx[:], initial=hp, op0=ALU.mult, op1=ALU.add),
                [a[ct], bx[ct], hpb], [ho[ct]])
            S.dma(S.q_pool, ybT[ct * 128:(ct + 1) * 128, j * 512:(j + 1) * 512], ho[ct][:], R=[ho[ct]], W=[ybT])
            hprev[ct] = (ho[ct][:, 511:512], ho[ct])
    sc.close()


def phase_mla(S, C, G, l, pbT, ycT, T):
    nc = S.nc
    sc = Scope(S)
    st = {}
    nblk = T // 512
    cvt = sc.sb("p4_cv", [128, NCV])
    S.dma(S.q_sync, cvt[:], C.cv[l], W=[cvt])
    wq = sc.sb("p4_wq", [128, 2, 1024], BF16)
    wqs = sc.sb("p4_wqs", [128, 2, 1024], BF16)
    wk = sc.sb("p4_wk", [128, 1024], BF16)
    wv = sc.sb("p4_wv", [128, 512], BF16)
    sc2 = Scope(S)
    stg = [sc2.sb(f"p4_stg{i}", [128, 1024]) for i in range(2)]
    jobs = [(C.wq[l][0:128, :], wq[:, 0, :], 1024, wq), (C.wq[l][128:256, :], wq[:, 1, :], 1024, wq),
            (C.wqs[l][0:128, :], wqs[:, 0, :], 1024, wqs), (C.wqs[l][128:256, :], wqs[:, 1, :], 1024, wqs),
            (C.wk[l], wk[:], 1024, wk), (C.wv[l], wv[:], 512, wv)]
    for n, (src, dst, ncol, dbuf) in enumerate(jobs):
        sg = stg[n % 2]
        S.dma(S.q_sync, sg[:, 0:ncol], src, W=[sg])
        S.cp(S.dve if n % 2 == 0 else S.act, dst, sg[:, 0:ncol], R=[sg], W=[dbuf])
    sc2.close()
    E65 = sc.sb("p4_E96", [128, 64])
    S.memset(S.dve, E65[:], 0.0, W=[E65])
    S.memset(S.dve, E65[64:65, :], 1.0, W=[E65])
    KT = [sc.sb(f"p4_KT{j}", [128, 8, 512], BF16) for j in range(nblk)]
    VP = [sc.sb(f"p4_VP{j}", [128, 4, 8, 96], BF16) for j in range(nblk)]
    for j in range(nblk):
        S.memset(S.pool, KT[j][32:64, :, :], 0.0, W=[KT[j]])
        S.memset(S.pool, VP[j][:, :, :, 64:96], 0.0, W=[VP[j]])
        S.memset(S.pool, VP[j][:, :, :, 64:65], 1.0, W=[VP[j]])
    QT = [[sc.sb(f"p4_QT{b}_{h}", [128, 512], BF16) for h in range(8)] for b in range(1)]
    cst = [sc.sb(f"p4_cs{i}", [128, 2, 512]) for i in range(1)]
    qls = [sc.sb(f"p4_ql{i}", [128, 2, 512]) for i in range(1)]
    kvls = [sc.sb(f"p4_kvl{i}", [128, 512]) for i in range(2)]
    krs = [sc.sb(f"p4_kr{i}", [32, 2, 512]) for i in range(2)]
    sq = sc.sb("p4_sq", [128, 2, 512])
    rq = sc.sb("p4_rq", [128, 512]); rk = sc.sb("p4_rk", [128, 512])
    qn = sc.sb("p4_qn", [128, 2, 512], BF16)
    ckv = sc.sb("p4_ckv", [128, 512], BF16)
    t1s = [sc.sb(f"p4_t1{i}", [128, 512]) for i in range(2)]
    t2s = [sc.sb(f"p4_t2{i}", [128, 512]) for i in range(2)]
    pTs = [sc.sb(f"p4_pT{i}", [128, 512], BF16) for i in range(4)]
    osbs = [sc.sb(f"p4_osb{i}", [96, 512]) for i in range(2)]
    rls = [sc.sb(f"p4_rl{i}", [64, 512]) for i in range(2)]
    ohs = [sc.sb(f"p4_oh{i}", [64, 512]) for i in range(2)]
    psS = G.ps[0:3]; psO = G.ps[3:5]; psL = G.ps[5]
    scale = 96.0 ** -0.5
    for j in range(nblk):
        blk = slice(j * 512, (j + 1) * 512)
        ql = rr(qls, st, "ql"); kvl = rr(kvls, st, "kvl"); kr = rr(krs, st, "kr"); cs = rr(cst, st, "cs")
        for k in range(2):
            S.dma(S.q_sync, ql[:, k, :], pbT[OB_QL + k * 128:OB_QL + (k + 1) * 128, blk], R=[pbT], W=[ql])
        S.dma(S.q_sync, kvl[:], pbT[OB_KV:OB_KV + 128, blk], R=[pbT], W=[kvl])
        S.dma(S.q_pool, kr[:, 0, :], pbT[OB_KR:OB_KR + 32, blk], R=[pbT], W=[kr])
        S.dma(S.q_pool, kr[:, 1, :], pbT[OB_KRS:OB_KRS + 32, blk], R=[pbT], W=[kr])
        S.dma(S.q_pool, cs[:], C.cs[:, :, blk], W=[cs])
        S.actf(sq[:], ql[:], AF.Square, R=[ql], W=[sq])
        pA = rr(psS, st, "psS")
        S.mm(pA[:], [(G.ones_f[:], sq[:, 0, :]), (G.ones_f[:], sq[:, 1, :])], R=[G.ones_f, sq], W=[pA])
        S.actf(rq[:], pA[:], AF.Ln, R=[pA], W=[rq], scale=1.0 / QL, bias=G.eps_col[:, 0:1])
        S.actf(rq[:], rq[:], AF.Exp, R=[rq], W=[rq], scale=-0.5)
        for k in range(2):
            S.stt(S.dve, qn[:, k, :], ql[:, k, :], cvt[:, CV_QNG + k:CV_QNG + k + 1], rq[:], ALU.mult, ALU.mult,
                  R=[ql, cvt, rq], W=[qn])
        S.actf(sq[:, 0, :], kvl[:], AF.Square, R=[kvl], W=[sq])
        pB = rr(psS, st, "psS")
        S.mm(pB[:], [(G.ones_f[:], sq[:, 0, :])], R=[G.ones_f, sq], W=[pB])
        S.actf(rk[:], pB[:], AF.Ln, R=[pB], W=[rk], scale=1.0 / KVL, bias=G.eps_col[:, 0:1])
        S.actf(rk[:], rk[:], AF.Exp, R=[rk], W=[rk], scale=-0.5)
        S.stt(S.dve, ckv[:], kvl[:], cvt[:, CV_KVNG:CV_KVNG + 1], rk[:], ALU.mult, ALU.mult, R=[kvl, cvt, rk], W=[ckv])
        t1 = rr(t1s, st, "t1"); t2 = rr(t2s, st, "t2")
        S.tt(S.dve, t1[0:32, :], kr[:, 0, :], cs[0:32, 0, :], ALU.mult, R=[kr, cs], W=[t1])
        S.tt(S.dve, t2[0:32, :], kr[:, 1, :], cs[0:32, 1, :], ALU.mult, R=[kr, cs], W=[t2])
        for h in range(8):
            S.tt(S.dve, KT[j][0:32, h, :], t1[0:32, :], t2[0:32, :], ALU.add, R=[t1, t2], W=[KT[j]])
        for h in range(8):
            Qh = QT[0][h]
            pq = rr(psS, st, "psS")
            S.mm(pq[:], [(wq[:, k, h * 128:(h + 1) * 128], qn[:, k, :]) for k in range(2)], R=[wq, qn], W=[pq])
            pqs = rr(psS, st, "psS")
            S.mm(pqs[:], [(wqs[:, k, h * 128:(h + 1) * 128], qn[:, k, :]) for k in range(2)], R=[wqs, qn], W=[pqs])
            t1 = rr(t1s, st, "t1"); t2 = rr(t2s, st, "t2")
            S.tt(S.dve, t1[:], pq[:], cs[:, 0, :], ALU.mult, R=[pq, cs], W=[t1])
            S.tt(S.dve, t2[:], pqs[:], cs[:, 1, :], ALU.mult, R=[pqs, cs], W=[t2])
            S.tt(S.dve, Qh[:], t1[:], t2[:], ALU.add, R=[t1, t2], W=[Qh])
            pk = rr(psS, st, "psS")
            S.mm(pk[:], [(wk[:, h * 128:(h + 1) * 128], ckv[:])], R=[wk, ckv], W=[pk])
            S.cp(S.act, KT[j][64:128, h, :], pk[64:128, :], R=[pk], W=[KT[j]])
        for tt in range(4):
            pv = rr(psS, st, "psS")
            S.mm(pv[:], [(ckv[:, tt * 128:(tt + 1) * 128], wv[:])], R=[ckv, wv], W=[pv])
            S.cp(S.dve, VP[j][:, tt, :, 0:64], pv[:].rearrange("p (h d) -> p h d", h=8),
                 R=[pv], W=[VP[j]])
        nkt = 4 * j + 4

        def issue_S(h, kt):
            i = kt - 4 * j
            c0 = 128 * max(i, 0)
            N = 512 - c0
            kb, ko = kt // 4, (kt % 4) * 128
            pS = rr(psS, st, "psS")
            S.mm(pS[:, 0:N], [(KT[kb][:, h, ko:ko + 128], QT[0][h][:, c0:512])], R=[KT[kb], QT[0][h]], W=[pS])
            return pS

        for hp in range(4):
            heads = (2 * hp, 2 * hp + 1)
            po = {h: psO[ii] for ii, h in enumerate(heads)}
            cur = {h: issue_S(h, 0) for h in heads}
            for kt in range(nkt):
                i = kt - 4 * j
                c0 = 128 * max(i, 0)
                N = 512 - c0
                kb = kt // 4
                nxt = {}
                pTh = {}
                for h in heads:
                    if kt + 1 < nkt:
                        nxt[h] = issue_S(h, kt + 1)
                    pT = rr(pTs, st, "pT"); pTh[h] = pT
                    S.actf(pT[:, 0:N], cur[h][:, 0:N], AF.Exp, R=[cur[h]], W=[pT], scale=scale)
                    if i >= 0:
                        S.tt(S.pool, pT[:, 0:128], pT[:, 0:128], G.mask_bf[:], ALU.mult, R=[pT, G.mask_bf], W=[pT])
                for h in heads:
                    S.mm(po[h][0:96, c0:512], [(VP[kb][:, kt % 4, h, :], pTh[h][:, 0:N])], R=[VP[kb], pTh[h]], W=[po[h]],
                         start=(kt == 0), stop=(kt == nkt - 1))
                cur = nxt
            for h in heads:
                osb = rr(osbs, st, "osb")
                S.cp(S.act, osb[:], po[h][0:96, :], R=[po[h]], W=[osb])
                S.mm(psL[0:64, :], [(E65[0:96, :], osb[0:96, :])], R=[E65, osb], W=[psL])
                rl = rr(rls, st, "rl")
                S.actf(rl[:], psL[0:64, :], AF.Ln, R=[psL], W=[rl])
                S.actf(rl[:], rl[:], AF.Exp, R=[rl], W=[rl], scale=-1.0)
                oh = rr(ohs, st, "oh")
                S.tt(S.dve, oh[:], osb[0:64, :], rl[:], ALU.mult, R=[osb, rl], W=[oh])
                S.dma(S.q_sync, ycT[h * 64:(h + 1) * 64, blk], oh[:], R=[oh], W=[ycT])
    sc.close()


INV_DT = BF16
CDEC = 0.6065306597126334
GN_EPS = 64e-5


def bc_mid(ap, n):
    return ap.unsqueeze(1).broadcast_to([ap.shape[0], n, ap.shape[1]])


def bc_last(ap, n):
    return ap.unsqueeze(2).broadcast_to([ap.shape[0], ap.shape[1], n])


def h3(ap):
    return ap.rearrange("p (h d) -> p h d", h=8)


def phase_rwkv(S, C, G, l, pa, yga, T, dbg=None):
    nc = S.nc
    sc = Scope(S)
    st = {}
    nch = T // 128
    rvt = sc.sb("p2_rv", [128, NRV])
    S.dma(S.q_sync, rvt[:], C.rv[l:l + 1, :].partition_broadcast(128), W=[rvt])
    w0a0 = sc.sb("p2_w0a0", [1, 1024])
    S.dma(S.q_sync, w0a0[:], C.w0a0[l:l + 1, :], W=[w0a0])
    w2a2 = sc.sb("p2_w2a2", [128, 512])
    S.dma(S.q_sync, w2a2[:], C.w2a2[l], W=[w2a2])
    mu_b = rvt[:, RV_MU:RV_MU + 1664]
    kk_b = rvt[:, RV_KK:RV_KK + 512]; ka_b = rvt[:, RV_KA:RV_KA + 512]; rk_b = rvt[:, RV_RK:RV_RK + 512]
    lg_b = rvt[:, RV_LG:RV_LG + 512]; lb_b = rvt[:, RV_LB:RV_LB + 512]
    M_strict, M_incl, M_lower = G.masks[:, 0, :], G.masks[:, 1, :], G.masks[:, 2, :]
    ident_i = G.ident_f if INV_DT == F32 else G.ident_bf
    f32t = lambda n: sc.sb(f"p2_{n}", [128, 512])
    b16t = lambda n: sc.sb(f"p2_{n}", [128, 512], BF16)
    uas = [sc.sb(f"p2_ua{i}", [128, 1664]) for i in range(2)]
    ups = [sc.sb(f"p2_up{i}", [128, 1664]) for i in range(2)]
    zas = [f32t(f"za{i}") for i in range(2)]
    Tm = Ctx()
    Tm.lw = sc.sb("p2_lw", [128, 128])
    Tm.sgw, Tm.av, Tm.gam, Tm.ig, Tm.gae = f32t("sgw"), f32t("av"), f32t("gam"), f32t("ig"), f32t("gae")
    Tm.kk, Tm.sqt, Tm.kkn, Tm.e1, Tm.knew, Tm.rkt = f32t("kk"), f32t("sqt"), f32t("kkn"), f32t("e1"), f32t("knew"), f32t("rkt")
    Tm.ssq = sc.sb("p2_ssq", [128, 32])

    def outset(i):
        O = Ctx()
        O.kap_b, O.ktl_b, O.btl_b, O.rtl_b, O.v_b = [b16t(f"{n}{i}") for n in ("kapb", "ktlb", "btlb", "rtlb", "vb")]
        O.gC = sc.sb(f"p2_gC{i}", [64, 8])
        O.bon, O.sz = f32t(f"bon{i}"), f32t(f"sz{i}")
        return O

    def a2set(i):
        Z = Ctx()
        Z.KR = sc.sb(f"p2_KR{i}", [64, 8, 2, 128], BF16)
        Z.KB = sc.sb(f"p2_KB{i}", [64, 8, 2, 128], BF16)
        Z.AK = sc.sb(f"p2_AK{i}", [128, 8, 2, 128], BF16)
        Z.AB2 = sc.sb(f"p2_AB2{i}", [128, 8, 128], BF16)
        Z.Qs = [[sc.sb(f"p2_Q{i}_{k}_{q}", [128, 4, 128], INV_DT) for q in range(2)] for k in range(2)]
        Z.QTs = [[sc.sb(f"p2_QT{i}_{k}_{q}", [128, 4, 128], INV_DT) for q in range(2)] for k in range(2)]
        Z.Ys = [[sc.sb(f"p2_Y{i}_{k}_{q}", [128, 4, 128], INV_DT) for q in range(2)] for k in range(2)]
        return Z

    OS = [outset(i) for i in range(3)]
    ZS = [a2set(i) for i in range(2)]
    Bt = Ctx()
    Bt.R_sb = sc.sb("p2_R", [128, 512], INV_DT); Bt.negU = b16t("negU")
    Bt.yt, Bt.yc, Bt.sq2 = f32t("yt"), f32t("yc"), f32t("sq2")
    Bt.ygo = [f32t(f"ygo{i}") for i in range(2)]
    Bt.ssb = sc.sb("p2_ssb", [128, 32])
    Hf = sc.sb("p2_Hf", [64, 512]); Hb = sc.sb("p2_Hb", [64, 512], BF16); Htmp = sc.sb("p2_Htmp", [64, 512])
    S.memset(S.dve, Hf[:], 0.0, W=[Hf]); S.memset(S.dve, Hb[:], 0.0, W=[Hb])

    def stageA1(c):
        X = Tm; O = OS[c % 3]
        ua, up, za = uas[c % 2], ups[c % 2], zas[c % 2]
        rows = slice(c * 128, (c + 1) * 128)
        S.dma(S.q_sync, ua[:], pa[rows, 0:1664], R=[pa], W=[ua])
        S.dma(S.q_sync, za[:], pa[rows, 1664:2176], R=[pa], W=[za])
        if c == 0:
            S.memset(S.pool, up[0:1, :], 0.0, W=[up])
            S.dma(S.q_sync, up[1:128, :], pa[0:127, 0:1664], R=[pa], W=[up])
        else:
            S.dma(S.q_sync, up[:], pa[c * 128 - 1:c * 128 + 127, 0:1664], R=[pa], W=[up])
        yield
        S.tt(S.dve, up[:], up[:], ua[:], ALU.subtract, R=[up, ua], W=[up])
        yield
        S.tt(S.dve, up[:], up[:], mu_b, ALU.mult, R=[up, rvt], W=[up])
        yield
        S.tt(S.dve, up[:], up[:], ua[:], ALU.add, R=[up, ua], W=[up])
        yield
        um = up
        r_, k_, v_ = um[:, 0:512], um[:, 512:1024], um[:, 1024:1536]
        pl_ = rr(G.ps, st, "ps")
        S.tr([(pl_[:, 0:128], um[:, 1536:1664])], G.ident_f[:], R=[um, G.ident_f], W=[pl_])
        S.actf(X.lw[0:64, :], pl_[0:64, 0:128], AF.Tanh, R=[pl_], W=[X.lw])
        S.cp(S.act, X.lw[64:128, :], pl_[64:128, 0:128], R=[pl_], W=[X.lw])
        S.cp(S.act, O.v_b[:], v_, R=[um], W=[O.v_b])
        S.actf(O.sz[:], za[:], AF.Silu, R=[za], W=[O.sz])
        yield
        pzw = rr(G.ps, st, "ps"); pza = rr(G.ps, st, "ps")
        S.mm(pzw[:], [(X.lw[0:64, :], w2a2[0:64, :]), (G.ones_f[0:1, :], w0a0[0:1, 0:512])], R=[X.lw, w2a2, w0a0, G.ones_f], W=[pzw])
        S.mm(pza[:], [(X.lw[64:128, :], w2a2[64:128, :]), (G.ones_f[0:1, :], w0a0[0:1, 512:1024])], R=[X.lw, w2a2, w0a0, G.ones_f], W=[pza])
        S.actf(X.sgw[:], pzw[:], AF.Sigmoid, R=[pzw], W=[X.sgw])
        S.actf(X.av[:], pza[:], AF.Sigmoid, R=[pza], W=[X.av])
        yield
        pci = rr(G.ps, st, "ps"); pce = rr(G.ps, st, "ps"); pgc = rr(G.ps, st, "ps")
        S.mm(pci[:], [(M_incl, X.sgw[:])], R=[G.masks, X.sgw], W=[pci])
        S.mm(pce[:], [(M_strict, X.sgw[:])], R=[G.masks, X.sgw], W=[pce])
        sgw = X.sgw
        S.group(S.pe, [(lambda h=h: nc.tensor.matmul(pgc[0:64, h:h + 1], lhsT=sgw[:, h * 64:(h + 1) * 64], rhs=G.ones_f[:, 0:1],
                                                      start=True, stop=True)) for h in range(8)], [X.sgw, G.ones_f], [pgc])
        yield
        S.actf(X.gam[:], pci[:], AF.Exp, R=[pci], W=[X.gam], scale=-CDEC)
        S.actf(X.ig[:], pci[:], AF.Exp, R=[pci], W=[X.ig], scale=CDEC)
        S.actf(X.gae[:], pce[:], AF.Exp, R=[pce], W=[X.gae], scale=-CDEC)
        S.actf(O.gC[:], pgc[0:64, 0:8], AF.Exp, R=[pgc], W=[O.gC], scale=-CDEC)
        yield
        ssq = X.ssq
        S.tt(S.dve, X.kk[:], k_, kk_b, ALU.mult, R=[um, rvt], W=[X.kk])
        S.actf(X.sqt[:], X.kk[:], AF.Square, R=[X.kk], W=[X.sqt])
        yield
        S.op(S.dve, lambda: nc.vector.tensor_reduce(out=ssq[:, 0:8], in_=h3(X.sqt[:]), axis=AX.X, op=ALU.add), [X.sqt], [ssq])
        S.actf(ssq[:, 8:16], ssq[:, 0:8], AF.Sqrt, R=[ssq], W=[ssq])
        S.ts(S.dve, ssq[:, 8:16], ssq[:, 8:16], 1e-12, op0=ALU.max, R=[ssq], W=[ssq])
        S.op(S.dve, lambda: nc.vector.reciprocal(out=ssq[:, 16:24], in_=ssq[:, 8:16]), [ssq], [ssq])
        yield
        S.tt(S.dve, h3(X.kkn[:]), h3(X.kk[:]), bc_last(ssq[:, 16:24], 64), ALU.mult, R=[X.kk, ssq], W=[X.kkn])
        yield
        S.stt(S.dve, X.e1[:], X.av[:], -1.0, ka_b, ALU.add, ALU.mult, R=[X.av, rvt], W=[X.e1])
        yield
        S.stt(S.dve, X.knew[:], X.e1[:], 1.0, k_, ALU.add, ALU.mult, R=[X.e1, um], W=[X.knew])
        yield
        S.tt(S.dve, O.kap_b[:], X.kkn[:], X.gae[:], ALU.mult, R=[X.kkn, X.gae], W=[O.kap_b])
        yield
        S.tt(S.dve, O.ktl_b[:], X.knew[:], X.ig[:], ALU.mult, R=[X.knew, X.ig], W=[O.ktl_b])
        yield
        S.tt(S.dve, X.e1[:], X.kkn[:], X.av[:], ALU.mult, R=[X.kkn, X.av], W=[X.e1])
        yield
        S.tt(S.dve, O.btl_b[:], X.e1[:], X.ig[:], ALU.mult, R=[X.e1, X.ig], W=[O.btl_b])
        yield
        S.tt(S.dve, O.rtl_b[:], r_, X.gam[:], ALU.mult, R=[um, X.gam], W=[O.rtl_b])
        yield
        S.tt(S.dve, X.rkt[:], r_, X.knew[:], ALU.mult, R=[um, X.knew], W=[X.rkt])
        yield
        S.tt(S.dve, X.rkt[:], X.rkt[:], rk_b, ALU.mult, R=[X.rkt, rvt], W=[X.rkt])
        S.op(S.dve, lambda: nc.vector.tensor_reduce(out=ssq[:, 24:32], in_=h3(X.rkt[:]), axis=AX.X, op=ALU.add), [X.rkt], [ssq])
        yield
        S.tt(S.dve, h3(O.bon[:]), h3(v_), bc_last(ssq[:, 24:32], 64), ALU.mult, R=[um, ssq], W=[O.bon])
        yield

    def stageA2(c):
        O = OS[c % 3]; Z = ZS[c % 2]
        for (src, dst, wi) in ((O.kap_b, Z.KR, 0), (O.rtl_b, Z.KR, 1), (O.ktl_b, Z.KB, 0), (O.btl_b, Z.KB, 1)):
            pb = rr(G.psb, st, "psb")
            S.tr([(pb[0:64, h * 128:(h + 1) * 128], src[:, h * 64:(h + 1) * 64]) for h in range(8)], G.ident_bf[:],
                 R=[src, G.ident_bf], W=[pb])
            S.cp(S.act, dst[:, :, wi, :], pb[0:64, :].rearrange("p (h t) -> p h t", h=8), R=[pb], W=[dst])
            yield
        KR, KB, AK, AB2 = Z.KR, Z.KB, Z.AK, Z.AB2
        Q, QT, Y = Z.Qs[0], Z.QTs[0], Z.Ys[0]
        for hp in range(4):
            p1 = rr(G.ps, st, "ps"); p2 = rr(G.ps, st, "ps")
            for hh in range(2):
                h = hp * 2 + hh
                S.mm(p1[:, hh * 256:(hh + 1) * 256], [(KB[:, h, 0, :], KR[:, h, :, :])], R=[KB, KR], W=[p1])
                S.mm(p2[:, hh * 256:(hh + 1) * 256], [(KB[:, h, 1, :], KR[:, h, :, :])], R=[KB, KR], W=[p2])
            hs = slice(hp * 2, hp * 2 + 2)
            p1v = p1[:].rearrange("p (h w t) -> p h w t", h=2, w=2)
            p2v = p2[:].rearrange("p (h w t) -> p h w t", h=2, w=2)
            for hh in range(2):
                h = hp * 2 + hh
                S.tt(S.dve, AK[:, h, :, :], p1v[:, hh, :, :], G.masks[:, 0:2, :], ALU.mult, R=[p1, G.masks], W=[AK])
            yield
            S.tt(S.dve, AB2[:, hs, :], p2v[:, :, 1, :], bc_mid(M_incl, 2), ALU.mult, R=[p2, G.masks], W=[AB2])
            hq_ = hp // 2; ls = slice((hp % 2) * 2, (hp % 2) * 2 + 2)
            S.stt(S.dve, QT[hq_][:, ls, :], p2v[:, :, 0, :], -1.0, bc_mid(M_strict, 2), ALU.mult, ALU.mult, R=[p2, G.masks], W=[QT[hq_]])
            S.tt(S.dve, Y[hq_][:, ls, :], QT[hq_][:, ls, :], bc_mid(ident_i[:], 2), ALU.add, R=[QT[hq_], ident_i], W=[Y[hq_]])
            yield
        for hq in range(2):
            p3 = rr(G.ps, st, "ps")
            for hh in range(4):
                h = hq * 4 + hh
                S.mm(p3[:, hh * 128:(hh + 1) * 128], [(KR[:, h, 0, :], KB[:, h, 1, :])], R=[KR, KB], W=[p3])
            hs = slice(hq * 4, hq * 4 + 4)
            S.stt(S.dve, Q[hq][:, :, :], p3[:].rearrange("p (h t) -> p h t", h=4), -1.0, bc_mid(M_lower, 4), ALU.mult, ALU.mult,
                  R=[p3, G.masks], W=[Q[hq]])
            yield
        k_ = 0
        for lev in range(1, 7):
            k_ ^= 1
            Qn, QTn, Yn = Z.Qs[k_], Z.QTs[k_], Z.Ys[k_]
            last = lev == 6
            for hq in range(2):
                hs = slice(hq * 4, hq * 4 + 4)
                pq = rr(G.ps, st, "ps")
                for hh in range(4):
                    h = hq * 4 + hh
                    S.mm(pq[:, hh * 128:(hh + 1) * 128], [(QT[hq][:, hh, :], Q[hq][:, hh, :])], R=[QT[hq], Q[hq]], W=[pq])
                S.cp(S.act, Qn[hq][:, :, :], pq[:].rearrange("p (h t) -> p h t", h=4), R=[pq], W=[Qn[hq]])
                if not last:
                    pqt = rr(G.ps, st, "ps")
                    for hh in range(4):
                        h = hq * 4 + hh
                        S.mm(pqt[:, hh * 128:(hh + 1) * 128], [(Q[hq][:, hh, :], QT[hq][:, hh, :])], R=[QT[hq], Q[hq]], W=[pqt])
                    S.cp(S.act, QTn[hq][:, :, :], pqt[:].rearrange("p (h t) -> p h t", h=4), R=[pqt], W=[QTn[hq]])
                yield
            for hq in range(2):
                hs = slice(hq * 4, hq * 4 + 4)
                py = rr(G.ps, st, "ps")
                for hh in range(4):
                    h = hq * 4 + hh
                    S.mm(py[:, hh * 128:(hh + 1) * 128], [(Qn[hq][:, hh, :], Y[hq][:, hh, :])], R=[Qn[hq], Y[hq]], W=[py])
                S.tt(S.dve, Yn[hq][:, :, :], py[:].rearrange("p (h t) -> p h t", h=4), Y[hq][:, :, :], ALU.add, R=[py, Y[hq]], W=[Yn[hq]])
                yield
            Q, QT, Y = Qn, QTn, Yn
        Z.Yfin = Y

    def stageB(c):
        O = OS[c % 3]; Z = ZS[c % 2]
        KR, AK, AB2, Y = Z.KR, Z.AK, Z.AB2, Z.Yfin
        v_b, negU, R_sb, ssb = O.v_b, Bt.negU, Bt.R_sb, Bt.ssb
        rows = slice(c * 128, (c + 1) * 128)
        pR = rr(G.ps, st, "ps")
        for h in range(8):
            cs_ = slice(h * 64, (h + 1) * 64)
            S.mm(pR[:, cs_], [(KR[:, h, 0, :], Hb[:, cs_]), (AK[:, h, 0, :], v_b[:, cs_])], R=[KR, Hb, AK, v_b], W=[pR])
        S.cp(S.act, R_sb[:], pR[:], R=[pR], W=[R_sb])
        yield
        pU = rr(G.ps, st, "ps")
        for h in range(8):
            cs_ = slice(h * 64, (h + 1) * 64)
            S.mm(pU[:, cs_], [(Y[h // 4][:, h % 4, :], R_sb[:, cs_])], R=[Y[h // 4], R_sb], W=[pU])
        S.actf(negU[:], pU[:], AF.Copy, R=[pU], W=[negU], scale=-1.0)
        yield
        pH = rr(G.ps, st, "ps")
        for h in range(8):
            cs_ = slice(h * 64, (h + 1) * 64)
            S.mm(pH[0:64, cs_], [(O.ktl_b[:, cs_], v_b[:, cs_]), (O.btl_b[:, cs_], negU[:, cs_])], R=[O.ktl_b, v_b, O.btl_b, negU], W=[pH])
        pY = rr(G.ps, st, "ps")
        for h in range(8):
            cs_ = slice(h * 64, (h + 1) * 64)
            S.mm(pY[:, cs_], [(KR[:, h, 1, :], Hb[:, cs_]), (AK[:, h, 1, :], v_b[:, cs_]), (AB2[:, h, :], negU[:, cs_])],
                 R=[KR, Hb, AK, v_b, AB2, negU], W=[pY])
        S.cp(S.act, Bt.yt[:], pY[:], R=[pY], W=[Bt.yt])
        S.tt(S.dve, Htmp[:], pH[0:64, :], Hf[:], ALU.add, R=[pH, Hf], W=[Htmp])
        S.tt(S.dve, h3(Hf[:]), h3(Htmp[:]), bc_last(O.gC[:], 64), ALU.mult, R=[Htmp, O.gC], W=[Hf])
        S.cp(S.act, Hb[:], Hf[:], R=[Hf], W=[Hb])
        yield
        yt, yc, sq2 = Bt.yt, Bt.yc, Bt.sq2
        S.op(S.dve, lambda: nc.vector.tensor_reduce(out=ssb[:, 8:16], in_=h3(yt[:]), axis=AX.X, op=ALU.add), [yt], [ssb])
        S.ts(S.dve, ssb[:, 8:16], ssb[:, 8:16], -1.0 / 64, R=[ssb], W=[ssb])
        S.tt(S.dve, h3(yc[:]), h3(yt[:]), bc_last(ssb[:, 8:16], 64), ALU.add, R=[yt, ssb], W=[yc])
        S.actf(sq2[:], yc[:], AF.Square, R=[yc], W=[sq2])
        yield
        S.op(S.dve, lambda: nc.vector.tensor_reduce(out=ssb[:, 16:24], in_=h3(sq2[:]), axis=AX.X, op=ALU.add), [sq2], [ssb])
        S.actf(ssb[:, 24:32], ssb[:, 16:24], AF.Sqrt, R=[ssb], W=[ssb], scale=1.0 / 64, bias=G.eps_col[:, 1:2])
        S.op(S.dve, lambda: nc.vector.reciprocal(out=ssb[:, 16:24], in_=ssb[:, 24:32]), [ssb], [ssb])
        S.tt(S.dve, h3(yc[:]), h3(yc[:]), bc_last(ssb[:, 16:24], 64), ALU.mult, R=[yc, ssb], W=[yc])
        yield
        S.tt(S.dve, yc[:], yc[:], lg_b, ALU.mult, R=[yc, rvt], W=[yc])
        S.tt(S.dve, yc[:], yc[:], lb_b, ALU.add, R=[yc, rvt], W=[yc])
        yield
        S.tt(S.dve, yc[:], yc[:], O.bon[:], ALU.add, R=[yc, O.bon], W=[yc])
        ygo = Bt.ygo[c % 2]
        S.tt(S.dve, ygo[:], yc[:], O.sz[:], ALU.mult, R=[yc, O.sz], W=[ygo])
        S.dma(S.q_sync, yga[rows, :], ygo[:], R=[ygo], W=[yga])
        yield

    def drain(g):
        for _ in g:
            pass

    def step(g):
        if g is None:
            return None
        try:
            next(g)
            return g
        except StopIteration:
            return None

    drain(stageA1(0))
    if nch > 1:
        drain(stageA1(1))
    drain(stageA2(0))
    for c in range(nch):
        gB = stageB(c)
        gA2 = stageA2(c + 1) if c + 1 < nch else None
        gA1 = stageA1(c + 2) if c + 2 < nch else None
        while gB is not None or gA2 is not None or gA1 is not None:
            gA2 = step(gA2)
            gA1 = step(gA1)
            gA2 = step(gA2)
            gA1 = step(gA1)
            gA2 = step(gA2)
            gB = step(gB)
    sc.close()


def phase_out(S, C, G, l, hsrc, hdst, pbT, yga, ybT, ycT, T, final):
    nc = S.nc
    sc = Scope(S)
    st = {}
    nblk = T // 512
    cvt = sc.sb("p5_cv", [128, NCV])
    S.dma(S.q_sync, cvt[:], C.cv[l], W=[cvt])
    wo = sc.sb("p5_wo", [128, 12, 1024], BF16)
    sc2 = Scope(S)
    stg = [sc2.sb(f"p5_stg{i}", [128, 1024]) for i in range(2)]
    for k in range(12):
        sg = stg[k % 2]
        S.dma(S.q_sync if k % 2 == 0 else S.q_pool, sg[:], C.w_out[l][k * 128:(k + 1) * 128, :], W=[sg])
        S.cp(S.dve if k % 2 == 0 else S.act, wo[:, k, :], sg[:], R=[sg], W=[wo])
    sc2.close()
    if final:
        fg = sc.sb("p5_fg", [128, D])
        S.dma(S.q_sync, fg[:], C.final_g[0:1, :].partition_broadcast(128), W=[fg])
        junk = sc.sb("p5_junk", [128, D], BF16)
        ssf = [sc.sb(f"p5_ssf{i}", [128, 4]) for i in range(2)]
    ygT = [sc.sb(f"p5_ygT{i}", [128, 12, 512], BF16) for i in range(2)]
    ybs = [sc.sb(f"p5_yb{i}", [128, 512]) for i in range(5)]
    zbs = [sc.sb(f"p5_zb{i}", [128, 512]) for i in range(3)]
    sqs = [sc.sb(f"p5_sq{i}", [128, 512]) for i in range(2)]
    rstd = [sc.sb(f"p5_rstd{i}", [128, 512]) for i in range(2)]
    t1s = [sc.sb(f"p5_t1{i}", [128, 512]) for i in range(2)]
    t2s = [sc.sb(f"p5_t2{i}", [128, 512]) for i in range(2)]
    yas = [sc.sb(f"p5_ya{i}", [128, 512]) for i in range(2)]
    yabs = [sc.sb(f"p5_yab{i}", [128, 512], BF16) for i in range(2)]
    hts = [sc.sb(f"p5_h{i}", [128, D]) for i in range(2)]
    hos = [sc.sb(f"p5_ho{i}", [128, D]) for i in range(2)]
    def fin(j):
        blk = slice(j * 512, (j + 1) * 512)
        yg = ygT[j % 2]
        for (src, zoff, goff, kbase) in ((ybT, OB_ZB, CV_LOG, 4), (ycT, OB_ZC, CV_MOG, 8)):
            ys = []
            pS = rr(G.ps, st, "ps")
            for ct in range(4):
                yb = rr(ybs, st, "yb"); ys.append(yb)
                S.dma(S.q_sync, yb[:], src[ct * 128:(ct + 1) * 128, blk], R=[src], W=[yb])
                sq = rr(sqs, st, "sq")
                S.actf(sq[:], yb[:], AF.Square, R=[yb], W=[sq])
                S.mm(pS[:], [(G.ones_f[:], sq[:])], R=[G.ones_f, sq], W=[pS], start=(ct == 0), stop=(ct == 3))
            rs = rr(rstd, st, "rstd")
            S.actf(rs[:], pS[:], AF.Ln, R=[pS], W=[rs], scale=1.0 / 512, bias=G.eps_col[:, 0:1])
            S.actf(rs[:], rs[:], AF.Exp, R=[rs], W=[rs], scale=-0.5)
            for ct in range(4):
                zb = rr(zbs, st, "zb")
                S.dma(S.q_pool, zb[:], pbT[zoff + ct * 128:zoff + (ct + 1) * 128, blk], R=[pbT], W=[zb])
                t1 = rr(t1s, st, "t1"); t2 = rr(t2s, st, "t2")
                S.actf(t1[:], zb[:], AF.Silu, R=[zb], W=[t1])
                S.stt(S.dve, t2[:], ys[ct][:], cvt[:, goff + ct:goff + ct + 1], rs[:], ALU.mult, ALU.mult, R=[ys[ct], cvt, rs], W=[t2])
                S.tt(S.dve, yg[:, kbase + ct, :], t1[:], t2[:], ALU.mult, R=[t1, t2], W=[yg])
        for tt in range(4):
            t = 4 * j + tt
            ya = rr(yas, st, "ya"); yab = rr(yabs, st, "yab")
            S.dma(S.q_sync, ya[:], yga[t * 128:(t + 1) * 128, :], R=[yga], W=[ya])
            S.cp(S.act, yab[:], ya[:], R=[ya], W=[yab])
            pb = rr(G.psb, st, "psb")
            S.tr([(pb[:, ct * 128:(ct + 1) * 128], yab[:, ct * 128:(ct + 1) * 128]) for ct in range(4)], G.ident_bf[:],
                 R=[yab, G.ident_bf], W=[pb])
            S.cp(S.act, yg[:, 0:4, tt * 128:(tt + 1) * 128], pb[:, 0:512].rearrange("p (c t) -> p c t", c=4), R=[pb], W=[yg])

    def outproj(j):
        yg = ygT[j % 2]
        for tt in range(4):
            t = 4 * j + tt
            ht = rr(hts, st, "h"); ho = rr(hos, st, "ho")
            S.dma(S.q_sync, ht[:], hsrc[t * 128:(t + 1) * 128, :], R=[hsrc], W=[ht])
            for half in range(2):
                cs_ = slice(half * 512, (half + 1) * 512)
                pO = rr(G.ps, st, "ps")
                S.mm(pO[:], [(yg[:, k, tt * 128:(tt + 1) * 128], wo[:, k, cs_]) for k in range(12)], R=[yg, wo], W=[pO])
                S.tt(S.dve, ho[:, cs_], pO[:], ht[:, cs_], ALU.add, R=[pO, ht], W=[ho])
            if final:
                s_ = rr(ssf, st, "ssf")
                S.actf(junk[:], ho[:], AF.Square, R=[ho], W=[junk, s_], accum=s_[:, 0:1])
                S.actf(s_[:, 1:2], s_[:, 0:1], AF.Sqrt, R=[s_], W=[s_], scale=1.0 / D, bias=G.eps_col[:, 0:1])
                S.op(S.dve, lambda a=s_: nc.vector.reciprocal(out=a[:, 2:3], in_=a[:, 1:2]), [s_], [s_])
                S.stt(S.dve, ht[:], ho[:], s_[:, 2:3], fg[:], ALU.mult, ALU.mult, R=[ho, s_, fg], W=[ht])
                S.dma(S.q_pool, hdst[t * 128:(t + 1) * 128, :], ht[:], R=[ht], W=[hdst])
            else:
                S.dma(S.q_pool, hdst[t * 128:(t + 1) * 128, :], ho[:], R=[ho], W=[hdst])

    fin(0)
    for j in range(nblk):
        if j + 1 < nblk:
            fin(j + 1)
        outproj(j)
    sc.close()


def build(T, L, phases=None, debug=False):
    nc = bass.Bass("TRN2", target_bir_lowering=False)
    S = Sched(nc)
    C = declare_inputs(S, T, L)
    G = Ctx()
    load_consts(S, C, G)
    G.eps_col = S.sb("eps_col", [128, 2])
    S.memset(S.dve, G.eps_col[:, 0:1], EPS, W=[G.eps_col])
    S.memset(S.dve, G.eps_col[:, 1:2], GN_EPS, W=[G.eps_col])
    full = phases is None
    if full:
        phases = ("p1", "p2", "p3", "p4", "p5")
    dbg = lambda name: "ExternalOutput" if (debug and name in phases) else "Internal"
    pa = S.dram("pa", [T, NA], F32, kind=dbg("p1"))
    pbT = S.dram("pbT", [NB, T], F32, kind=dbg("p1"))
    ybT = S.dram("ybT", [512, T], F32, kind=dbg("p3"))
    ycT = S.dram("ycT", [512, T], F32, kind=dbg("p4"))
    yga = S.dram("yga", [T, 512], F32, kind=dbg("p2"))
    hb = [S.dram(f"hbuf{i}", [T, D], F32) for i in range(2)]
    hout = S.dram("hout", [T, D], F32, kind="ExternalOutput" if (full or "p5" in phases) else "Internal")
    outs = []
    nl = L if full else 1
    for l in range(nl):
        hsrc = C.x if l == 0 else hb[(l - 1) % 2]
        last = l == nl - 1
        hdst = hout if last else hb[l % 2]
        phase_in_proj(S, C, G, l, hsrc, pa, pbT, T)
        if "p2" in phases: phase_rwkv(S, C, G, l, pa, yga, T)
        if "p3" in phases: phase_lru(S, C, G, l, pbT, ybT, T)
        if "p4" in phases: phase_mla(S, C, G, l, pbT, ycT, T)
        if "p5" in phases: phase_out(S, C, G, l, hsrc, hdst, pbT, yga, ybT, ycT, T, final=(full and last))
    if debug:
        for nm, b in (("p1", pa), ("p1", pbT), ("p3", ybT), ("p4", ycT), ("p2", yga)):
            if nm in phases: outs.append(b)
    if full or "p5" in phases: outs.append(hout)
    S.finish(outs)
    emit_all(S)
    return nc, S

from concourse.bass_utils import run_bass_kernel_spmd

T_FULL = 4096
L_FULL = 4
_IN_NAMES = ["x", "wA", "wB", "rv", "w0a0", "w2a2", "cv", "gw", "wq", "wqs", "wk", "wv", "w_out", "final_g",
             "ident_bf", "ident_f", "cs", "masks", "mask_bf"]
_NC_CACHE = {}


def kernel(**inputs):
    x = np.asarray(inputs["x"], np.float32)
    B, T, _ = x.shape
    L = np.asarray(inputs["w_in"]).shape[0]
    hp = host_prep(inputs, T)
    key = (T, L)
    if key not in _NC_CACHE:
        _NC_CACHE[key] = build(T, L)[0]
    nc = _NC_CACHE[key]
    in_maps = []
    for b in range(B):
        m = {k: hp[k] for k in _IN_NAMES if k != "x"}
        m["x"] = np.ascontiguousarray(x[b])
        in_maps.append(m)
    res = run_bass_kernel_spmd(nc, in_maps, core_ids=list(range(B)))
    return np.stack([np.asarray(r["hout"], np.float32) for r in res.results], axis=0)
```
